# Optimizing a Trainium2 kernel written in Bass

```python
import jax
import jax.numpy as jnp
from jax import lax
import numpy as np

D_MODEL = 1024
BATCH = 2
SEQ = 8192
DEPTH = 2

GRID_W = 64
CTX_LEN = 256
N_MIXERS = 2
N_FOURIER_LAYERS = (DEPTH + 1) // 2
N_HGRN_LAYERS = DEPTH // 2
FNET_GROUPS = 4
FNET_GROUP_DIM = D_MODEL // FNET_GROUPS
HGRN_HEADS = 8
HGRN_KEY_DIM = 128
HGRN_VALUE_DIM = D_MODEL // HGRN_HEADS
HGRN_F_DIM = HGRN_HEADS * HGRN_KEY_DIM
HGRN_CHUNK = 64
N_EXPERTS = 32
TOP_K = 4
D_FF = 1024
SWIGLU_ALPHA = 1.702
SWIGLU_LIMIT = 7.0
MOE_BLOCK = 256
NORM_EPS = 1e-6

kernel_name = 'hybrid_fnet_hgrn2_moe_dit'


def rms_norm(x, g):
    xf = x.astype(jnp.float32)
    y = xf * lax.rsqrt(jnp.mean(xf * xf, axis=-1, keepdims=True) + NORM_EPS)
    return (y * g.astype(jnp.float32)).astype(x.dtype)


def adaln(cond, w, b):
    m = jax.nn.silu(cond) @ w + b
    return jnp.split(m[..., None, :], 6, axis=-1)


def modulate(x, g, shift, scale):
    return rms_norm(x, g) * (1 + scale) + shift


def fourier_tokens(u, grid):
    bsz, length, _ = u.shape
    uf = u.astype(jnp.float32)
    if grid is None:
        ug = uf.reshape(bsz, length, FNET_GROUPS, FNET_GROUP_DIM)
        y = jnp.fft.fftn(ug, axes=(1, 3), norm='ortho').real
    else:
        rows, cols = grid
        ug = uf.reshape(bsz, rows, cols, FNET_GROUPS, FNET_GROUP_DIM)
        y = jnp.fft.fftn(ug, axes=(1, 2, 4), norm='ortho').real
    return y.reshape(bsz, length, D_MODEL).astype(u.dtype)


def fourier_mixer(h_ctx, h_lat, w_in, w_out, grid, need_ctx):
    y_lat = fourier_tokens(h_lat @ w_in, grid) @ w_out
    y_ctx = fourier_tokens(h_ctx @ w_in, None) @ w_out if need_ctx else None
    return y_ctx, y_lat


def gla_chunk_scan(q, k, v, log_f, s0):
    bsz, length, heads, _ = q.shape
    n_chunks = length // HGRN_CHUNK

    def chunks(t):
        return t.reshape(bsz, n_chunks, HGRN_CHUNK, heads, t.shape[-1]).transpose(1, 0, 3, 2, 4)

    lower_tri = jnp.tril(jnp.ones((HGRN_CHUNK, HGRN_CHUNK), dtype=bool))

    def step(s, blk):
        qc, kc, vc, ac = blk
        b = jnp.cumsum(ac, axis=2)
        b_last = b[:, :, -1:, :]
        diff = b[:, :, :, None, :] - b[:, :, None, :, :]
        decay = jnp.where(lower_tri[:, :, None], jnp.exp(jnp.minimum(diff, 0.0)), 0.0)
        scores = jnp.einsum('bhtk,bhsk,bhtsk->bhts', qc, kc, decay)
        o = (jnp.einsum('bhts,bhsv->bhtv', scores, vc)
             + jnp.einsum('bhtk,bhkv->bhtv', qc * jnp.exp(b), s))
        s_new = (jnp.exp(b_last[:, :, 0, :])[..., None] * s
                 + jnp.einsum('bhsk,bhsv->bhkv', kc * jnp.exp(b_last - b), vc))
        return s_new, o

    s_fin, o = lax.scan(step, s0, (chunks(q), chunks(k), chunks(v), chunks(log_f)))
    o = o.transpose(1, 0, 3, 2, 4).reshape(bsz, length, heads, v.shape[-1])
    return o, s_fin


def bidir_scan(q, k_fw, a_fw, k_bw, a_bw, v, s_fw, s_bw):
    o_fw, s_fw = gla_chunk_scan(q, k_fw, v, a_fw, s_fw)
    flip = lambda t: t[:, ::-1]
    o_bw, s_bw = gla_chunk_scan(flip(q), flip(k_bw), flip(v), flip(a_bw), s_bw)
    return o_fw + flip(o_bw), s_fw, s_bw


def hgrn2_mixer(h_ctx, h_lat, w_in, lb, norm_g, w_out, need_ctx):
    splits = [HGRN_F_DIM, 2 * HGRN_F_DIM, 3 * HGRN_F_DIM, 3 * HGRN_F_DIM + D_MODEL]

    def project(h):
        bsz, length, _ = h.shape
        q, f_fw, f_bw, i_in, g = jnp.split(h @ w_in, splits, axis=-1)
        heads = lambda t: t.reshape(bsz, length, HGRN_HEADS, -1)
        fg_fw = lb[0] + (1 - lb[0]) * jax.nn.sigmoid(f_fw.astype(jnp.float32))
        fg_bw = lb[1] + (1 - lb[1]) * jax.nn.sigmoid(f_bw.astype(jnp.float32))
        scan_in = (heads(jax.nn.silu(q).astype(jnp.float32)),
                   heads(1 - fg_fw), heads(jnp.log(fg_fw)),
                   heads(1 - fg_bw), heads(jnp.log(fg_bw)),
                   heads(i_in.astype(jnp.float32)))
        return scan_in, g

    def readout(o, g):
        bsz, length = o.shape[:2]
        on = (o * lax.rsqrt(jnp.mean(o * o, axis=-1, keepdims=True) + NORM_EPS)
              * norm_g.astype(jnp.float32).reshape(HGRN_HEADS, HGRN_VALUE_DIM))
        y = on.reshape(bsz, length, D_MODEL).astype(g.dtype) * jax.nn.silu(g)
        return y @ w_out

    ctx_in, g_ctx = project(h_ctx)
    lat_in, g_lat = project(h_lat)
    s0 = jnp.zeros((h_lat.shape[0], HGRN_HEADS, HGRN_KEY_DIM, HGRN_VALUE_DIM), jnp.float32)
    o_ctx, s_fw, s_bw = bidir_scan(*ctx_in, s0, s0)
    o_lat, _, _ = bidir_scan(*lat_in, s_fw, s_bw)
    y_lat = readout(o_lat, g_lat)
    y_ctx = readout(o_ctx, g_ctx) if need_ctx else None
    return y_ctx, y_lat


def moe_ffn(h, w_r, b_r, w1, b1, w2, b2):
    n_tok = h.shape[0]
    logits = (h @ w_r + b_r).astype(jnp.float32)
    top_val, top_idx = lax.top_k(logits, TOP_K)
    gate = jax.nn.softmax(top_val, axis=-1)
    n_assign = n_tok * TOP_K
    flat_e = top_idx.reshape(-1)
    order = jnp.argsort(flat_e)
    e_sorted = flat_e[order]
    tok_sorted = order // TOP_K
    gate_sorted = gate.reshape(-1)[order]
    counts = jnp.bincount(flat_e, length=N_EXPERTS)
    padded = (counts + MOE_BLOCK - 1) // MOE_BLOCK * MOE_BLOCK
    pad_end = jnp.cumsum(padded)
    pad_start = pad_end - padded
    start = jnp.cumsum(counts) - counts
    dest = pad_start[e_sorted] + jnp.arange(n_assign) - start[e_sorted]
    n_blocks = -(-n_assign // MOE_BLOCK) + N_EXPERTS
    n_rows = n_blocks * MOE_BLOCK
    row_tok = jnp.zeros((n_rows,), jnp.int32).at[dest].set(tok_sorted.astype(jnp.int32))
    row_gate = jnp.zeros((n_rows,), jnp.float32).at[dest].set(gate_sorted)
    block_exp = jnp.minimum(
        jnp.searchsorted(pad_end, jnp.arange(n_blocks) * MOE_BLOCK, side='right'), N_EXPERTS - 1)
    xb = h[row_tok].reshape(n_blocks, MOE_BLOCK, h.shape[-1])

    def expert_block(args):
        xblk, e = args
        u = xblk @ w1[e] + b1[e]
        glu, lin = jnp.split(u, 2, axis=-1)
        glu = jnp.minimum(glu, SWIGLU_LIMIT)
        lin = jnp.clip(lin, -SWIGLU_LIMIT, SWIGLU_LIMIT)
        act = glu * jax.nn.sigmoid(SWIGLU_ALPHA * glu) * (lin + 1)
        return act @ w2[e] + b2[e]

    yb = lax.map(expert_block, (xb, block_exp)).reshape(n_rows, -1)
    y = yb * row_gate[:, None].astype(yb.dtype)
    return jax.ops.segment_sum(y, row_tok, num_segments=n_tok)


def setup_inputs(seed: int = 0) -> dict:
    key = jax.random.key(seed)
    ks = jax.random.split(key, 21)
    nrm = lambda k, shape, scale: jax.random.normal(k, shape, jnp.float32) * scale
    D = D_MODEL
    return {
        'x': nrm(ks[0], (BATCH, SEQ, D), 1.0),
        'c': nrm(ks[1], (BATCH, D), 1.0),
        'ctx': nrm(ks[2], (BATCH, CTX_LEN, D), 1.0),
        'c_ctx': nrm(ks[3], (D,), 1.0),
        'mod_w': nrm(ks[4], (DEPTH, D, 6 * D), 0.5 * D ** -0.5),
        'mod_b': nrm(ks[5], (DEPTH, 6 * D), 0.02),
        'norm1_g': 1.0 + nrm(ks[6], (DEPTH, D), 0.02),
        'norm2_g': 1.0 + nrm(ks[7], (DEPTH, D), 0.02),
        'fourier_w_in': nrm(ks[8], (N_FOURIER_LAYERS, D, D), D ** -0.5),
        'fourier_w_out': nrm(ks[9], (N_FOURIER_LAYERS, D, D), D ** -0.5),
        'hgrn_w_in': nrm(ks[10], (N_HGRN_LAYERS, D, 3 * HGRN_F_DIM + 2 * D), D ** -0.5),
        'hgrn_lower_bounds': nrm(ks[11], (DEPTH, 2, HGRN_F_DIM), 0.5),
        'hgrn_norm_g': 1.0 + nrm(ks[12], (N_HGRN_LAYERS, D), 0.02),
        'hgrn_w_out': nrm(ks[13], (N_HGRN_LAYERS, D, D), D ** -0.5),
        'router_w': nrm(ks[14], (DEPTH, D, N_EXPERTS), D ** -0.5),
        'router_b': nrm(ks[15], (DEPTH, N_EXPERTS), 0.01),
        'expert_w1': nrm(ks[16], (DEPTH, N_EXPERTS, D, 2 * D_FF), D ** -0.5),
        'expert_b1': nrm(ks[17], (DEPTH, N_EXPERTS, 2 * D_FF), 0.01),
        'expert_w2': nrm(ks[18], (DEPTH, N_EXPERTS, D_FF, D), D_FF ** -0.5),
        'expert_b2': nrm(ks[19], (DEPTH, N_EXPERTS, D), 0.01),
        'final_norm_g': 1.0 + nrm(ks[20], (D,), 0.02),
    }


def reference(x, c, ctx, c_ctx, mod_w, mod_b, norm1_g, norm2_g, fourier_w_in, fourier_w_out,
              hgrn_w_in, hgrn_lower_bounds, hgrn_norm_g, hgrn_w_out, router_w, router_b,
              expert_w1, expert_b1, expert_w2, expert_b2, final_norm_g):
    lb_soft = jax.nn.softmax(hgrn_lower_bounds.astype(jnp.float32), axis=0)
    lower_bounds = jnp.cumsum(lb_soft, axis=0) - lb_soft[0]
    rows = x.shape[1] // GRID_W
    grid = (rows, GRID_W)
    x_lat, x_ctx = x, ctx
    for i in range(DEPTH):
        need_ctx = i < DEPTH - 1
        sh1, sc1, g1, sh2, sc2, g2 = adaln(c, mod_w[i], mod_b[i])
        csh1, csc1, cg1, csh2, csc2, cg2 = adaln(c_ctx, mod_w[i], mod_b[i])
        h_lat = modulate(x_lat, norm1_g[i], sh1, sc1)
        h_ctx = modulate(x_ctx, norm1_g[i], csh1, csc1)
        j = i // N_MIXERS
        if i % N_MIXERS == 0:
            y_ctx, y_lat = fourier_mixer(h_ctx, h_lat, fourier_w_in[j], fourier_w_out[j],
                                         grid, need_ctx)
        else:
            y_ctx, y_lat = hgrn2_mixer(h_ctx, h_lat, hgrn_w_in[j], lower_bounds[i],
                                       hgrn_norm_g[j], hgrn_w_out[j], need_ctx)
        x_lat = x_lat + g1 * y_lat
        h_lat = modulate(x_lat, norm2_g[i], sh2, sc2)
        moe_args = (router_w[i], router_b[i], expert_w1[i], expert_b1[i],
                    expert_w2[i], expert_b2[i])
        if need_ctx:
            x_ctx = x_ctx + cg1 * y_ctx
            h_ctx = modulate(x_ctx, norm2_g[i], csh2, csc2)
            n_ctx = x_ctx.shape[0] * x_ctx.shape[1]
            tokens = jnp.concatenate([h_ctx.reshape(-1, D_MODEL), h_lat.reshape(-1, D_MODEL)], axis=0)
            out = moe_ffn(tokens, *moe_args)
            x_ctx = x_ctx + cg2 * out[:n_ctx].reshape(x_ctx.shape)
            x_lat = x_lat + g2 * out[n_ctx:].reshape(x_lat.shape)
        else:
            out = moe_ffn(h_lat.reshape(-1, D_MODEL), *moe_args)
            x_lat = x_lat + g2 * out.reshape(x_lat.shape)
    return rms_norm(x_lat, final_norm_g)
```

```python
import numpy as np
from contextlib import ExitStack, contextmanager
import concourse.bass as bass
import concourse.mybir as mybir
from concourse.bass_utils import run_bass_kernel_spmd

F32 = mybir.dt.float32
BF16 = mybir.dt.bfloat16
AF = mybir.ActivationFunctionType
ALU = mybir.AluOpType
AX = mybir.AxisListType

D = 1024
KC = 8
NE = 32
EPS = 1e-6
NCORES = 8


class Sched:
    ENG = ("pe", "dve", "act", "pool", "sp")

    def __init__(self, nc, n_dma_sems=48):
        self.nc = nc
        self.e = {"pe": nc.tensor, "dve": nc.vector, "act": nc.scalar,
                  "pool": nc.gpsimd, "sp": nc.sync}
        self.sem = {k: nc.alloc_semaphore("sem_" + k) for k in self.ENG}
        self.cnt = {k: 0 for k in self.ENG}
        self.dsem = [nc.alloc_semaphore("dsem%d" % i) for i in range(n_dma_sems)]
        self.dval = [0] * n_dma_sems
        self.dnext = 0
        self.n_hw = (n_dma_sems * 3) // 4
        self.dnext_sw = self.n_hw
        self.seen = {k: {} for k in self.ENG}
        self.lastw = {}
        self.readers = {}
        self.multiw = {}
        self.genreaders = {}
        self.pslock = {}
        self.know = {}
        self.ccsems = []
        self.cctoks = []

    def _wait(self, eng, tok):
        key, sem, val = tok
        if key == "pe" and eng == "pe":
            return
        if self.seen[eng].get(key, 0) >= val:
            return
        self.e[eng].wait_ge(sem, val)
        self.seen[eng][key] = val
        snap = self.know.get((key, val))
        if snap:
            mine = self.seen[eng]
            for k2, v2 in snap.items():
                if mine.get(k2, 0) < v2:
                    mine[k2] = v2

    def _deps(self, eng, reads, writes):
        for r in reads:
            if r.startswith("+"):
                for t in self.multiw.get(r, {}).values():
                    self._wait(eng, t)
                continue
            t = self.lastw.get(r)
            if t is not None:
                self._wait(eng, t)
        for w in writes:
            if not w.startswith("+"):
                t = self.lastw.get(w)
                if t is not None:
                    self._wait(eng, t)
            else:
                for t in self.genreaders.get(w, {}).values():
                    self._wait(eng, t)
            for t in self.readers.get(w, {}).values():
                self._wait(eng, t)

    def _record(self, tok, reads, writes):
        for w in writes:
            if w.startswith("+"):
                if self.readers.get(w):
                    self.multiw[w] = {}
                    self.genreaders[w] = dict(self.readers[w])
                self.multiw.setdefault(w, {})[tok[0]] = tok
            else:
                self.lastw[w] = tok
            self.readers[w] = {}
        for r in reads:
            if r in writes:
                continue
            self.readers.setdefault(r, {})[tok[0]] = tok

    def op(self, eng, emit, reads=(), writes=()):
        self._deps(eng, reads, writes)
        for r in reads:
            if r.startswith("ps"):
                t = self.pslock.get(r)
                if t is not None and t[0] != eng:
                    self._wait(eng, t)
        ins = emit(self.e[eng])
        self.cnt[eng] += 1
        ins.then_inc(self.sem[eng], 1)
        tok = (eng, self.sem[eng], self.cnt[eng])
        self.know[(eng, self.cnt[eng])] = dict(self.seen[eng])
        self._record(tok, reads, writes)
        for r in list(reads) + list(writes):
            if r.startswith("ps"):
                self.pslock[r] = tok
        return tok

    def dma(self, q, out, in_, reads=(), writes=()):
        self._deps(q, reads, writes)
        if q == "pool":
            i = self.dnext_sw
            self.dnext_sw = self.n_hw + (i + 1 - self.n_hw) % (len(self.dsem) - self.n_hw)
        else:
            i = self.dnext
            self.dnext = (i + 1) % self.n_hw
        key = "d%d" % i
        if self.dval[i] > 0:
            self._wait(q, (key, self.dsem[i], self.dval[i]))
        self.dval[i] += 16
        self.e[q].dma_start(out=out, in_=in_).then_inc(self.dsem[i], 16)
        tok = (key, self.dsem[i], self.dval[i])
        self.know[(key, self.dval[i])] = dict(self.seen[q])
        self._record(tok, reads, writes)
        return tok

    def collective(self, kind, ins, outs, groups, reads=(), writes=()):
        q = "pool"
        self._deps(q, reads, writes)
        sem = self.nc.alloc_semaphore("ccsem%d" % len(self.ccsems))
        self.ccsems.append(sem)
        self.e[q].collective_compute(kind, ALU.bypass, replica_groups=groups, ins=ins, outs=outs).then_inc(sem, 1)
        tok = ("cc%d" % len(self.ccsems), sem, 1)
        self._record(tok, reads, writes)
        self.cctoks.append(tok)
        return tok

    def barrier(self, engines=None):
        engines = engines or self.ENG
        for eng in engines:
            for f in self.ENG:
                if self.cnt[f] > 0:
                    self._wait(eng, (f, self.sem[f], self.cnt[f]))
            for i, v in enumerate(self.dval):
                if v > 0:
                    self._wait(eng, ("d%d" % i, self.dsem[i], v))
            for tok in self.cctoks:
                self._wait(eng, tok)


def ps_banks(nc):
    return [nc.alloc_psum_tensor("psb%d" % i, [128, 512], F32) for i in range(8)]


class Ctx:
    def __init__(self, nc):
        self.nc = nc
        self.s = Sched(nc)
        self.ps = ps_banks(nc)
        self.uid = 0
        self.stacks = []
        a = lambda name, shape, dt: nc.alloc_sbuf_tensor(name, shape, dt)
        self.ident = a("ident_sb", [128, 128], F32)
        self.ones_row = a("ones_row", [1, 128], F32)
        self.eps_col = a("eps_col", [128, 1], F32)
        self.one_col = a("one_col", [128, 1], F32)

    def name(self, base):
        self.uid += 1
        return "%s_%d" % (base, self.uid)

    @contextmanager
    def scope(self):
        st = ExitStack()
        self.stacks.append(st)
        try:
            yield
        finally:
            self.s.barrier()
            self.s.lastw = {}
            self.s.readers = {}
            self.s.multiw = {}
            self.s.genreaders = {}
            self.s.know = {}
            self.stacks.pop()
            st.close()

    def A(self, name, shape, dt):
        g = self.nc.sbuf_tensor(self.name(name), shape, dt)
        return self.stacks[-1].enter_context(g)


def load_consts(cx, ident_dram):
    s = cx.s
    s.dma("sp", cx.ident[:], ident_dram, writes=["ident"])
    s.op("dve", lambda e: e.memset(cx.ones_row[:], 1.0), writes=["ones_row"])
    s.op("dve", lambda e: e.memset(cx.eps_col[:], EPS), writes=["eps_col"])
    s.op("dve", lambda e: e.memset(cx.one_col[:], 1.0), writes=["one_col"])


def emit_rstd(cx, x_ap, rows, junk, ss, rstd, xkey, tag):
    s = cx.s
    s.op("act", lambda e: e.activation(out=junk[0:rows, :], in_=x_ap, func=AF.Square,
                                       accum_out=ss[0:rows, :]),
         reads=[xkey, "eps_col"], writes=[tag + "junk", tag + "ss"])
    s.op("act", lambda e: e.activation(out=rstd[0:rows, :], in_=ss[0:rows, :], func=AF.Ln,
                                       bias=cx.eps_col[0:rows, :], scale=1.0 / D),
         reads=[tag + "ss", "eps_col"], writes=[tag + "rstd"])
    s.op("act", lambda e: e.activation(out=rstd[0:rows, :], in_=rstd[0:rows, :], func=AF.Exp,
                                       scale=-0.5),
         reads=[tag + "rstd"], writes=[tag + "rstd"])


def emit_transpose8(cx, src, rows, bank_a, bank_b, srckey):
    s = cx.s
    pa, pb = cx.ps[bank_a], cx.ps[bank_b]

    def emit_half(e, bank, k0):
        ins = None
        for q in range(4):
            kc = k0 + q
            ins = e.transpose(out=bank[:, q * 128:q * 128 + rows],
                              in_=src[0:rows, kc * 128:(kc + 1) * 128],
                              identity=cx.ident[0:rows, 0:rows])
        return ins
    s.op("pe", lambda e: emit_half(e, pa, 0), reads=[srckey, "ident"], writes=["ps%d" % bank_a])
    s.op("pe", lambda e: emit_half(e, pb, 4), reads=[srckey, "ident"], writes=["ps%d" % bank_b])


def emit_mod_cols(cx, modw, modb_row, silu_cols, ncond, col_blocks, out_cols, tag):
    s = cx.s
    with cx.scope():
        wbuf = [cx.A("mcw", [128, KC, 128], F32) for _ in range(2)]
        for i, blk in enumerate(col_blocks):
            wb = wbuf[i % 2]
            wk = tag + "mcw%d" % (i % 2)
            s.dma("sp", wb[:], modw[:, blk * 128:(blk + 1) * 128].rearrange("(kc p) n -> p kc n", p=128),
                  writes=[wk])
            bank = 4 + (i % 4)

            def emit(e, wb=wb, blk=blk, bank=bank):
                pt = cx.ps[bank]
                for kc in range(KC):
                    e.matmul(pt[:, 0:ncond], wb[:, kc, :], silu_cols[:, kc, :], start=(kc == 0), stop=False)
                return e.matmul(pt[:, 0:ncond], modb_row[0:1, blk * 128:(blk + 1) * 128],
                                cx.ones_row[0:1, 0:ncond], start=False, stop=True)
            s.op("pe", emit, reads=[wk, tag + "silu", tag + "modb", "ones_row"], writes=["ps%d" % bank])
            s.op("dve", lambda e, i=i, bank=bank: e.tensor_copy(out_cols[:, i, :], cx.ps[bank][:, 0:ncond]),
                 reads=["ps%d" % bank], writes=["+" + tag + "cols"])


def emit_mod_rows(cx, modw, modb_row, silu_rep, ncond, col0, out_rep, tag, okey):
    s = cx.s
    with cx.scope():
        wbuf = [cx.A("mrw", [128, KC, 512], F32) for _ in range(2)]
        for h in range(2):
            wb = wbuf[h]
            wk = tag + "mrw%d" % h
            c0 = col0 + h * 512
            s.dma("sp", wb[:], modw[:, c0:c0 + 512].rearrange("(kc p) n -> p kc n", p=128), writes=[wk])
            for c in range(ncond):
                bank = 4 + ((2 * h + c) % 4)

                def emit(e, wb=wb, c=c, c0=c0, bank=bank):
                    pt = cx.ps[bank]
                    for kc in range(KC):
                        e.matmul(pt[:, :], silu_rep[:, c, kc, :], wb[:, kc, :], start=(kc == 0), stop=False)
                    return e.matmul(pt[:, :], cx.ones_row[0:1, :], modb_row[0:1, c0:c0 + 512],
                                    start=False, stop=True)
                s.op("pe", emit, reads=[wk, tag + "silurep", tag + "modb", "ones_row"], writes=["ps%d" % bank])
                s.op("dve", lambda e, c=c, h=h, bank=bank: e.tensor_copy(
                    out_rep[:, c, h * 512:(h + 1) * 512], cx.ps[bank][:, :]),
                    reads=["ps%d" % bank], writes=[okey])


def emit_silu_cond(cx, cond_cols_dram, ncond, tag, need_rep):
    s = cx.s
    cc = cx.A("condc", [128, KC, ncond], F32)
    sg = cx.A("conds", [128, KC, ncond], F32)
    ck, sk = tag + "silu", tag + "sg"
    s.dma("sp", cc[:], cond_cols_dram, writes=[ck])
    s.op("act", lambda e: e.activation(out=sg[:], in_=cc[:], func=AF.Exp, scale=-1.0), reads=[ck], writes=[sk])
    s.op("dve", lambda e: e.tensor_scalar(out=sg[:], in0=sg[:], scalar1=1.0, scalar2=None, op0=ALU.add),
         reads=[sk], writes=[sk])
    s.op("dve", lambda e: e.reciprocal(out=sg[:], in_=sg[:]), reads=[sk], writes=[sk])
    s.op("dve", lambda e: e.tensor_tensor(out=cc[:], in0=cc[:], in1=sg[:], op=ALU.mult),
         reads=[sk, ck], writes=[ck])
    rep = None
    if need_rep:
        rep = cx.A("condrep", [128, ncond, KC, 128], F32)
        s.op("pool", lambda e: e.memset(rep[:], 1.0), writes=[tag + "silurep"])
        for c in range(ncond):
            for kc in range(KC):
                s.op("dve", lambda e, c=c, kc=kc: e.tensor_scalar(
                    out=rep[:, c, kc, :], in0=rep[:, c, kc, :], scalar1=cc[:, kc, c:c + 1], scalar2=None,
                    op0=ALU.mult), reads=[ck, tag + "silurep"], writes=[tag + "silurep"])
    return cc, rep


def emit_post(cx, tiles, ncond, dr, final_norm, n_exp=NE, tagp="P"):
    s = cx.s
    A = cx.A
    T = sum(r for _, r, _ in tiles)
    nt = len(tiles)
    col0 = []
    c = 0
    for _, r, _ in tiles:
        col0.append(c)
        c += r
    tg = tagp
    ngr = -(-T // 512)
    gsz = -(-T // ngr)
    groups = []
    c = 0
    while c < T:
        w = min(gsz, T - c)
        groups.append((c, w))
        c += w

    with cx.scope():
        XA = A("XA", [128, nt, D], F32)
        H2T = A("H2T", [128, KC, T], BF16)
        gates = A("gates", [128, nt, n_exp], F32)
        g2rep = A("g2rep", [128, ncond, D], F32)
        tmpa = [A("tmpa", [128, 512], F32) for _ in range(2)]
        junk = A("junk", [128, D], F32)
        ss = A("ss", [128, 1], F32)
        rstd = A("rstd", [128, 1], F32)
        b1c = A("b1c", [128, n_exp * 16], F32)
        s.dma("sp", b1c[:], dr["b1c"], writes=[tg + "b1c"])
        b1c1 = A("b1c1", [128, n_exp * 16], F32)
        s.op("dve", lambda e: e.tensor_scalar(out=b1c1[:], in0=b1c[:], scalar1=1.0, scalar2=None, op0=ALU.add),
             reads=[tg + "b1c"], writes=[tg + "b1c1"])
        if final_norm:
            finrep = A("finrep", [128, D], F32)
            s.dma("sp", finrep[:], dr["fin_rep"], writes=[tg + "finrep"])

        with cx.scope():
            modb_row = A("modb", [1, 6 * D], F32)
            s.dma("sp", modb_row[:], dr["modb"], writes=[tg + "modb"])
            silu_cols, silu_rep = emit_silu_cond(cx, dr["cond_cols"], ncond, tg, True)
            m2 = A("m2cols", [128, 16, ncond], F32)
            emit_mod_cols(cx, dr["modw"], modb_row, silu_cols, ncond, list(range(24, 40)), m2, tg)
            n2g = A("n2g", [128, KC], F32)
            s.dma("sp", n2g[:], dr["n2g_col"], writes=[tg + "n2g"])
            A2 = A("A2", [128, KC, ncond], F32)
            for c in range(ncond):
                s.op("dve", lambda e, c=c: e.scalar_tensor_tensor(
                    out=A2[:, :, c], in0=m2[:, 8:16, c], scalar=1.0, in1=n2g[:, :], op0=ALU.add, op1=ALU.mult),
                    reads=["+" + tg + "cols", tg + "n2g"], writes=[tg + "A2"])
            g1rep = A("g1rep", [128, ncond, D], F32)
            emit_mod_rows(cx, dr["modw"], modb_row, silu_rep, ncond, 2 * D, g1rep, tg + "g1", tg + "g1rep")
            emit_mod_rows(cx, dr["modw"], modb_row, silu_rep, ncond, 5 * D, g2rep, tg + "g2", tg + "g2rep")

            wout = A("wout", [128, KC, D], BF16)
            s.dma("pool", wout[:], dr["wout"].rearrange("(kc p) n -> p kc n", p=128), writes=[tg + "wout"])
            wr = A("wr", [128, KC, n_exp], F32)
            s.dma("sp", wr[:], dr["wr"].rearrange("(kc p) n -> p kc n", p=128), writes=[tg + "wr"])
            br = A("br", [1, n_exp], F32)
            s.dma("sp", br[:], dr["br"], writes=[tg + "br"])
            b2 = A("b2", [n_exp, D], F32)
            s.dma("sp", b2[:], dr["b2"], writes=[tg + "b2"])

            yt = [A("yt", [128, D], F32) for _ in range(2)]
            tmpf = [[A("tmpf", [128, 512], F32) for _ in range(2)] for _ in range(2)]
            ssf = [A("ssf", [128, 1], F32) for _ in range(2)]
            rstdf = [A("rstdf", [128, 1], F32) for _ in range(2)]
            junkf = [A("junkf", [128, D], BF16) for _ in range(2)]
            if "ypre_cands" in dr:
                cand = [[A("cand", [128, 4, 256], BF16) for _ in range(4)] for _ in range(2)]
                sel = A("sel", [128, 4], F32)
                s.dma("sp", sel[:], dr["sel"], writes=[tg + "sel"])
            xt = [A("xt", [128, D], F32) for _ in range(2)]
            xn = [A("xn", [128, D], F32) for _ in range(2)]
            ypT = [A("ypT", [128, KC, 128], BF16) for _ in range(2)]
            h2f = [A("h2f", [128, KC, 128], F32) for _ in range(2)]
            lg = [A("lg", [128, n_exp], F32) for _ in range(2)]
            mx8 = [A("mx8", [128, 8], F32) for _ in range(2)]
            negm = [A("negm", [128, 1], F32) for _ in range(2)]
            msk = [A("msk", [128, n_exp], F32) for _ in range(2)]
            ex = [A("ex", [128, n_exp], F32) for _ in range(2)]
            ssum = [A("ssum", [128, 1], F32) for _ in range(2)]
            gT = [A("gT", [n_exp, 128], F32) for _ in range(2)]

            def front_gen(ti, row0, rows, cond):
                b = ti % 2
                B0 = 4 * b
                ytk, xtk, xnk, ypk, h2k = (tg + "yt%d" % b, tg + "xt%d" % b, tg + "xn%d" % b,
                                           "+" + tg + "ypT%d" % b, "+" + tg + "h2f%d" % b)
                xak = tg + "XA%d" % ti
                R = slice(0, rows)
                if "ypre_cands" in dr:
                    cands = dr["ypre_cands"](row0, rows)
                    for kq, cap in enumerate(cands):
                        s.dma("sp", cand[b][kq][R, :, :], cap, reads=dr.get("ypre_keys", []), writes=[tg + "cand%d_%d" % (b, kq)])
                    s.op("dve", lambda e, b=b, R=R: e.tensor_scalar(
                        out=yt[b][R, :], in0=cand[b][0][R, :, :].rearrange("p g c -> p (g c)"), scalar1=sel[R, 0:1],
                        scalar2=None, op0=ALU.mult), reads=[tg + "cand%d_0" % b, tg + "sel"], writes=[ytk])
                    yield
                    for kq in range(1, 4):
                        s.op("dve", lambda e, b=b, R=R, kq=kq: e.scalar_tensor_tensor(
                            out=yt[b][R, :], in0=cand[b][kq][R, :, :].rearrange("p g c -> p (g c)"),
                            scalar=sel[R, kq:kq + 1], in1=yt[b][R, :], op0=ALU.mult, op1=ALU.add),
                            reads=[tg + "cand%d_%d" % (b, kq), tg + "sel", ytk], writes=[ytk])
                        yield
                else:
                    s.dma("sp", yt[b][R, :], dr["ypre"][row0:row0 + rows, :], writes=[ytk])
                xsrc = dr["xres_tile"](row0, rows) if "xres_tile" in dr else dr["xres"][row0:row0 + rows, :]
                s.dma("sp", xt[b][R, :], xsrc, writes=[xtk])
                emit_transpose8(cx, yt[b], rows, B0, B0 + 1, ytk)
                yield
                s.op("act", lambda e, b=b, rows=rows: e.copy(
                    out=ypT[b][:, 0:4, 0:rows],
                    in_=cx.ps[B0][:, :].rearrange("p (q t) -> p q t", q=4)[:, :, 0:rows]),
                    reads=["ps%d" % B0], writes=[ypk])
                yield
                s.op("dve", lambda e, b=b, rows=rows: e.tensor_copy(
                    ypT[b][:, 4:8, 0:rows], cx.ps[B0 + 1][:, :].rearrange("p (q t) -> p q t", q=4)[:, :, 0:rows]),
                    reads=["ps%d" % (B0 + 1)], writes=[ypk])
                yield
                for h in range(2):
                    bank = B0 + 2 + h
                    H = slice(h * 512, (h + 1) * 512)

                    def emit(e, b=b, H=H, bank=bank, rows=rows):
                        ins = None
                        for kc in range(KC):
                            ins = e.matmul(cx.ps[bank][0:rows, :], ypT[b][:, kc, 0:rows], wout[:, kc, H],
                                           start=(kc == 0), stop=(kc == KC - 1))
                        return ins
                    s.op("pe", emit, reads=[ypk, tg + "wout"], writes=["ps%d" % bank])
                    yield
                    tk = tg + "tmpf%d_%d" % (b, h)
                    s.op("dve", lambda e, h=h, H=H, bank=bank, R=R, cond=cond: e.tensor_tensor(
                        out=tmpf[b][h][R, :], in0=cx.ps[bank][R, :], in1=g1rep[R, cond, H], op=ALU.mult),
                        reads=["ps%d" % bank, tg + "g1rep"], writes=[tk])
                    yield
                    s.op("pool", lambda e, h=h, H=H, R=R, b=b, ti=ti: e.tensor_tensor(
                        out=XA[R, ti, H], in0=tmpf[b][h][R, :], in1=xt[b][R, H], op=ALU.add),
                        reads=[tk, xtk], writes=[xak])
                    yield
                emit_rstd(cx, XA[R, ti, :], rows, junkf[b], ssf[b], rstdf[b], xak, tg + "f%d" % b)
                yield
                s.op("dve", lambda e, R=R, b=b, ti=ti: e.tensor_scalar(
                    out=xn[b][R, :], in0=XA[R, ti, :], scalar1=rstdf[b][R, :], scalar2=None, op0=ALU.mult),
                    reads=[xak, tg + "f%d" % b + "rstd"], writes=[xnk])
                yield
                emit_transpose8(cx, xn[b], rows, B0, B0 + 1, xnk)
                yield
                for kc in range(KC):
                    bank = B0 + kc // 4
                    q = kc % 4
                    if kc % 2 == 0:
                        s.op("act", lambda e, b=b, kc=kc, q=q, bank=bank, rows=rows, cond=cond: e.activation(
                            out=h2f[b][:, kc, 0:rows], in_=cx.ps[bank][:, q * 128:q * 128 + rows],
                            func=AF.Identity, bias=m2[:, kc, cond:cond + 1], scale=A2[:, kc, cond:cond + 1]),
                            reads=["ps%d" % bank, tg + "A2", "+" + tg + "cols"], writes=[h2k])
                        yield
                    else:
                        s.op("dve", lambda e, b=b, kc=kc, q=q, bank=bank, rows=rows, cond=cond: e.tensor_scalar(
                            out=h2f[b][:, kc, 0:rows], in0=cx.ps[bank][:, q * 128:q * 128 + rows],
                            scalar1=A2[:, kc, cond:cond + 1], scalar2=m2[:, kc, cond:cond + 1],
                            op0=ALU.mult, op1=ALU.add),
                            reads=["ps%d" % bank, tg + "A2", "+" + tg + "cols"], writes=[h2k])
                        yield
                c0 = col0[ti]
                s.op("pool", lambda e, b=b, rows=rows, c0=c0: e.tensor_copy(
                    H2T[:, :, c0:c0 + rows], h2f[b][:, :, 0:rows]), reads=[h2k], writes=[tg + "H2T%d" % ti])
                yield

                def emit_r(e, b=b, rows=rows):
                    for kc in range(KC):
                        e.matmul(cx.ps[B0 + 2][0:rows, 0:n_exp], h2f[b][:, kc, 0:rows], wr[:, kc, :],
                                 start=(kc == 0), stop=False)
                    return e.matmul(cx.ps[B0 + 2][0:rows, 0:n_exp], cx.ones_row[0:1, 0:rows], br[0:1, :],
                                    start=False, stop=True)
                s.op("pe", emit_r, reads=[h2k, tg + "wr", tg + "br", "ones_row"], writes=["ps%d" % (B0 + 2)])
                yield
                s.op("dve", lambda e, R=R: e.tensor_copy(lg[b][R, :], cx.ps[B0 + 2][R, 0:n_exp]),
                     reads=["ps%d" % (B0 + 2)], writes=[tg + "lg%d" % b])
                yield
                s.op("dve", lambda e, R=R: e.max(out=mx8[b][R, :], in_=lg[b][R, :]), reads=[tg + "lg%d" % b], writes=[tg + "mx8%d" % b])
                yield
                s.op("dve", lambda e, R=R: e.tensor_scalar(out=negm[b][R, :], in0=mx8[b][R, 0:1], scalar1=-1.0,
                                                           scalar2=None, op0=ALU.mult),
                     reads=[tg + "mx8%d" % b], writes=[tg + "negm%d" % b])
                yield
                s.op("dve", lambda e, R=R: e.tensor_scalar(out=msk[b][R, :], in0=lg[b][R, :], scalar1=mx8[b][R, 3:4],
                                                           scalar2=None, op0=ALU.is_ge),
                     reads=[tg + "lg%d" % b, tg + "mx8%d" % b], writes=[tg + "msk%d" % b])
                yield
                s.op("act", lambda e, R=R: e.activation(out=ex[b][R, :], in_=lg[b][R, :], func=AF.Exp, bias=negm[b][R, :],
                                                        scale=1.0),
                     reads=[tg + "lg%d" % b, tg + "negm%d" % b], writes=[tg + "ex%d" % b])
                yield
                s.op("dve", lambda e, R=R: e.tensor_tensor(out=ex[b][R, :], in0=ex[b][R, :], in1=msk[b][R, :], op=ALU.mult),
                     reads=[tg + "ex%d" % b, tg + "msk%d" % b], writes=[tg + "ex%d" % b])
                yield
                s.op("dve", lambda e, R=R: e.reduce_sum(out=ssum[b][R, :], in_=ex[b][R, :], axis=AX.X),
                     reads=[tg + "ex%d" % b], writes=[tg + "ssum%d" % b])
                yield
                s.op("dve", lambda e, R=R: e.reciprocal(out=ssum[b][R, :], in_=ssum[b][R, :]), reads=[tg + "ssum%d" % b],
                     writes=[tg + "ssum%d" % b])
                yield
                gk = tg + "gates%d" % ti
                s.op("dve", lambda e, R=R, ti=ti: e.tensor_scalar(out=gates[R, ti, :], in0=ex[b][R, :],
                                                                  scalar1=ssum[b][R, :], scalar2=None, op0=ALU.mult),
                     reads=[tg + "ex%d" % b, tg + "ssum%d" % b], writes=[gk])
                yield
                s.op("pe", lambda e, R=R, ti=ti, rows=rows: e.transpose(
                    out=cx.ps[B0 + 3][0:n_exp, 0:rows], in_=gates[R, ti, :], identity=cx.ident[R, R]),
                    reads=[gk, "ident"], writes=["ps%d" % (B0 + 3)])
                yield
                s.op("dve", lambda e, rows=rows: e.tensor_copy(gT[b][:, 0:rows], cx.ps[B0 + 3][0:n_exp, 0:rows]),
                     reads=["ps%d" % (B0 + 3)], writes=[tg + "gT%d" % b])
                yield
                for h in range(2):
                    bank = B0 + h
                    H = slice(h * 512, (h + 1) * 512)
                    s.op("pe", lambda e, H=H, bank=bank, rows=rows: e.matmul(
                        cx.ps[bank][0:rows, :], gT[b][:, 0:rows], b2[:, H], start=True, stop=True),
                        reads=[tg + "gT%d" % b, tg + "b2"], writes=["ps%d" % bank])
                    yield
                    tk = tg + "tmpf%d_%d" % (b, h)
                    s.op("dve", lambda e, h=h, H=H, bank=bank, R=R, cond=cond: e.tensor_tensor(
                        out=tmpf[b][h][R, :], in0=cx.ps[bank][R, :], in1=g2rep[R, cond, H], op=ALU.mult),
                        reads=["ps%d" % bank, tg + "g2rep"], writes=[tk])
                    yield
                    s.op("pool", lambda e, h=h, H=H, R=R, ti=ti: e.tensor_tensor(
                        out=XA[R, ti, H], in0=tmpf[b][h][R, :], in1=XA[R, ti, H], op=ALU.add),
                        reads=[tk, xak], writes=[xak])
                    yield

            for ti0 in range(0, len(tiles), 2):
                gens = [front_gen(ti, *tiles[ti]) for ti in range(ti0, min(ti0 + 2, len(tiles)))]
                alive = list(gens)
                while alive:
                    for g_ in list(alive):
                        try:
                            next(g_)
                        except StopIteration:
                            alive.remove(g_)

        with cx.scope():
            ACTT = A("ACTT", [128, KC, T], BF16)
            NW1 = 3
            w1b = [A("w1b", [128, KC, 256], BF16) for _ in range(NW1)]
            w2b = [A("w2b", [128, KC, D], BF16) for _ in range(2)]
            wstage = [A("wstage", [128, 2048], F32) for _ in range(4)]
            gbuf = [A("gbuf", [128, 512], F32) for _ in range(2)]
            sgbuf = [A("sgbuf", [128, 512], F32) for _ in range(2)]
            l0buf = [A("l0buf", [128, 512], F32) for _ in range(2)]
            tbuf = [A("tbuf", [128, 512], F32) for _ in range(2)]
            h2keys = [tg + "H2T%d" % ti for ti in range(nt)]
            wsc = [0]

            def fetch_w1(ci):
                ex_i, j = divmod(ci, KC)
                sb_ = wstage[wsc[0] % len(wstage)]
                sk_w = tg + "wst%d" % (wsc[0] % len(wstage))
                wsc[0] += 1
                s.dma("sp", sb_[:], dr["w1r"][ex_i, j], writes=[sk_w])
                s.op("act", lambda e, wb=w1b[ci % NW1], sb_=sb_: e.copy(
                    out=wb[:, :, :].rearrange("p kc n -> p (kc n)"), in_=sb_[:]),
                    reads=[sk_w], writes=[tg + "w1b%d" % (ci % NW1)])

            def fetch_w2(ex_i, qq):
                sb_ = wstage[wsc[0] % len(wstage)]
                sk_w = tg + "wst%d" % (wsc[0] % len(wstage))
                wsc[0] += 1
                s.dma("sp", sb_[:], dr["w2r"][ex_i][:, qq * 2048:(qq + 1) * 2048], writes=[sk_w])
                s.op("act", lambda e, wb=w2b[ex_i % 2], qq=qq, sb_=sb_: e.copy(
                    out=wb[:, 2 * qq:2 * qq + 2, :].rearrange("p j n -> p (j n)"), in_=sb_[:]),
                    reads=[sk_w], writes=[tg + "w2b%d" % (ex_i % 2)])

            nchunks = n_exp * KC
            fetch_w1(0)
            if nchunks > 1:
                fetch_w1(1)
            it = 0
            st2 = 0
            for ex_i in range(n_exp):
                wb2 = w2b[ex_i % 2]
                w2k = tg + "w2b%d" % (ex_i % 2)
                for j in range(KC):
                    ci = ex_i * KC + j
                    if ci + 2 < nchunks:
                        fetch_w1(ci + 2)
                    if j % 2 == 0:
                        fetch_w2(ex_i, j // 2)
                    wb1 = w1b[ci % NW1]
                    w1k = tg + "w1b%d" % (ci % NW1)
                    bg = b1c[:, ex_i * 16 + j:ex_i * 16 + j + 1]
                    bl1 = b1c1[:, ex_i * 16 + 8 + j:ex_i * 16 + 8 + j + 1]
                    for (g0, gw) in groups:
                        p = it % 2
                        it += 1
                        bg_bank, bl_bank = 2 * p, 2 * p + 1

                        def emit1(e, wb1=wb1, g0=g0, gw=gw, bg_bank=bg_bank, bl_bank=bl_bank):
                            ins = None
                            for kc in range(KC):
                                ins = e.matmul(cx.ps[bg_bank][:, 0:gw], wb1[:, kc, 0:128], H2T[:, kc, g0:g0 + gw],
                                               start=(kc == 0), stop=(kc == KC - 1))
                            for kc in range(KC):
                                ins = e.matmul(cx.ps[bl_bank][:, 0:gw], wb1[:, kc, 128:256], H2T[:, kc, g0:g0 + gw],
                                               start=(kc == 0), stop=(kc == KC - 1))
                            return ins
                        s.op("pe", emit1, reads=[w1k] + h2keys, writes=["ps%d" % bg_bank, "ps%d" % bl_bank])
                        gk_, sk_, l0k, tk_ = (tg + "gb%d" % p, tg + "sb%d" % p, tg + "l0%d" % p, tg + "tb%d" % p)
                        W = slice(0, gw)
                        s.op("dve", lambda e, p=p, W=W, bg=bg, bank=bg_bank: e.tensor_scalar(
                            out=gbuf[p][:, W], in0=cx.ps[bank][:, W], scalar1=bg, scalar2=7.0,
                            op0=ALU.add, op1=ALU.min), reads=["ps%d" % bg_bank, tg + "b1c"], writes=[gk_])
                        s.op("act", lambda e, p=p, W=W: e.activation(
                            out=sgbuf[p][:, W], in_=gbuf[p][:, W], func=AF.Sigmoid, scale=1.702),
                            reads=[gk_], writes=[sk_])
                        s.op("dve", lambda e, p=p, W=W, bl1=bl1, bank=bl_bank: e.tensor_scalar(
                            out=l0buf[p][:, W], in0=cx.ps[bank][:, W], scalar1=bl1, scalar2=8.0,
                            op0=ALU.add, op1=ALU.min), reads=["ps%d" % bl_bank, tg + "b1c1"], writes=[l0k])
                        s.op("pool", lambda e, p=p, W=W: e.tensor_tensor(
                            out=tbuf[p][:, W], in0=gbuf[p][:, W], in1=sgbuf[p][:, W], op=ALU.mult),
                            reads=[gk_, sk_], writes=[tk_])
                        s.op("dve", lambda e, p=p, W=W, j=j, g0=g0, gw=gw: e.scalar_tensor_tensor(
                            out=ACTT[:, j, g0:g0 + gw], in0=l0buf[p][:, W], scalar=-6.0, in1=tbuf[p][:, W],
                            op0=ALU.max, op1=ALU.mult),
                            reads=[tk_, l0k], writes=[tg + "ACTT%d_%d" % (j, g0)])
                for ti, (row0, rows, cond) in enumerate(tiles):
                    c0 = col0[ti]
                    gs = [g0 for (g0, gw) in groups if g0 < c0 + rows and c0 < g0 + gw]
                    akeys = [tg + "ACTT%d_%d" % (j, g0) for j in range(KC) for g0 in gs]
                    xak = tg + "XA%d" % ti
                    R = slice(0, rows)
                    for h in range(2):
                        bank = 4 + (st2 % 4)
                        pp = st2 % 2
                        st2 += 1
                        H = slice(h * 512, (h + 1) * 512)

                        def emit2(e, c0=c0, rows=rows, H=H, bank=bank, wb2=wb2):
                            ins = None
                            for j in range(KC):
                                ins = e.matmul(cx.ps[bank][0:rows, :], ACTT[:, j, c0:c0 + rows], wb2[:, j, H],
                                               start=(j == 0), stop=(j == KC - 1))
                            return ins
                        s.op("pe", emit2, reads=akeys + [w2k], writes=["ps%d" % bank])
                        tk = tg + "tmpa%d" % pp
                        s.op("dve", lambda e, H=H, bank=bank, R=R, cond=cond, pp=pp: e.tensor_tensor(
                            out=tmpa[pp][R, :], in0=cx.ps[bank][R, :], in1=g2rep[R, cond, H], op=ALU.mult),
                            reads=["ps%d" % bank, tg + "g2rep"], writes=[tk])
                        s.op("dve", lambda e, H=H, R=R, ti=ti, pp=pp, ex_i=ex_i: e.scalar_tensor_tensor(
                            out=XA[R, ti, H], in0=tmpa[pp][R, :], scalar=gates[R, ti, ex_i:ex_i + 1],
                            in1=XA[R, ti, H], op0=ALU.mult, op1=ALU.add),
                            reads=[tk, tg + "gates%d" % ti, xak], writes=[xak])

        for ti, (row0, rows, cond) in enumerate(tiles):
            xak = tg + "XA%d" % ti
            R = slice(0, rows)
            if final_norm:
                emit_rstd(cx, XA[R, ti, :], rows, junk, ss, rstd, xak, tg)
                s.op("dve", lambda e, R=R, ti=ti: e.scalar_tensor_tensor(
                    out=XA[R, ti, :], in0=XA[R, ti, :], scalar=rstd[R, :], in1=finrep[R, :],
                    op0=ALU.mult, op1=ALU.mult), reads=[xak, tg + "rstd", tg + "finrep"], writes=[xak])
            xdst = dr["xout_tile"](row0, rows) if "xout_tile" in dr else dr["xout"][row0:row0 + rows, :]
            s.dma("sp", xdst, XA[R, ti, :], reads=[xak], writes=[tg + "xout%d" % ti])
        if "h1" in dr:
            h1 = dr["h1"]
            with cx.scope():
                nA1, nm1 = emit_mod1(cx, h1, ncond, tg + "n")
                hxn = [A("hxn", [128, D], F32) for _ in range(2)]
                hTf = [A("hTf", [128, KC, 128], BF16) for _ in range(2)]
                for ti, (row0, rows, cond) in enumerate(tiles):
                    b = ti % 2
                    emit_norm_T(cx, XA[:, ti, :], rows, junk, ss, rstd, hxn[b], tg + "XA%d" % ti, tg + "hxn%d" % b, tg)
                    emit_evac_hT(cx, rows, hTf[b], nA1, nm1, cond, "+" + tg + "hTf%d" % b, tg + "n")
                    s.dma("sp", h1["tile_dst"](row0, rows).rearrange("p (kc t) -> p kc t", kc=KC),
                          hTf[b][:, :, 0:rows], reads=["+" + tg + "hTf%d" % b], writes=[tg + "h1o%d" % ti])


def lay_w1(w1):
    E = w1.shape[0]
    v = w1.reshape(E, 8, 128, 2, 8, 128)
    v = v.transpose(0, 4, 2, 1, 3, 5)
    return np.ascontiguousarray(v).reshape(E, 8, 128, 2048)


def lay_b1(b1):
    E = b1.shape[0]
    return np.ascontiguousarray(b1.reshape(E, 16, 128).transpose(2, 0, 1)).reshape(128, E * 16)


def lay_w2(w2):
    E = w2.shape[0]
    return np.ascontiguousarray(w2.reshape(E, 8, 128, 1024).transpose(0, 2, 1, 3)).reshape(E, 128, 8192)


def lay_cols(v):
    v = np.asarray(v, dtype=np.float32).reshape(-1, 8, 128)
    return np.ascontiguousarray(v.transpose(2, 1, 0))


def dft_consts():
    def cs(n, scale):
        k = np.arange(n)
        ang = 2 * np.pi * np.outer(k, k) / n
        return np.cos(ang) * scale, np.sin(ang) * scale
    cch, sch = cs(256, 1 / 16.0)
    cr, sr = cs(128, 1 / np.sqrt(128.0))
    cc, sc = cs(64, 1 / 8.0)
    c256, s256 = cs(256, 1 / 16.0)
    bdc = np.zeros((128, 128))
    bds = np.zeros((128, 128))
    for i in range(2):
        bdc[i * 64:(i + 1) * 64, i * 64:(i + 1) * 64] = cc
        bds[i * 64:(i + 1) * 64, i * 64:(i + 1) * 64] = -sc
    f = lambda a: np.ascontiguousarray(a, dtype=np.float32)
    return dict(cs_ch=f(np.concatenate([cch, sch], 1)),
                rs1=f(np.concatenate([cr, sr], 1)),
                rs2=f(np.concatenate([-sr, cr], 1)),
                bdc=f(bdc), bds=f(bds),
                c256=f(c256), s256n=f(-s256))


def emit_norm_T(cx, src, rows, junk, ss, rstd, xn, srckey, xnkey, tag, banks=(0, 1)):
    s = cx.s
    R = slice(0, rows)
    emit_rstd(cx, src[R, :], rows, junk, ss, rstd, srckey, tag)
    s.op("dve", lambda e: e.tensor_scalar(out=xn[R, :], in0=src[R, :], scalar1=rstd[R, :], scalar2=None,
                                          op0=ALU.mult), reads=[srckey, tag + "rstd"], writes=[xnkey])
    emit_transpose8(cx, xn, rows, banks[0], banks[1], xnkey)


def emit_evac_hT(cx, rows, hT, A1, B1, cond, hkey, tag, banks=(0, 1)):
    s = cx.s
    for kc in range(KC):
        bank, q = banks[kc // 4], kc % 4
        if kc // 4 == 0:
            s.op("act", lambda e, kc=kc, q=q, bank=bank: e.activation(
                out=hT[:, kc, 0:rows], in_=cx.ps[bank][:, q * 128:q * 128 + rows], func=AF.Identity,
                bias=B1[:, kc, cond:cond + 1], scale=A1[:, kc, cond:cond + 1]),
                reads=["ps%d" % bank, tag + "A1", "+" + tag + "cols"], writes=[hkey])
        else:
            s.op("dve", lambda e, kc=kc, q=q, bank=bank: e.tensor_scalar(
                out=hT[:, kc, 0:rows], in0=cx.ps[bank][:, q * 128:q * 128 + rows],
                scalar1=A1[:, kc, cond:cond + 1], scalar2=B1[:, kc, cond:cond + 1], op0=ALU.mult, op1=ALU.add),
                reads=["ps%d" % bank, tag + "A1", "+" + tag + "cols"], writes=[hkey])


def emit_norm_to_hT(cx, src, rows, hT, A1, B1, cond, junk, ss, rstd, xn, srckey, xnkey, hkey, tag):
    emit_norm_T(cx, src, rows, junk, ss, rstd, xn, srckey, xnkey, tag)
    emit_evac_hT(cx, rows, hT, A1, B1, cond, hkey, tag)


def emit_mod1(cx, dr, ncond, tag):
    s, A = cx.s, cx.A
    m1 = A("m1cols", [128, 16, ncond], F32)
    A1 = A("A1", [128, KC, ncond], F32)
    with cx.scope():
        modb_row = A("modb", [1, 6 * D], F32)
        s.dma("sp", modb_row[:], dr["modb"], writes=[tag + "modb"])
        silu_cols, _ = emit_silu_cond(cx, dr["cond_cols"], ncond, tag, False)
        emit_mod_cols(cx, dr["modw"], modb_row, silu_cols, ncond, list(range(0, 16)), m1, tag)
        n1g = A("n1g", [128, KC], F32)
        s.dma("sp", n1g[:], dr["n1g_col"], writes=[tag + "n1g"])
        for c in range(ncond):
            s.op("dve", lambda e, c=c: e.scalar_tensor_tensor(
                out=A1[:, :, c], in0=m1[:, 8:16, c], scalar=1.0, in1=n1g[:, :], op0=ALU.add, op1=ALU.mult),
                reads=["+" + tag + "cols", tag + "n1g"], writes=[tag + "A1"])
    return A1, m1


def emit_fourier(cx, dr, tag="F"):
    s, A = cx.s, cx.A
    tg = tag
    with cx.scope():
        A1, m1 = emit_mod1(cx, dr, 2, tg)
        WW = A("WW", [128, KC, 512], BF16)
        rs1 = A("rs1", [128, 256], BF16)
        rs2 = A("rs2", [128, 256], BF16)
        bdc = A("bdc", [128, 128], BF16)
        bds = A("bds", [128, 128], BF16)
        c256 = A("c256", [128, 2, 256], BF16)
        s256 = A("s256", [128, 2, 256], BF16)
        s.dma("pool", rs1[:], dr["rs1"], writes=[tg + "rs1"])
        s.dma("pool", rs2[:], dr["rs2"], writes=[tg + "rs2"])
        s.dma("pool", bdc[:], dr["bdc"], writes=[tg + "bdc"])
        s.dma("pool", bds[:], dr["bds"], writes=[tg + "bds"])
        s.dma("pool", c256[:], dr["c256"].rearrange("(k p) n -> p k n", p=128), writes=[tg + "c256"])
        s.dma("pool", s256[:], dr["s256n"].rearrange("(k p) n -> p k n", p=128), writes=[tg + "s256"])
        with cx.scope():
            wT = A("wT", [128, 2, D], F32)
            csch = A("csch", [128, 2, 512], F32)
            s.dma("sp", wT[:], dr["winT"].rearrange("(k p) n -> p k n", p=128), writes=[tg + "wT"])
            s.dma("sp", csch[:], dr["cs_ch"].rearrange("(k p) n -> p k n", p=128), writes=[tg + "csch"])
            for ic in range(KC):
                bank = 4 + ic % 4

                def emit(e, ic=ic, bank=bank):
                    e.matmul(cx.ps[bank][:, :], wT[:, 0, ic * 128:(ic + 1) * 128], csch[:, 0, :], start=True, stop=False)
                    return e.matmul(cx.ps[bank][:, :], wT[:, 1, ic * 128:(ic + 1) * 128], csch[:, 1, :],
                                    start=False, stop=True)
                s.op("pe", emit, reads=[tg + "wT", tg + "csch"], writes=["ps%d" % bank])
                s.op("act", lambda e, ic=ic, bank=bank: e.copy(out=WW[:, ic, :], in_=cx.ps[bank][:, :]),
                     reads=["ps%d" % bank], writes=["+" + tg + "WW"])

        xt = [A("xt", [128, D], F32) for _ in range(2)]
        xn = [A("xn", [128, D], F32) for _ in range(2)]
        hT = [A("hT", [128, KC, 128], BF16) for _ in range(2)]
        junk = A("junk", [128, D], F32)
        ss = A("ss", [128, 1], F32)
        rstd = A("rstd", [128, 1], F32)
        odt = dr.get("out_dt", F32)
        yo = [A("yo", [128, 512], odt) for _ in range(2)]
        lat_chunks = dr["ylat_chunks"]
        gr_per_chunk = 128 // len(lat_chunks)

        with cx.scope():
            PQc = A("PQc", [128, 2, 512], BF16)
            for m in range(2):
                b = m % 2
                s.dma("sp", xt[b][:], dr["ctx"][m * 128:(m + 1) * 128, :], writes=[tg + "xt%d" % b])
                emit_norm_to_hT(cx, xt[b], 128, hT[b], A1, m1, 0, junk, ss, rstd, xn[b],
                                tg + "xt%d" % b, tg + "xn%d" % b, "+" + tg + "hT%d" % b, tg)

                def emit(e, b=b):
                    ins = None
                    for kc in range(KC):
                        ins = e.matmul(cx.ps[2][:, :], hT[b][:, kc, :], WW[:, kc, :], start=(kc == 0),
                                       stop=(kc == KC - 1))
                    return ins
                s.op("pe", emit, reads=["+" + tg + "hT%d" % b, "+" + tg + "WW"], writes=["ps2"])
                s.op("act", lambda e, m=m: e.copy(out=PQc[:, m, :], in_=cx.ps[2][:, :]), reads=["ps2"],
                     writes=["+" + tg + "PQc"])
            for m in range(2):
                def emit(e, m=m):
                    M = slice(m * 128, (m + 1) * 128)
                    e.matmul(cx.ps[3][:, 0:256], c256[:, 0, M], PQc[:, 0, 0:256], start=True, stop=False)
                    e.matmul(cx.ps[3][:, 0:256], c256[:, 1, M], PQc[:, 1, 0:256], start=False, stop=False)
                    e.matmul(cx.ps[3][:, 0:256], s256[:, 0, M], PQc[:, 0, 256:512], start=False, stop=False)
                    return e.matmul(cx.ps[3][:, 0:256], s256[:, 1, M], PQc[:, 1, 256:512], start=False, stop=True)
                s.op("pe", emit, reads=["+" + tg + "PQc", tg + "c256", tg + "s256"], writes=["ps3"])
                s.op("dve", lambda e, m=m: e.tensor_copy(yo[m][:, 0:256], cx.ps[3][:, 0:256]), reads=["ps3"],
                     writes=[tg + "yo%d" % m])
                s.dma("sp", dr["yctx"][m * 128:(m + 1) * 128, :], yo[m][:, 0:256], reads=[tg + "yo%d" % m],
                      writes=["+yctxo"])
            if "after_ctx" in dr:
                dr["after_ctx"]()

        PQ = A("PQ", [128, 2, 128, 2, 64], BF16)
        AB = A("AB", [128, 2, 128, 128], BF16)
        xv = dr["x"].rearrange("(r c) d -> c r d", c=64)
        xt3 = xt + [A("xt", [128, D], F32)]

        def stL(c):
            s.dma("sp", xt3[c % 3][:], xv[c], writes=[tg + "xl%d" % (c % 3)])

        def stN(c):
            banks = (0, 1) if c % 2 == 0 else (4, 5)
            src_, skey, xnk = xt3[c % 3], tg + "xl%d" % (c % 3), tg + "xn%d" % (c % 2)
            emit_rstd(cx, src_[:, :], 128, junk, ss, rstd, skey, tg)
            yield
            s.op("dve", lambda e: e.tensor_scalar(out=xn[c % 2][:, :], in0=src_[:, :], scalar1=rstd[:, :], scalar2=None,
                                                  op0=ALU.mult), reads=[skey, tg + "rstd"], writes=[xnk])
            yield
            emit_transpose8(cx, xn[c % 2], 128, banks[0], banks[1], xnk)
            yield

        def stE(c):
            b = c % 2
            banks = (0, 1) if c % 2 == 0 else (4, 5)
            hkey = "+" + tg + "hT%d" % b
            for kc in range(KC):
                bank, q = banks[kc // 4], kc % 4
                if kc // 4 == 0:
                    s.op("act", lambda e, kc=kc, q=q, bank=bank: e.activation(
                        out=hT[b][:, kc, :], in_=cx.ps[bank][:, q * 128:(q + 1) * 128], func=AF.Identity,
                        bias=m1[:, kc, 1:2], scale=A1[:, kc, 1:2]),
                        reads=["ps%d" % bank, tg + "A1", "+" + tg + "cols"], writes=[hkey])
                else:
                    s.op("dve", lambda e, kc=kc, q=q, bank=bank: e.tensor_scalar(
                        out=hT[b][:, kc, :], in0=cx.ps[bank][:, q * 128:(q + 1) * 128],
                        scalar1=A1[:, kc, 1:2], scalar2=m1[:, kc, 1:2], op0=ALU.mult, op1=ALU.add),
                        reads=["ps%d" % bank, tg + "A1", "+" + tg + "cols"], writes=[hkey])
                if kc % 2 == 1:
                    yield
            bank = 2 + c % 2

            def emit(e, b=b, bank=bank):
                ins = None
                for kc in range(KC):
                    ins = e.matmul(cx.ps[bank][:, :], hT[b][:, kc, :], WW[:, kc, :], start=(kc == 0),
                                   stop=(kc == KC - 1))
                return ins
            s.op("pe", emit, reads=[hkey, "+" + tg + "WW"], writes=["ps%d" % bank])
            yield
            for pq in range(2):
                src2 = cx.ps[bank][:, pq * 256:(pq + 1) * 256].rearrange("p (l h) -> p l h", l=2)
                dst = PQ[:, pq, :, :, c].rearrange("p h l -> p l h")
                if c % 2 == 0:
                    s.op("act", lambda e, src2=src2, dst=dst: e.copy(out=dst, in_=src2), reads=["ps%d" % bank],
                         writes=["+" + tg + "PQ"])
                else:
                    s.op("dve", lambda e, src2=src2, dst=dst: e.tensor_copy(dst, src2), reads=["ps%d" % bank],
                         writes=["+" + tg + "PQ"])
                yield

        def run2(gens):
            alive = [g for g in gens if g is not None]
            while alive:
                for g in list(alive):
                    try:
                        next(g)
                    except StopIteration:
                        alive.remove(g)

        stL(0)
        stL(1)
        run2([stN(0)])
        for c in range(64):
            if c + 2 < 64:
                stL(c + 2)
            run2([stN(c + 1) if c + 1 < 64 else None, stE(c)])
        for hh in range(128):
            bank = 4 + hh % 4

            def emit(e, hh=hh, bank=bank):
                e.matmul(cx.ps[bank][:, 0:256], PQ[:, 0, hh, :, :].rearrange("p l c -> p (l c)"), rs1[:, :],
                         start=True, stop=False)
                return e.matmul(cx.ps[bank][:, 0:256], PQ[:, 1, hh, :, :].rearrange("p l c -> p (l c)"), rs2[:, :],
                                start=False, stop=True)
            s.op("pe", emit, reads=["+" + tg + "PQ", tg + "rs1", tg + "rs2"], writes=["ps%d" % bank])
            src = cx.ps[bank][:, 0:256].rearrange("p (a r) -> p a r", a=2)
            dst = AB[:, :, :, hh]
            if hh % 2 == 0:
                s.op("act", lambda e, src=src, dst=dst: e.copy(out=dst, in_=src), reads=["ps%d" % bank],
                     writes=["+" + tg + "AB"])
            else:
                s.op("dve", lambda e, src=src, dst=dst: e.tensor_copy(dst, src), reads=["ps%d" % bank],
                     writes=["+" + tg + "AB"])
        yvs = [ch.rearrange("(r c) n -> c r n", c=64) for ch in lat_chunks]
        for r4 in range(32):
            yv = yvs[(r4 * 4) // gr_per_chunk]
            rr0 = (r4 * 4) % gr_per_chunk
            bank = r4 % 2
            b = r4 % 2

            def emit(e, r4=r4, bank=bank):
                e.matmul(cx.ps[bank][:, :], bdc[:, :], AB[:, 0, r4 * 4:(r4 + 1) * 4, :].rearrange("p r h -> p (r h)"),
                         start=True, stop=False)
                return e.matmul(cx.ps[bank][:, :], bds[:, :],
                                AB[:, 1, r4 * 4:(r4 + 1) * 4, :].rearrange("p r h -> p (r h)"), start=False, stop=True)
            s.op("pe", emit, reads=["+" + tg + "AB", tg + "bdc", tg + "bds"], writes=["ps%d" % bank])
            if r4 % 2 == 0:
                s.op("act", lambda e, b=b, bank=bank: e.copy(out=yo[b][:, :], in_=cx.ps[bank][:, :]),
                     reads=["ps%d" % bank], writes=[tg + "yo%d" % b])
            else:
                s.op("dve", lambda e, b=b, bank=bank: e.tensor_copy(yo[b][:, :], cx.ps[bank][:, :]),
                     reads=["ps%d" % bank], writes=[tg + "yo%d" % b])
            for lo in range(2):
                s.dma("sp", yv[:, rr0:rr0 + 4, lo * 128:(lo + 1) * 128],
                      yo[b][lo * 64:(lo + 1) * 64, :].rearrange("p (r h) -> p r h", r=4),
                      reads=[tg + "yo%d" % b], writes=["+ylatc%d" % ((r4 * 4) // gr_per_chunk)])
            if "after_out" in dr and (r4 * 4 + 4) % gr_per_chunk == 0:
                dr["after_out"]((r4 * 4) // gr_per_chunk)


def hgrn_consts():
    t = np.arange(128)
    same = (t[:, None] // 64) == (t[None, :] // 64)
    tri_fw = (same & (t[:, None] <= t[None, :])).astype(np.float32)
    tri_bw = (same & (t[:, None] >= t[None, :])).astype(np.float32)
    su_fw = (same & (t[:, None] > t[None, :])).astype(np.float32)
    su_bw = (same & (t[:, None] < t[None, :])).astype(np.float32)
    return dict(tri_fw=tri_fw, tri_bw=tri_bw, su_fw=su_fw, su_bw=su_bw)


def emit_hgrn(cx, dr, tag="H", n_lat_tiles=64, n_ctx_tiles=2):
    s, A, nc = cx.s, cx.A, cx.nc
    tg = tag
    NL = n_lat_tiles
    pass
    with cx.scope():
        if "h_tile" not in dr:
            A1, m1 = emit_mod1(cx, dr, 2, tg)
        W = A("W5", [128, KC, 1280], BF16)
        s.dma("pool", W[:], dr["win5"].rearrange("(kc p) n -> p kc n", p=128), writes=[tg + "W"])
        tri = [A("tri", [128, 128], F32) for _ in range(2)]
        su = [A("su", [128, 128], F32) for _ in range(2)]
        for d, nm in enumerate(("fw", "bw")):
            s.dma("sp", tri[d][:], dr["tri_" + nm], writes=[tg + "tri%d" % d])
            s.dma("sp", su[d][:], dr["su_" + nm], writes=[tg + "su%d" % d])
        hng = A("hng", [128, 256], F32)
        s.dma("sp", hng[:], dr["hng_rep"], writes=[tg + "hng"])
        lb = A("lb", [128, 2, 256], F32)
        oml = A("oml", [128, 2, 256], F32)
        with cx.scope():
            lbr = A("lbr", [128, 2, 2, 256], F32)
            s.dma("sp", lbr[:], dr["lbrep"], writes=[tg + "lbr"])
            s.op("dve", lambda e: e.tensor_tensor(out=lb[:], in0=lbr[:, 1], in1=lbr[:, 0], op=ALU.subtract),
                 reads=[tg + "lbr"], writes=[tg + "lb"])
            s.op("act", lambda e: e.activation(out=lb[:], in_=lb[:], func=AF.Exp, scale=-1.0),
                 reads=[tg + "lb"], writes=[tg + "lb"])
            s.op("dve", lambda e: e.tensor_scalar(out=lb[:], in0=lb[:], scalar1=1.0, scalar2=None, op0=ALU.add),
                 reads=[tg + "lb"], writes=[tg + "lb"])
            s.op("dve", lambda e: e.reciprocal(out=lb[:], in_=lb[:]), reads=[tg + "lb"], writes=[tg + "lb"])
            s.op("dve", lambda e: e.tensor_scalar(out=oml[:], in0=lb[:], scalar1=-1.0, scalar2=1.0, op0=ALU.mult,
                                                  op1=ALU.add), reads=[tg + "lb"], writes=[tg + "oml"])

        OFW = A("OFW", [128, max(NL, 1), 256], F32)
        xt = [A("xt", [128, D], F32) for _ in range(2)]
        xn = [A("xn", [128, D], F32) for _ in range(2)]
        hT = [A("hT", [128, KC, 128], BF16) for _ in range(2)]
        junk = A("junk", [128, D], F32)
        ss = A("ss", [128, 1], F32)
        rstd = A("rstd", [128, 1], F32)
        NB = 2
        EA = [A("EA", [128, 768], F32) for _ in range(NB)]
        fg = [A("fg", [128, 256], F32) for _ in range(NB)]
        logf = [A("logf", [128, 256], F32) for _ in range(NB)]
        kf = [A("kf", [128, 256], F32) for _ in range(NB)]
        kh = [A("kh", [128, 256], BF16) for _ in range(NB)]
        kh2 = [A("kh2", [128, 256], BF16) for _ in range(NB)]
        rmask = A("rmask", [128, 2], F32)
        s.op("dve", lambda e: e.memset(rmask[:], 0.0), writes=[tg + "rmask"])
        s.op("dve", lambda e: e.memset(rmask[0:64, 0:1], 1.0), writes=[tg + "rmask"])
        s.op("dve", lambda e: e.memset(rmask[64:128, 1:2], 1.0), writes=[tg + "rmask"])
        vv = [A("vv", [128, 256], BF16) for _ in range(NB)]
        qs = [A("qs", [128, 256], F32) for _ in range(NB)]
        gg = [A("gg", [128, 256], F32) for _ in range(NB)]
        ec = [A("ec", [128, 256], F32) for _ in range(NB)]
        ebT = [[A("ebT", [128, 128], F32) for _ in range(NB)] for _ in range(2)]
        enbT = [[A("enbT", [128, 128], F32) for _ in range(NB)] for _ in range(2)]
        Z = [[A("Z", [128, 256], BF16) for _ in range(NB)] for _ in range(2)]
        ktT = [[A("ktT", [128, 128], BF16) for _ in range(NB)] for _ in range(2)]
        scm = [[A("scm", [128, 128], BF16) for _ in range(NB)] for _ in range(2)]
        S = [[A("S", [128, 128], F32) for _ in range(3)] for _ in range(2)]
        Sb = [[A("Sb", [128, 128], BF16) for _ in range(4)] for _ in range(2)]
        obuf = [A("obuf", [128, 256], F32) for _ in range(NB)]
        yout = [A("yout", [128, 256], dr.get("out_dt", F32)) for _ in range(NB)]
        ssq = A("ssq", [128, 2], F32)
        rsq = A("rsq", [128, 2], F32)
        junkB = A("junkB", [128, 256], F32)
        for hd in range(2):
            for b in range(NB):
                s.op("pool", lambda e, hd=hd, b=b: e.memset(Z[hd][b][:], 0.0), writes=[tg + "Z%d_%d" % (hd, b)])

        steps = []
        for d in range(2):
            if d == 0:
                order = [("ctx", i) for i in range(n_ctx_tiles)] + [("lat", i) for i in range(NL)]
            else:
                order = [("ctx", i) for i in reversed(range(n_ctx_tiles))] + [("lat", i) for i in reversed(range(NL))]
            for idx, (kind, i) in enumerate(order):
                steps.append((d, kind, i, idx == 0))
        scur = [0, 0]
        sbc = [0, 0]

        xt4 = xt + [A("xt", [128, D], F32) for _ in range(2)]
        hT4 = [A("hT4", [128, KC, 128], BF16) for _ in range(4)]
        hscr_t = nc.dram_tensor(cx.name("hscr"), [n_ctx_tiles + max(NL, 1), 128, KC * 128], BF16)
        hscr = [hscr_t.ap()[j_] for j_ in range(n_ctx_tiles + max(NL, 1))]

        def stageL(n):
            d, kind, i, first_of_sweep = steps[n]
            if "h_tile" in dr:
                for (dst_cols, hsrc_ap) in dr["h_tile"](kind, i):
                    s.dma("sp", hT4[n % 4][:, :, dst_cols], hsrc_ap.rearrange("p (kc t) -> p kc t", kc=KC),
                          writes=["+" + tg + "hl%d" % (n % 4)])
                return
            if d == 1:
                slot = i if kind == "ctx" else n_ctx_tiles + i
                s.dma("sp", hT4[n % 4][:], hscr[slot].rearrange("p (kc t) -> p kc t", kc=KC),
                      reads=["hscr%d" % slot], writes=["+" + tg + "hl%d" % (n % 4)])
                return
            if "x_tile" in dr:
                src = dr["x_tile"](kind, i)
            else:
                src = dr["x"][i * 128:(i + 1) * 128, :] if kind == "lat" else dr["xctx"][i * 128:(i + 1) * 128, :]
            s.dma("sp", xt4[n % 4][:], src, writes=[tg + "xl%d" % (n % 4)])

        def stageA(n):
            d, kind, i, first_of_sweep = steps[n]
            lat = kind == "lat"
            b = n % 2
            k = lambda nm: tg + nm + "%d" % b
            if d == 0 and "h_tile" not in dr:
                emit_norm_T(cx, xt4[n % 4], 128, junk, ss, rstd, xn[b], tg + "xl%d" % (n % 4), k("xn"), tg)
                yield
                emit_evac_hT(cx, 128, hT[b], A1, m1, 1 if lat else 0, "+" + k("hT"), tg)
                slot = i if kind == "ctx" else n_ctx_tiles + i
                s.dma("sp", hscr[slot].rearrange("p (kc t) -> p kc t", kc=KC), hT[b][:], reads=["+" + k("hT")],
                      writes=["hscr%d" % slot])
                hsrc, hkey_ = hT[b], "+" + k("hT")
            else:
                hsrc, hkey_ = hT4[n % 4], "+" + tg + "hl%d" % (n % 4)
            c0 = 256 if d == 0 else 512

            def emit_p(e, hsrc=hsrc, c0=c0):
                ins = None
                for kc in range(KC):
                    ins = e.matmul(cx.ps[2][:, :], hsrc[:, kc, :], W[:, kc, c0:c0 + 512], start=(kc == 0),
                                   stop=(kc == KC - 1))
                return ins
            s.op("pe", emit_p, reads=[hkey_, tg + "W"], writes=["ps2"])
            yield
            if lat:
                def emit_q(e, hsrc=hsrc, d=d):
                    ins = None
                    if d == 1:
                        for kc in range(KC):
                            ins = e.matmul(cx.ps[3][:, 0:256], hsrc[:, kc, :], W[:, kc, 1024:1280],
                                           start=(kc == 0), stop=(kc == KC - 1))
                    for hd in range(2):
                        for kc in range(KC):
                            ins = e.matmul(cx.ps[3][:, 256 + hd * 128:384 + hd * 128],
                                           W[:, kc, hd * 128:(hd + 1) * 128], hsrc[:, kc, :],
                                           start=(kc == 0), stop=(kc == KC - 1))
                    return ins
                s.op("pe", emit_q, reads=[hkey_, tg + "W"], writes=["ps3"])
                yield
            fsl = cx.ps[2][:, 0:256] if d == 0 else cx.ps[2][:, 256:512]
            isl = cx.ps[2][:, 256:512] if d == 0 else cx.ps[2][:, 0:256]
            yield
            EAb = EA[b]
            s.op("act", lambda e, EAb=EAb, fsl=fsl: e.activation(out=EAb[:, 0:256], in_=fsl, func=AF.Exp, scale=-1.0),
                 reads=["ps2"], writes=[k("EA")])
            yield
            s.op("act", lambda e, b=b, isl=isl: e.copy(out=vv[b][:], in_=isl), reads=["ps2"], writes=[k("vv")])
            yield
            if lat and d == 1:
                s.op("act", lambda e, EAb=EAb: e.activation(out=EAb[:, 256:768], in_=cx.ps[3][:, 0:512], func=AF.Exp,
                                                            scale=-1.0), reads=["ps3", k("EA")], writes=[k("EA")])
                yield
                reg = EAb[:, 0:768]
            elif lat:
                s.op("act", lambda e, EAb=EAb: e.activation(out=EAb[:, 512:768], in_=cx.ps[3][:, 256:512], func=AF.Exp,
                                                            scale=-1.0), reads=["ps3", k("EA")], writes=[k("EA")])
                yield
                reg = EAb[:, :].rearrange("p (a x) -> p a x", a=3)[:, 0:3:2, :]
            else:
                reg = EAb[:, 0:256]
            s.op("act", lambda e, reg=reg: e.activation(out=reg, in_=reg, func=AF.Ln, bias=cx.one_col[:, :], scale=1.0),
                 reads=[k("EA"), "one_col"], writes=[k("EA")])
            yield
            s.op("act", lambda e, reg=reg: e.activation(out=reg, in_=reg, func=AF.Exp, scale=-1.0),
                 reads=[k("EA")], writes=[k("EA")])
            yield
            s.op("dve", lambda e, b=b, d=d, EAb=EAb: e.tensor_tensor(out=fg[b][:], in0=EAb[:, 0:256], in1=oml[:, d, :],
                                                                     op=ALU.mult),
                 reads=[k("EA"), tg + "oml"], writes=[k("fg")])
            yield
            s.op("dve", lambda e, b=b, d=d: e.tensor_tensor(out=fg[b][:], in0=fg[b][:], in1=lb[:, d, :],
                                                            op=ALU.add),
                 reads=[k("fg"), tg + "lb"], writes=[k("fg")])
            yield
            s.op("act", lambda e, b=b: e.activation(out=logf[b][:], in_=fg[b][:], func=AF.Ln),
                 reads=[k("fg")], writes=[k("logf")])
            yield
            s.op("pool", lambda e, b=b: e.tensor_scalar(out=kf[b][:], in0=fg[b][:], scalar1=-1.0, scalar2=1.0,
                                                        op0=ALU.mult, op1=ALU.add),
                 reads=[k("fg")], writes=[k("kf")])
            yield
            yield
            if lat:
                qsl = cx.ps[3][:, 256:512]
                s.op("dve", lambda e, b=b, qsl=qsl, EAb=EAb: e.tensor_tensor(out=qs[b][:], in0=EAb[:, 512:768], in1=qsl,
                                                                             op=ALU.mult),
                     reads=[k("EA"), "ps3"], writes=[k("qs")])
                yield
                if d == 1:
                    gsl = cx.ps[3][:, 0:256]
                    s.op("dve", lambda e, b=b, gsl=gsl, EAb=EAb: e.tensor_tensor(out=gg[b][:], in0=EAb[:, 256:512],
                                                                                 in1=gsl, op=ALU.mult),
                         reads=[k("EA"), "ps3"], writes=[k("gg")])
                    yield
                    s.op("pool", lambda e, b=b: e.tensor_tensor(out=gg[b][:], in0=gg[b][:], in1=hng[:],
                                                                op=ALU.mult),
                         reads=[k("gg"), tg + "hng"], writes=[k("gg")])
                    yield

        def stageB(n):
            d, kind, i, first_of_sweep = steps[n]
            lat = kind == "lat"
            b = n % 2
            k = lambda nm: tg + nm + "%d" % b
            if first_of_sweep:
                scur[0] = scur[1] = 0
                sbc[0] = sbc[1] = 0
                for hd in range(2):
                    s.op("dve", lambda e, hd=hd: e.memset(S[hd][0][:], 0.0), writes=[tg + "S%d_0" % hd])
                    yield
            s.op("pe", lambda e, b=b, d=d: e.matmul(cx.ps[6][:, 0:256], su[d][:, :], logf[b][:, :],
                                                    start=True, stop=True),
                 reads=[k("logf"), tg + "su%d" % d], writes=["ps6"])
            yield
            s.op("act", lambda e, b=b: e.activation(out=ec[b][:], in_=cx.ps[6][:, 0:256], func=AF.Exp),
                 reads=["ps6"], writes=[k("ec")])
            yield
            s.op("dve", lambda e, b=b: e.scalar_tensor_tensor(
                out=kh[b][:], in0=kf[b][:], scalar=rmask[:, 0:1], in1=ec[b][:], op0=ALU.mult, op1=ALU.mult),
                reads=[k("kf"), k("ec"), tg + "rmask"], writes=["+" + k("kh")])
            yield
            s.op("dve", lambda e, b=b: e.scalar_tensor_tensor(
                out=kh2[b][:], in0=kf[b][:], scalar=rmask[:, 1:2], in1=ec[b][:], op0=ALU.mult, op1=ALU.mult),
                reads=[k("kf"), k("ec"), tg + "rmask"], writes=["+" + k("kh")])
            yield
            first, second = (0, 1) if d == 0 else (1, 0)
            yield

            def head_gen(hd):
                H = slice(hd * 128, (hd + 1) * 128)
                kb = lambda nm: tg + nm + "%d_%d" % (hd, b)
                hb = 4 + hd
                hbk = "ps%d" % hb
                BT, KT, SC, OO = slice(0, 128), slice(128, 256), slice(256, 384), slice(384, 512)

                def emit_bt(e, b=b, d=d, H=H, hb=hb, lat=lat):
                    ins = e.matmul(cx.ps[hb][:, BT], logf[b][:, H], tri[d][:, :], start=True, stop=True)
                    if lat:
                        ins = e.transpose(out=cx.ps[hb][:, KT], in_=kf[b][:, H], identity=cx.ident[:, :])
                    return ins
                s.op("pe", emit_bt, reads=[k("logf"), tg + "tri%d" % d, k("kf"), "ident"], writes=[hbk])
                yield
                s.op("act", lambda e, b=b, hd=hd, hb=hb: e.activation(out=ebT[hd][b][:], in_=cx.ps[hb][:, BT],
                                                                      func=AF.Exp),
                     reads=[hbk], writes=[kb("ebT")])
                yield
                if lat:
                    s.op("dve", lambda e, b=b, hd=hd, hb=hb: e.tensor_scalar(
                        out=enbT[hd][b][:], in0=cx.ps[hb][:, BT], scalar1=-87.0, scalar2=None, op0=ALU.max),
                        reads=[hbk], writes=[kb("enbT")])
                    yield
                    s.op("act", lambda e, b=b, hd=hd: e.activation(
                        out=enbT[hd][b][:], in_=enbT[hd][b][:], func=AF.Exp, scale=-1.0),
                        reads=[kb("enbT")], writes=[kb("enbT")])
                    yield
                    Zv = Z[hd][b][:, :].rearrange("p (a x) -> p a x", a=4)[:, 0:4:3, :]
                    s.op("dve", lambda e, b=b, hd=hd, H=H, Zv=Zv: e.tensor_tensor(
                        out=Zv, in0=qs[b][:, H].rearrange("p (a x) -> p a x", a=2),
                        in1=ebT[hd][b][:, :].rearrange("p (a x) -> p a x", a=2), op=ALU.mult),
                        reads=[k("qs"), kb("ebT")], writes=[kb("Z")])
                    yield
                    s.op("dve", lambda e, b=b, hd=hd, hb=hb: e.tensor_tensor(
                        out=ktT[hd][b][:], in0=cx.ps[hb][:, KT], in1=enbT[hd][b][:], op=ALU.mult),
                        reads=[hbk, kb("enbT")], writes=[kb("ktT")])
                    yield
                    s.op("pe", lambda e, b=b, hd=hd, hb=hb, Zv=Zv: e.matmul(
                        cx.ps[hb][:, SC].rearrange("p (a x) -> p a x", a=2), ktT[hd][b][:, :], Zv,
                        start=True, stop=True),
                        reads=[kb("ktT"), kb("Z")], writes=[hbk])
                    yield
                    s.op("dve", lambda e, b=b, hd=hd, hb=hb, d=d: e.tensor_tensor(
                        out=scm[hd][b][:], in0=cx.ps[hb][:, SC], in1=tri[d][:, :], op=ALU.mult),
                        reads=[hbk, tg + "tri%d" % d], writes=[kb("scm")])
                    yield
                yield
                kvb = 6 if hd == 0 else 7
                kvk = "ps%d" % kvb
                o0 = 256 if hd == 0 else 0
                kv1 = slice(o0, o0 + 128)
                kv2 = slice(o0 + 128, o0 + 256)
                P1 = slice(first * 64, first * 64 + 64)
                P2 = slice(second * 64, second * 64 + 64)

                khf = kh if first == 0 else kh2
                khs = kh2 if first == 0 else kh

                def emit_kv(e, b=b, H=H, kv1=kv1, kv2=kv2, khf=khf, khs=khs, kvb=kvb):
                    e.matmul(cx.ps[kvb][:, kv1], khf[b][:, H], vv[b][:, H], start=True, stop=True)
                    return e.matmul(cx.ps[kvb][:, kv2], khs[b][:, H], vv[b][:, H], start=True, stop=True)
                s.op("pe", emit_kv, reads=["+" + k("kh"), k("vv")], writes=[kvk])
                yield
                if d == 0:
                    c1, c2 = 63, 127
                else:
                    c1, c2 = 64, 0
                si, sm, so = scur[hd] % 3, (scur[hd] + 1) % 3, (scur[hd] + 2) % 3
                scur[hd] += 2
                kS = lambda j: tg + "S%d_%d" % (hd, j)
                if lat:
                    bi, bm = sbc[hd] % 4, (sbc[hd] + 1) % 4
                    sbc[hd] += 2
                    kSb = lambda j: tg + "Sb%d_%d" % (hd, j)
                    s.op("pool", lambda e, hd=hd, si=si, bi=bi: e.tensor_copy(Sb[hd][bi][:], S[hd][si][:]),
                         reads=[kS(si)], writes=[kSb(bi)])
                    yield
                s.op("dve", lambda e, hd=hd, b=b, si=si, sm=sm, c1=c1, kv1=kv1, kvb=kvb: e.scalar_tensor_tensor(
                    out=S[hd][sm][:], in0=S[hd][si][:], scalar=ebT[hd][b][:, c1:c1 + 1], in1=cx.ps[kvb][:, kv1],
                    op0=ALU.mult, op1=ALU.add),
                    reads=[kS(si), kb("ebT"), kvk], writes=[kS(sm)])
                yield
                if lat:
                    s.op("pool", lambda e, hd=hd, sm=sm, bm=bm: e.tensor_copy(Sb[hd][bm][:], S[hd][sm][:]),
                         reads=[kS(sm)], writes=[kSb(bm)])
                    yield
                s.op("dve", lambda e, hd=hd, b=b, sm=sm, so=so, c2=c2, kv2=kv2, kvb=kvb: e.scalar_tensor_tensor(
                    out=S[hd][so][:], in0=S[hd][sm][:], scalar=ebT[hd][b][:, c2:c2 + 1], in1=cx.ps[kvb][:, kv2],
                    op0=ALU.mult, op1=ALU.add),
                    reads=[kS(sm), kb("ebT"), kvk], writes=[kS(so)])
                yield
                if lat:
                    Zf = Z[hd][b][:, first * 128:(first + 1) * 128]
                    Zs = Z[hd][b][:, second * 128:(second + 1) * 128]

                    def emit_o(e, b=b, hd=hd, H=H, Zf=Zf, Zs=Zs, bi=bi, bm=bm, hb=hb):
                        e.matmul(cx.ps[hb][:, OO], scm[hd][b][:, :], vv[b][:, H], start=True, stop=False)
                        e.matmul(cx.ps[hb][:, OO], Zf, Sb[hd][bi][:, :], start=False, stop=False)
                        return e.matmul(cx.ps[hb][:, OO], Zs, Sb[hd][bm][:, :], start=False, stop=True)
                    s.op("pe", emit_o, reads=[kb("scm"), k("vv"), kb("Z"), kSb(bi), kSb(bm)], writes=[hbk])
                    yield
                    if d == 0:
                        s.op("act", lambda e, i=i, H=H, hb=hb: e.copy(out=OFW[:, i, H], in_=cx.ps[hb][:, OO]),
                             reads=[hbk], writes=[tg + "OFW%d_%d" % (i, hd)])
                        yield
                    else:
                        s.op("dve", lambda e, b=b, i=i, H=H, hb=hb: e.tensor_tensor(
                            out=obuf[b][:, H], in0=cx.ps[hb][:, OO], in1=OFW[:, i, H], op=ALU.add),
                            reads=[hbk, tg + "OFW%d_%d" % (i, hd)], writes=["+" + k("obuf")])
                        yield
                        s.op("act", lambda e, b=b, hd=hd, H=H: e.activation(
                            out=junk[:, H], in_=obuf[b][:, H], func=AF.Square, accum_out=ssq[:, hd:hd + 1]),
                            reads=["+" + k("obuf")], writes=[tg + "junk", "+" + tg + "ssq"])
                        yield

            yield from interleave_gen([head_gen(0), head_gen(1)])

            if lat and d == 1:
                s.op("act", lambda e: e.activation(out=rsq[:], in_=ssq[:], func=AF.Ln, bias=cx.eps_col[:, :],
                                                   scale=1.0 / 128),
                     reads=["+" + tg + "ssq", "eps_col"], writes=[tg + "rsq"])
                s.op("act", lambda e: e.activation(out=rsq[:], in_=rsq[:], func=AF.Exp, scale=-0.5),
                     reads=[tg + "rsq"], writes=[tg + "rsq"])
                for hd in range(2):
                    H = slice(hd * 128, (hd + 1) * 128)
                    s.op("dve", lambda e, b=b, hd=hd, H=H: e.scalar_tensor_tensor(
                        out=yout[b][:, H], in0=obuf[b][:, H], scalar=rsq[:, hd:hd + 1], in1=gg[b][:, H],
                        op0=ALU.mult, op1=ALU.mult),
                        reads=["+" + k("obuf"), tg + "rsq", k("gg")], writes=["+" + k("yout")])
                ydst = dr["ypre_tile"](i) if "ypre_tile" in dr else dr["ypre"][i * 128:(i + 1) * 128, :]
                ykey = dr["ypre_key"](i) if "ypre_key" in dr else tg + "ypre%d" % i
                s.dma("sp", ydst, yout[b][:], reads=["+" + k("yout")], writes=[ykey])
                if "after_out" in dr:
                    dr["after_out"](i)

        def interleave_gen(gens):
            alive = list(gens)
            while alive:
                for g in list(alive):
                    try:
                        next(g)
                    except StopIteration:
                        alive.remove(g)
                yield

        def run_interleaved(gens):
            alive = [g for g in gens if g is not None]
            while alive:
                for g in list(alive):
                    try:
                        next(g)
                    except StopIteration:
                        alive.remove(g)

        for n in range(min(3, len(steps))):
            stageL(n)
        run_interleaved([stageA(0)])
        for n in range(len(steps)):
            run_interleaved([stageA(n + 1) if n + 1 < len(steps) else None, stageB(n)])
            if n + 3 < len(steps):
                stageL(n + 3)


def lay_win5(w_in, hp):
    sec = lambda j: w_in[:, j * D + hp * 256: j * D + (hp + 1) * 256]
    return np.ascontiguousarray(np.concatenate([sec(0), sec(1), sec(3), sec(2), sec(4)], axis=1))


def _dt(nc, name, shape, kind="ExternalInput", dtype=F32):
    return nc.dram_tensor(name, list(shape), dtype, kind=kind).ap()


def build_fourier_prog():
    nc = bass.Bass("TRN2", target_bir_lowering=False)
    dr = dict(x=_dt(nc, "x", [8192, D]), ctx=_dt(nc, "ctx", [256, D]),
              yctx=_dt(nc, "yctx", [256, 256], "ExternalOutput"),
              modw=_dt(nc, "modw", [D, 6 * D]), modb=_dt(nc, "modb", [1, 6 * D]),
              cond_cols=_dt(nc, "cond_cols", [128, 8, 2]), n1g_col=_dt(nc, "n1g_col", [128, 8]),
              winT=_dt(nc, "winT", [256, D]), cs_ch=_dt(nc, "cs_ch", [256, 512]), rs1=_dt(nc, "rs1", [128, 256]),
              rs2=_dt(nc, "rs2", [128, 256]), bdc=_dt(nc, "bdc", [128, 128]), bds=_dt(nc, "bds", [128, 128]),
              c256=_dt(nc, "c256", [256, 256]), s256n=_dt(nc, "s256n", [256, 256]))
    dr["ylat_chunks"] = [_dt(nc, "ylat", [8192, 256], "ExternalOutput")]
    ident = _dt(nc, "ident", [128, 128])
    cx = Ctx(nc)
    load_consts(cx, ident)
    emit_fourier(cx, dr)
    cx.s.barrier(["sp"])
    return nc


def build_hgrn_prog():
    nc = bass.Bass("TRN2", target_bir_lowering=False)
    dr = dict(x=_dt(nc, "x", [8192, D]), xctx=_dt(nc, "xctx", [256, D]),
              ypre=_dt(nc, "ypre", [8192, 256], "ExternalOutput"),
              modw=_dt(nc, "modw", [D, 6 * D]), modb=_dt(nc, "modb", [1, 6 * D]),
              cond_cols=_dt(nc, "cond_cols", [128, 8, 2]), n1g_col=_dt(nc, "n1g_col", [128, 8]),
              win5=_dt(nc, "win5", [D, 1280]), lbrep=_dt(nc, "lbrep", [128, 2, 2, 256]),
              hng_rep=_dt(nc, "hng_rep", [128, 256]), tri_fw=_dt(nc, "tri_fw", [128, 128]),
              tri_bw=_dt(nc, "tri_bw", [128, 128]), su_fw=_dt(nc, "su_fw", [128, 128]),
              su_bw=_dt(nc, "su_bw", [128, 128]))
    ident = _dt(nc, "ident", [128, 128])
    cx = Ctx(nc)
    load_consts(cx, ident)
    emit_hgrn(cx, dr)
    cx.s.barrier(["sp"])
    return nc


def post_tiles(with_ctx):
    p0 = [(i * 128, 128, 1) for i in range(0, 8)]
    p1 = [(i * 128, 128, 1) for i in range(8, 16)]
    if with_ctx:
        p0 = p0 + [(2048, 64, 0)]
    return [p0, p1]


def build_post_prog(with_ctx, final_norm):
    nc = bass.Bass("TRN2", target_bir_lowering=False)
    T = 2112 if with_ctx else 2048
    dr = dict(xres=_dt(nc, "xres", [T, D]), ypre=_dt(nc, "ypre", [T, D]), xout=_dt(nc, "xout", [T, D], "ExternalOutput"),
              wout=_dt(nc, "wout", [D, D]), modw=_dt(nc, "modw", [D, 6 * D]), modb=_dt(nc, "modb", [1, 6 * D]),
              cond_cols=_dt(nc, "cond_cols", [128, 8, 2]), n2g_col=_dt(nc, "n2g_col", [128, 8]),
              wr=_dt(nc, "wr", [D, NE]), br=_dt(nc, "br", [1, NE]), w1r=_dt(nc, "w1r", [NE, 8, 128, 2048]),
              b1c=_dt(nc, "b1c", [128, NE * 16]), w2r=_dt(nc, "w2r", [NE, 128, 8192]), b2=_dt(nc, "b2", [NE, D]),
              fin_rep=_dt(nc, "fin_rep", [128, D]))
    ident = _dt(nc, "ident", [128, 128])
    cx = Ctx(nc)
    load_consts(cx, ident)
    for pi, tiles in enumerate(post_tiles(with_ctx)):
        emit_post(cx, tiles, 2, dr, final_norm, tagp="P%d" % pi)
    cx.s.barrier(["sp"])
    return nc


def _run(nc, in_maps):
    res = run_bass_kernel_spmd(nc, in_maps, core_ids=list(range(NCORES)))
    return res.results


_DEBUG = None


def kernel_unfused(x, c, ctx, c_ctx, mod_w, mod_b, norm1_g, norm2_g, fourier_w_in, fourier_w_out,
           hgrn_w_in, hgrn_lower_bounds, hgrn_norm_g, hgrn_w_out, router_w, router_b,
           expert_w1, expert_b1, expert_w2, expert_b2, final_norm_g):
    dbg = _DEBUG
    f32 = lambda a: np.ascontiguousarray(np.asarray(a, dtype=np.float32))
    x, c, ctx, c_ctx = f32(x), f32(c), f32(ctx), f32(c_ctx)
    mod_w, mod_b = f32(mod_w), f32(mod_b)
    norm1_g, norm2_g = f32(norm1_g), f32(norm2_g)
    ident = np.eye(128, dtype=np.float32)
    B = x.shape[0]
    cond_cols = [lay_cols(np.stack([c_ctx, c[b]])) for b in range(B)]

    fw_in = f32(fourier_w_in)[0]
    consts = dft_consts()
    ims = []
    for j in range(NCORES):
        b, g = j // 4, j % 4
        m = dict(x=x[b], ctx=ctx[b], modw=mod_w[0], modb=mod_b[0][None, :], cond_cols=cond_cols[b],
                 n1g_col=lay_cols(norm1_g[0])[:, :, 0], winT=np.ascontiguousarray(fw_in[:, g * 256:(g + 1) * 256].T),
                 ident=ident)
        m.update(consts)
        ims.append(m)
    r = _run(build_fourier_prog(), ims)
    y_lat = np.empty((B, 8192, D), np.float32)
    y_ctx = np.empty((B, 256, D), np.float32)
    for j in range(NCORES):
        b, g = j // 4, j % 4
        y_lat[b][:, g * 256:(g + 1) * 256] = r[j]["ylat"]
        y_ctx[b][:, g * 256:(g + 1) * 256] = r[j]["yctx"]

    def run_post(layer, xl, xc, yl, yc, wout, with_ctx, final_norm):
        w1r, w2r = lay_w1(f32(expert_w1[layer])), lay_w2(f32(expert_w2[layer]))
        b1c, b2 = lay_b1(f32(expert_b1[layer])), f32(expert_b2[layer])
        xl_f, yl_f = xl.reshape(-1, D), yl.reshape(-1, D)
        ims = []
        for j in range(NCORES):
            b = j // 4
            xr, yp = xl_f[j * 2048:(j + 1) * 2048], yl_f[j * 2048:(j + 1) * 2048]
            if with_ctx:
                xr = np.concatenate([xr, xc.reshape(-1, D)[j * 64:(j + 1) * 64]], 0)
                yp = np.concatenate([yp, yc.reshape(-1, D)[j * 64:(j + 1) * 64]], 0)
            ims.append(dict(xres=np.ascontiguousarray(xr), ypre=np.ascontiguousarray(yp), wout=wout, modw=mod_w[layer],
                            modb=mod_b[layer][None, :], cond_cols=cond_cols[b],
                            n2g_col=lay_cols(norm2_g[layer])[:, :, 0], wr=f32(router_w[layer]),
                            br=f32(router_b[layer])[None, :], w1r=w1r, b1c=b1c, w2r=w2r, b2=b2,
                            fin_rep=np.ascontiguousarray(np.broadcast_to(f32(final_norm_g), (128, D))), ident=ident))
        r = _run(build_post_prog(with_ctx, final_norm), ims)
        xo = np.concatenate([r[j]["xout"][0:2048] for j in range(NCORES)], 0).reshape(B, 8192, D)
        xco = None
        if with_ctx:
            xco = np.concatenate([r[j]["xout"][2048:2112] for j in range(NCORES)], 0).reshape(B, 256, D)
        return xo, xco

    if dbg is not None:
        dbg["y_lat"], dbg["y_ctx"] = y_lat, y_ctx
    x1_lat, x1_ctx = run_post(0, x, ctx, y_lat, y_ctx, f32(fourier_w_out)[0], True, False)
    if dbg is not None:
        dbg["x1_lat"], dbg["x1_ctx"] = x1_lat, x1_ctx

    hw_in = f32(hgrn_w_in)[0]
    lbr = f32(hgrn_lower_bounds)
    hng = f32(hgrn_norm_g)[0]
    hc = hgrn_consts()
    ims = []
    for j in range(NCORES):
        b, hp = j // 4, j % 4
        cols = slice(hp * 256, (hp + 1) * 256)
        m = dict(x=x1_lat[b], xctx=x1_ctx[b], modw=mod_w[1], modb=mod_b[1][None, :], cond_cols=cond_cols[b],
                 n1g_col=lay_cols(norm1_g[1])[:, :, 0], win5=lay_win5(hw_in, hp),
                 lbrep=np.ascontiguousarray(np.broadcast_to(lbr[:, :, cols], (128, 2, 2, 256))),
                 hng_rep=np.ascontiguousarray(np.broadcast_to(hng[cols], (128, 256))), ident=ident)
        m.update(hc)
        ims.append(m)
    r = _run(build_hgrn_prog(), ims)
    y1 = np.empty((B, 8192, D), np.float32)
    for j in range(NCORES):
        b, hp = j // 4, j % 4
        y1[b][:, hp * 256:(hp + 1) * 256] = r[j]["ypre"]

    if dbg is not None:
        dbg["y1"] = y1
    out, _ = run_post(1, x1_lat, None, y1, None, f32(hgrn_w_out)[0], False, True)
    return out


GROUPS = [[0, 1, 2, 3], [4, 5, 6, 7]]


def build_fused_prog():
    nc = bass.Bass("TRN2", target_bir_lowering=False)
    E = lambda name, shape: _dt(nc, name, shape)
    I = lambda name, shape, dt=F32: nc.dram_tensor(name, list(shape), dt)
    ext = dict(
        x=E("x", [8192, D]), ctx=E("ctx", [256, D]), xres0=E("xres0", [2112, D]),
        cond_cols=E("cond_cols", [128, 8, 2]), sel=E("sel", [128, 4]), ident=E("ident", [128, 128]),
        modw0=E("modw0", [D, 6 * D]), modb0=E("modb0", [1, 6 * D]), modw1=E("modw1", [D, 6 * D]),
        modb1=E("modb1", [1, 6 * D]),
        n1g0=E("n1g0", [128, 8]), n1g1=E("n1g1", [128, 8]), n2g0=E("n2g0", [128, 8]), n2g1=E("n2g1", [128, 8]),
        winT=E("winT", [256, D]), fwout=E("fwout", [D, D]), hwout=E("hwout", [D, D]),
        win5=E("win5", [D, 1280]), lbrep=E("lbrep", [128, 2, 2, 256]), hng_rep=E("hng_rep", [128, 256]),
        fin_rep=E("fin_rep", [128, D]))
    for nm, shp in (("cs_ch", [256, 512]), ("rs1", [128, 256]), ("rs2", [128, 256]), ("bdc", [128, 128]),
                    ("bds", [128, 128]), ("c256", [256, 256]), ("s256n", [256, 256]), ("tri_fw", [128, 128]),
                    ("tri_bw", [128, 128]), ("su_fw", [128, 128]), ("su_bw", [128, 128])):
        ext[nm] = E(nm, shp)
    for l in range(2):
        ext["wr%d" % l] = E("wr%d" % l, [D, NE])
        ext["br%d" % l] = E("br%d" % l, [1, NE])
        ext["w1r%d" % l] = E("w1r%d" % l, [NE, 8, 128, 2048])
        ext["b1c%d" % l] = E("b1c%d" % l, [128, NE * 16])
        ext["w2r%d" % l] = E("w2r%d" % l, [NE, 128, 8192])
        ext["b2%d" % l] = E("b2%d" % l, [NE, D])
    xout = _dt(nc, "xout", [2048, D], "ExternalOutput")

    yF = [I("yF%d" % c, [2048, 256], BF16) for c in range(4)]
    yFc = I("yFc", [256, 256], BF16)
    G1 = [I("G1_%d" % c, [4 * 2048, 256], BF16) for c in range(4)]
    G1c = I("G1c", [4 * 256, 256], BF16)
    x1 = [I("x1_%d" % c, [256, D]) for c in range(8)]
    x1c = I("x1c", [64, D])
    h1s = [I("h1s_%d" % c, [4 * 128, KC * 128], BF16) for c in range(4)]
    h1sc = I("h1sc", [128, KC * 64], BF16)
    G2 = [I("G2_%d" % c, [4 * 512, KC * 128], BF16) for c in range(4)]
    G2c = I("G2c", [4 * 128, KC * 64], BF16)
    yH = [I("yH%d" % c, [2048, 256], BF16) for c in range(4)]
    G3 = [I("G3_%d" % c, [4 * 2048, 256], BF16) for c in range(4)]

    cx = Ctx(nc)
    s = cx.s
    load_consts(cx, ext["ident"])

    drF = dict(x=ext["x"], ctx=ext["ctx"], yctx=yFc.ap(), ylat_chunks=[t.ap() for t in yF], out_dt=BF16,
               modw=ext["modw0"], modb=ext["modb0"], cond_cols=ext["cond_cols"], n1g_col=ext["n1g0"],
               winT=ext["winT"])
    for nm in ("cs_ch", "rs1", "rs2", "bdc", "bds", "c256", "s256n"):
        drF[nm] = ext[nm]
    drF["after_out"] = lambda c: s.collective("AllGather", [yF[c].ap().opt()], [G1[c].ap().opt()], GROUPS,
                                              reads=["+ylatc%d" % c])
    drF["after_ctx"] = lambda: s.collective("AllGather", [yFc.ap().opt()], [G1c.ap().opt()], GROUPS,
                                            reads=["+yctxo"])
    emit_fourier(cx, drF)
    s.barrier()

    def x1_tile(row0, rows):
        if row0 >= 2048:
            return x1c.ap()[0:rows, :]
        return x1[row0 // 256].ap()[row0 % 256:row0 % 256 + rows, :]

    def cands0(row0, rows):
        if row0 >= 2048:
            v = G1c.ap().rearrange("(g r) c -> r g c", g=4)
            return [v[64 * k:64 * k + rows] for k in range(4)]
        return [G1[k].ap().rearrange("(g r) c -> r g c", g=4)[row0:row0 + rows] for k in range(4)]

    def post_dr(l, wout, cands, xres_tile, xout_tile):
        return dict(ypre_cands=cands, sel=ext["sel"], xres_tile=xres_tile, xout_tile=xout_tile, wout=wout,
                    modw=ext["modw%d" % l], modb=ext["modb%d" % l], cond_cols=ext["cond_cols"],
                    n2g_col=ext["n2g%d" % l], wr=ext["wr%d" % l], br=ext["br%d" % l], w1r=ext["w1r%d" % l],
                    b1c=ext["b1c%d" % l], w2r=ext["w2r%d" % l], b2=ext["b2%d" % l], fin_rep=ext["fin_rep"])

    def h1_dst(row0, rows):
        if row0 >= 2048:
            return h1sc.ap()[:, :]
        lt = row0 // 128
        return h1s[lt // 4].ap()[(lt % 4) * 128:(lt % 4) * 128 + 128, :]

    dr0 = post_dr(0, ext["fwout"], cands0, lambda row0, rows: ext["xres0"][row0:row0 + rows, :], x1_tile)
    dr0["h1"] = dict(modw=ext["modw1"], modb=ext["modb1"], cond_cols=ext["cond_cols"], n1g_col=ext["n1g1"],
                     tile_dst=h1_dst)
    for pi, tiles in enumerate(post_tiles(True)):
        emit_post(cx, tiles, 2, dr0, False, tagp="P0%d" % pi)
        for c in range(2 * pi, 2 * pi + 2):
            s.collective("AllGather", [h1s[c].ap().opt()], [G2[c].ap().opt()], GROUPS)
        if pi == 0:
            s.collective("AllGather", [h1sc.ap().opt()], [G2c.ap().opt()], GROUPS)
    s.barrier()

    def hx_tile(kind, i):
        if kind == "ctx":
            return [(slice(64 * q, 64 * q + 64), G2c.ap()[(2 * i + q) * 128:(2 * i + q) * 128 + 128, :])
                    for q in range(2)]
        r, lt = i // 16, i % 16
        return [(slice(0, 128), G2[lt // 4].ap()[r * 512 + (lt % 4) * 128:r * 512 + (lt % 4) * 128 + 128, :])]

    drH = dict(h_tile=hx_tile, ypre_tile=lambda i: yH[i // 16].ap()[(128 * i) % 2048:(128 * i) % 2048 + 128, :],
               out_dt=BF16, modw=ext["modw1"], modb=ext["modb1"], cond_cols=ext["cond_cols"], n1g_col=ext["n1g1"],
               win5=ext["win5"], lbrep=ext["lbrep"], hng_rep=ext["hng_rep"])
    for nm in ("tri_fw", "tri_bw", "su_fw", "su_bw"):
        drH[nm] = ext[nm]
    drH["ypre_key"] = lambda i: "+yHc%d" % (i // 16)

    def after_h(i):
        if i % 16 == 0:
            s.collective("AllGather", [yH[i // 16].ap().opt()], [G3[i // 16].ap().opt()], GROUPS,
                         reads=["+yHc%d" % (i // 16)])
    drH["after_out"] = after_h
    emit_hgrn(cx, drH)
    s.barrier()

    cands1 = lambda row0, rows: [G3[k].ap().rearrange("(g r) c -> r g c", g=4)[row0:row0 + rows] for k in range(4)]
    dr1 = post_dr(1, ext["hwout"], cands1, x1_tile, lambda row0, rows: xout[row0:row0 + rows, :])
    for pi, tiles in enumerate(post_tiles(False)):
        emit_post(cx, tiles, 2, dr1, True, tagp="P1%d" % pi)
    s.barrier(["sp"])
    return nc


def kernel(x, c, ctx, c_ctx, mod_w, mod_b, norm1_g, norm2_g, fourier_w_in, fourier_w_out,
           hgrn_w_in, hgrn_lower_bounds, hgrn_norm_g, hgrn_w_out, router_w, router_b,
           expert_w1, expert_b1, expert_w2, expert_b2, final_norm_g):
    f32 = lambda a: np.ascontiguousarray(np.asarray(a, dtype=np.float32))
    x, c, ctx, c_ctx = f32(x), f32(c), f32(ctx), f32(c_ctx)
    mod_w, mod_b = f32(mod_w), f32(mod_b)
    norm1_g, norm2_g = f32(norm1_g), f32(norm2_g)
    B = x.shape[0]
    shared = dict(ident=np.eye(128, dtype=np.float32),
                  modw0=mod_w[0], modb0=mod_b[0][None, :], modw1=mod_w[1], modb1=mod_b[1][None, :],
                  n1g0=lay_cols(norm1_g[0])[:, :, 0], n1g1=lay_cols(norm1_g[1])[:, :, 0],
                  n2g0=lay_cols(norm2_g[0])[:, :, 0], n2g1=lay_cols(norm2_g[1])[:, :, 0],
                  fwout=f32(fourier_w_out)[0], hwout=f32(hgrn_w_out)[0],
                  fin_rep=np.ascontiguousarray(np.broadcast_to(f32(final_norm_g), (128, D))))
    shared.update(dft_consts())
    shared.update(hgrn_consts())
    for l in range(2):
        shared["wr%d" % l] = f32(router_w[l])
        shared["br%d" % l] = f32(router_b[l])[None, :]
        shared["w1r%d" % l] = lay_w1(f32(expert_w1[l]))
        shared["b1c%d" % l] = lay_b1(f32(expert_b1[l]))
        shared["w2r%d" % l] = lay_w2(f32(expert_w2[l]))
        shared["b2%d" % l] = f32(expert_b2[l])
    fw_in = f32(fourier_w_in)[0]
    hw_in = f32(hgrn_w_in)[0]
    lbr = f32(hgrn_lower_bounds)
    hng = f32(hgrn_norm_g)[0]
    x_f, ctx_f = x.reshape(-1, D), ctx.reshape(-1, D)
    ims = []
    for j in range(NCORES):
        b, g = j // 4, j % 4
        cols = slice(g * 256, (g + 1) * 256)
        sel = np.zeros((128, 4), np.float32)
        sel[:, g] = 1.0
        m = dict(shared)
        m.update(x=x[b], ctx=ctx[b],
                 xres0=np.ascontiguousarray(np.concatenate([x_f[j * 2048:(j + 1) * 2048], ctx_f[j * 64:(j + 1) * 64]], 0)),
                 cond_cols=lay_cols(np.stack([c_ctx, c[b]])), sel=sel,
                 winT=np.ascontiguousarray(fw_in[:, cols].T), win5=lay_win5(hw_in, g),
                 lbrep=np.ascontiguousarray(np.broadcast_to(lbr[:, :, cols], (128, 2, 2, 256))),
                 hng_rep=np.ascontiguousarray(np.broadcast_to(hng[cols], (128, 256))))
        ims.append(m)
    r = _run(build_fused_prog(), ims)
    return np.concatenate([r[j]["xout"] for j in range(NCORES)], 0).reshape(B, 8192, D)
```

```python
import numpy as np
from contextlib import ExitStack, contextmanager
import concourse.bass as bass
import concourse.mybir as mybir
from concourse.bass_utils import run_bass_kernel_spmd

F32 = mybir.dt.float32
BF16 = mybir.dt.bfloat16
AF = mybir.ActivationFunctionType
ALU = mybir.AluOpType
AX = mybir.AxisListType

D = 1024
KC = 8
NE = 32
EPS = 1e-6
NCORES = 8


class Sched:
    ENG = ("pe", "dve", "act", "pool", "sp")

    def __init__(self, nc, n_dma_sems=48):
        self.nc = nc
        self.e = {"pe": nc.tensor, "dve": nc.vector, "act": nc.scalar,
                  "pool": nc.gpsimd, "sp": nc.sync}
        self.sem = {k: nc.alloc_semaphore("sem_" + k) for k in self.ENG}
        self.cnt = {k: 0 for k in self.ENG}
        self.dsem = [nc.alloc_semaphore("dsem%d" % i) for i in range(n_dma_sems)]
        self.dval = [0] * n_dma_sems
        self.dnext = 0
        self.n_hw = (n_dma_sems * 3) // 4
        self.dnext_sw = self.n_hw
        self.seen = {k: {} for k in self.ENG}
        self.lastw = {}
        self.readers = {}
        self.multiw = {}
        self.genreaders = {}
        self.pslock = {}
        self.know = {}
        self.ccsems = []
        self.cctoks = []

    def _wait(self, eng, tok):
        key, sem, val = tok
        if key == "pe" and eng == "pe":
            return
        if self.seen[eng].get(key, 0) >= val:
            return
        self.e[eng].wait_ge(sem, val)
        self.seen[eng][key] = val
        snap = self.know.get((key, val))
        if snap:
            mine = self.seen[eng]
            for k2, v2 in snap.items():
                if mine.get(k2, 0) < v2:
                    mine[k2] = v2

    def _deps(self, eng, reads, writes):
        for r in reads:
            if r.startswith("+"):
                for t in self.multiw.get(r, {}).values():
                    self._wait(eng, t)
                continue
            t = self.lastw.get(r)
            if t is not None:
                self._wait(eng, t)
        for w in writes:
            if not w.startswith("+"):
                t = self.lastw.get(w)
                if t is not None:
                    self._wait(eng, t)
            else:
                for t in self.genreaders.get(w, {}).values():
                    self._wait(eng, t)
            for t in self.readers.get(w, {}).values():
                self._wait(eng, t)

    def _record(self, tok, reads, writes):
        for w in writes:
            if w.startswith("+"):
                if self.readers.get(w):
                    self.multiw[w] = {}
                    self.genreaders[w] = dict(self.readers[w])
                self.multiw.setdefault(w, {})[tok[0]] = tok
            else:
                self.lastw[w] = tok
            self.readers[w] = {}
        for r in reads:
            if r in writes:
                continue
            self.readers.setdefault(r, {})[tok[0]] = tok

    def op(self, eng, emit, reads=(), writes=()):
        self._deps(eng, reads, writes)
        for r in reads:
            if r.startswith("ps"):
                t = self.pslock.get(r)
                if t is not None and t[0] != eng:
                    self._wait(eng, t)
        ins = emit(self.e[eng])
        self.cnt[eng] += 1
        ins.then_inc(self.sem[eng], 1)
        tok = (eng, self.sem[eng], self.cnt[eng])
        self.know[(eng, self.cnt[eng])] = dict(self.seen[eng])
        self._record(tok, reads, writes)
        for r in list(reads) + list(writes):
            if r.startswith("ps"):
                self.pslock[r] = tok
        return tok

    def dma(self, q, out, in_, reads=(), writes=()):
        self._deps(q, reads, writes)
        if q == "pool":
            i = self.dnext_sw
            self.dnext_sw = self.n_hw + (i + 1 - self.n_hw) % (len(self.dsem) - self.n_hw)
        else:
            i = self.dnext
            self.dnext = (i + 1) % self.n_hw
        key = "d%d" % i
        if self.dval[i] > 0:
            self._wait(q, (key, self.dsem[i], self.dval[i]))
        self.dval[i] += 16
        self.e[q].dma_start(out=out, in_=in_).then_inc(self.dsem[i], 16)
        tok = (key, self.dsem[i], self.dval[i])
        self.know[(key, self.dval[i])] = dict(self.seen[q])
        self._record(tok, reads, writes)
        return tok

    def collective(self, kind, ins, outs, groups, reads=(), writes=()):
        q = "pool"
        self._deps(q, reads, writes)
        sem = self.nc.alloc_semaphore("ccsem%d" % len(self.ccsems))
        self.ccsems.append(sem)
        self.e[q].collective_compute(kind, ALU.bypass, replica_groups=groups, ins=ins, outs=outs).then_inc(sem, 1)
        tok = ("cc%d" % len(self.ccsems), sem, 1)
        self._record(tok, reads, writes)
        self.cctoks.append(tok)
        return tok

    def barrier(self, engines=None):
        engines = engines or self.ENG
        for eng in engines:
            for f in self.ENG:
                if self.cnt[f] > 0:
                    self._wait(eng, (f, self.sem[f], self.cnt[f]))
            for i, v in enumerate(self.dval):
                if v > 0:
                    self._wait(eng, ("d%d" % i, self.dsem[i], v))
            for tok in self.cctoks:
                self._wait(eng, tok)


def ps_banks(nc):
    return [nc.alloc_psum_tensor("psb%d" % i, [128, 512], F32) for i in range(8)]


class Ctx:
    def __init__(self, nc):
        self.nc = nc
        self.s = Sched(nc)
        self.ps = ps_banks(nc)
        self.uid = 0
        self.stacks = []
        a = lambda name, shape, dt: nc.alloc_sbuf_tensor(name, shape, dt)
        self.ident = a("ident_sb", [128, 128], F32)
        self.ones_row = a("ones_row", [1, 128], F32)
        self.eps_col = a("eps_col", [128, 1], F32)
        self.one_col = a("one_col", [128, 1], F32)

    def name(self, base):
        self.uid += 1
        return "%s_%d" % (base, self.uid)

    @contextmanager
    def scope(self):
        st = ExitStack()
        self.stacks.append(st)
        try:
            yield
        finally:
            self.s.barrier()
            self.s.lastw = {}
            self.s.readers = {}
            self.s.multiw = {}
            self.s.genreaders = {}
            self.s.know = {}
            self.stacks.pop()
            st.close()

    def A(self, name, shape, dt):
        g = self.nc.sbuf_tensor(self.name(name), shape, dt)
        return self.stacks[-1].enter_context(g)


def load_consts(cx, ident_dram):
    s = cx.s
    s.dma("sp", cx.ident[:], ident_dram, writes=["ident"])
    s.op("dve", lambda e: e.memset(cx.ones_row[:], 1.0), writes=["ones_row"])
    s.op("dve", lambda e: e.memset(cx.eps_col[:], EPS), writes=["eps_col"])
    s.op("dve", lambda e: e.memset(cx.one_col[:], 1.0), writes=["one_col"])


def emit_rstd(cx, x_ap, rows, junk, ss, rstd, xkey, tag):
    s = cx.s
    s.op("act", lambda e: e.activation(out=junk[0:rows, :], in_=x_ap, func=AF.Square,
                                       accum_out=ss[0:rows, :]),
         reads=[xkey, "eps_col"], writes=[tag + "junk", tag + "ss"])
    s.op("act", lambda e: e.activation(out=rstd[0:rows, :], in_=ss[0:rows, :], func=AF.Ln,
                                       bias=cx.eps_col[0:rows, :], scale=1.0 / D),
         reads=[tag + "ss", "eps_col"], writes=[tag + "rstd"])
    s.op("act", lambda e: e.activation(out=rstd[0:rows, :], in_=rstd[0:rows, :], func=AF.Exp,
                                       scale=-0.5),
         reads=[tag + "rstd"], writes=[tag + "rstd"])


def emit_transpose8(cx, src, rows, bank_a, bank_b, srckey):
    s = cx.s
    pa, pb = cx.ps[bank_a], cx.ps[bank_b]

    def emit_half(e, bank, k0):
        ins = None
        for q in range(4):
            kc = k0 + q
            ins = e.transpose(out=bank[:, q * 128:q * 128 + rows],
                              in_=src[0:rows, kc * 128:(kc + 1) * 128],
                              identity=cx.ident[0:rows, 0:rows])
        return ins
    s.op("pe", lambda e: emit_half(e, pa, 0), reads=[srckey, "ident"], writes=["ps%d" % bank_a])
    s.op("pe", lambda e: emit_half(e, pb, 4), reads=[srckey, "ident"], writes=["ps%d" % bank_b])


def emit_mod_cols(cx, modw, modb_row, silu_cols, ncond, col_blocks, out_cols, tag):
    s = cx.s
    with cx.scope():
        wbuf = [cx.A("mcw", [128, KC, 128], F32) for _ in range(2)]
        for i, blk in enumerate(col_blocks):
            wb = wbuf[i % 2]
            wk = tag + "mcw%d" % (i % 2)
            s.dma("sp", wb[:], modw[:, blk * 128:(blk + 1) * 128].rearrange("(kc p) n -> p kc n", p=128),
                  writes=[wk])
            bank = 4 + (i % 4)

            def emit(e, wb=wb, blk=blk, bank=bank):
                pt = cx.ps[bank]
                for kc in range(KC):
                    e.matmul(pt[:, 0:ncond], wb[:, kc, :], silu_cols[:, kc, :], start=(kc == 0), stop=False)
                return e.matmul(pt[:, 0:ncond], modb_row[0:1, blk * 128:(blk + 1) * 128],
                                cx.ones_row[0:1, 0:ncond], start=False, stop=True)
            s.op("pe", emit, reads=[wk, tag + "silu", tag + "modb", "ones_row"], writes=["ps%d" % bank])
            s.op("dve", lambda e, i=i, bank=bank: e.tensor_copy(out_cols[:, i, :], cx.ps[bank][:, 0:ncond]),
                 reads=["ps%d" % bank], writes=["+" + tag + "cols"])


def emit_mod_rows(cx, modw, modb_row, silu_rep, ncond, col0, out_rep, tag, okey):
    s = cx.s
    with cx.scope():
        wbuf = [cx.A("mrw", [128, KC, 512], F32) for _ in range(2)]
        for h in range(2):
            wb = wbuf[h]
            wk = tag + "mrw%d" % h
            c0 = col0 + h * 512
            s.dma("sp", wb[:], modw[:, c0:c0 + 512].rearrange("(kc p) n -> p kc n", p=128), writes=[wk])
            for c in range(ncond):
                bank = 4 + ((2 * h + c) % 4)

                def emit(e, wb=wb, c=c, c0=c0, bank=bank):
                    pt = cx.ps[bank]
                    for kc in range(KC):
                        e.matmul(pt[:, :], silu_rep[:, c, kc, :], wb[:, kc, :], start=(kc == 0), stop=False)
                    return e.matmul(pt[:, :], cx.ones_row[0:1, :], modb_row[0:1, c0:c0 + 512],
                                    start=False, stop=True)
                s.op("pe", emit, reads=[wk, tag + "silurep", tag + "modb", "ones_row"], writes=["ps%d" % bank])
                s.op("dve", lambda e, c=c, h=h, bank=bank: e.tensor_copy(
                    out_rep[:, c, h * 512:(h + 1) * 512], cx.ps[bank][:, :]),
                    reads=["ps%d" % bank], writes=[okey])


def emit_silu_cond(cx, cond_cols_dram, ncond, tag, need_rep):
    s = cx.s
    cc = cx.A("condc", [128, KC, ncond], F32)
    sg = cx.A("conds", [128, KC, ncond], F32)
    ck, sk = tag + "silu", tag + "sg"
    s.dma("sp", cc[:], cond_cols_dram, writes=[ck])
    s.op("act", lambda e: e.activation(out=sg[:], in_=cc[:], func=AF.Exp, scale=-1.0), reads=[ck], writes=[sk])
    s.op("dve", lambda e: e.tensor_scalar(out=sg[:], in0=sg[:], scalar1=1.0, scalar2=None, op0=ALU.add),
         reads=[sk], writes=[sk])
    s.op("dve", lambda e: e.reciprocal(out=sg[:], in_=sg[:]), reads=[sk], writes=[sk])
    s.op("dve", lambda e: e.tensor_tensor(out=cc[:], in0=cc[:], in1=sg[:], op=ALU.mult),
         reads=[sk, ck], writes=[ck])
    rep = None
    if need_rep:
        rep = cx.A("condrep", [128, ncond, KC, 128], F32)
        s.op("pool", lambda e: e.memset(rep[:], 1.0), writes=[tag + "silurep"])
        for c in range(ncond):
            for kc in range(KC):
                s.op("dve", lambda e, c=c, kc=kc: e.tensor_scalar(
                    out=rep[:, c, kc, :], in0=rep[:, c, kc, :], scalar1=cc[:, kc, c:c + 1], scalar2=None,
                    op0=ALU.mult), reads=[ck, tag + "silurep"], writes=[tag + "silurep"])
    return cc, rep


def emit_post_params(cx, dr, ncond, tg):
    s, A = cx.s, cx.A
    m2 = A("m2cols", [128, 16, ncond], F32)
    A2 = A("A2", [128, KC, ncond], F32)
    g1rep = A("g1rep", [128, ncond, D], F32)
    g2rep = A("g2rep", [128, ncond, D], F32)
    with cx.scope():
        modb_row = A("modb", [1, 6 * D], F32)
        s.dma("sp", modb_row[:], dr["modb"], writes=[tg + "modb"])
        silu_cols, silu_rep = emit_silu_cond(cx, dr["cond_cols"], ncond, tg, True)
        emit_mod_cols(cx, dr["modw"], modb_row, silu_cols, ncond, list(range(24, 40)), m2, tg)
        n2g = A("n2g", [128, KC], F32)
        s.dma("sp", n2g[:], dr["n2g_col"], writes=[tg + "n2g"])
        for c in range(ncond):
            s.op("dve", lambda e, c=c: e.scalar_tensor_tensor(
                out=A2[:, :, c], in0=m2[:, 8:16, c], scalar=1.0, in1=n2g[:, :], op0=ALU.add, op1=ALU.mult),
                reads=["+" + tg + "cols", tg + "n2g"], writes=[tg + "A2"])
        emit_mod_rows(cx, dr["modw"], modb_row, silu_rep, ncond, 2 * D, g1rep, tg + "g1", tg + "g1rep")
        emit_mod_rows(cx, dr["modw"], modb_row, silu_rep, ncond, 5 * D, g2rep, tg + "g2", tg + "g2rep")
    return dict(m2=m2, A2=A2, g1rep=g1rep, g2rep=g2rep)


def emit_post(cx, tiles, ncond, dr, final_norm, n_exp=NE, tagp="P", shared=None):
    s = cx.s
    A = cx.A
    T = sum(r for _, r, _ in tiles)
    nt = len(tiles)
    col0 = []
    c = 0
    for _, r, _ in tiles:
        col0.append(c)
        c += r
    tg = tagp
    ngr = -(-T // 512)
    gsz = -(-T // ngr)
    groups = []
    c = 0
    while c < T:
        w = min(gsz, T - c)
        groups.append((c, w))
        c += w

    with cx.scope():
        XA = A("XA", [128, nt, D], F32)
        H2T = A("H2T", [128, KC, T], BF16)
        gates = A("gates", [128, nt, n_exp], F32)
        if shared is None:
            shared = emit_post_params(cx, dr, ncond, tg)
        m2, A2, g1rep, g2rep = shared["m2"], shared["A2"], shared["g1rep"], shared["g2rep"]
        tmpa = [A("tmpa", [128, 512], F32) for _ in range(2)]
        junk = A("junk", [128, D], F32)
        ss = A("ss", [128, 1], F32)
        rstd = A("rstd", [128, 1], F32)
        b1c = A("b1c", [128, n_exp * 16], F32)
        s.dma("sp", b1c[:], dr["b1c"], writes=[tg + "b1c"])
        b1c1 = A("b1c1", [128, n_exp * 16], F32)
        s.op("dve", lambda e: e.tensor_scalar(out=b1c1[:], in0=b1c[:], scalar1=1.0, scalar2=None, op0=ALU.add),
             reads=[tg + "b1c"], writes=[tg + "b1c1"])
        if final_norm:
            finrep = A("finrep", [128, D], F32)
            s.dma("sp", finrep[:], dr["fin_rep"], writes=[tg + "finrep"])

        with cx.scope():
            wout = A("wout", [128, KC, D], BF16)
            s.dma("pool", wout[:], dr["wout"].rearrange("(kc p) n -> p kc n", p=128), writes=[tg + "wout"])
            wr = A("wr", [128, KC, n_exp], F32)
            s.dma("sp", wr[:], dr["wr"].rearrange("(kc p) n -> p kc n", p=128), writes=[tg + "wr"])
            br = A("br", [1, n_exp], F32)
            s.dma("sp", br[:], dr["br"], writes=[tg + "br"])
            b2 = A("b2", [n_exp, D], F32)
            s.dma("sp", b2[:], dr["b2"], writes=[tg + "b2"])

            yt = [A("yt", [128, D], F32) for _ in range(2)]
            tmpf = [[A("tmpf", [128, 512], F32) for _ in range(2)] for _ in range(2)]
            ssf = [A("ssf", [128, 1], F32) for _ in range(2)]
            rstdf = [A("rstdf", [128, 1], F32) for _ in range(2)]
            junkf = [A("junkf", [128, D], BF16) for _ in range(2)]
            if "ypre_cands" in dr:
                cand = [[A("cand", [128, 4, 256], BF16) for _ in range(4)] for _ in range(2)]
                sel = A("sel", [128, 4], F32)
                s.dma("sp", sel[:], dr["sel"], writes=[tg + "sel"])
            xt = [A("xt", [128, D], F32) for _ in range(2)]
            xn = [A("xn", [128, D], F32) for _ in range(2)]
            ypT = [A("ypT", [128, KC, 128], BF16) for _ in range(2)]
            h2f = [A("h2f", [128, KC, 128], F32) for _ in range(2)]
            lg = [A("lg", [128, n_exp], F32) for _ in range(2)]
            mx8 = [A("mx8", [128, 8], F32) for _ in range(2)]
            negm = [A("negm", [128, 1], F32) for _ in range(2)]
            msk = [A("msk", [128, n_exp], F32) for _ in range(2)]
            ex = [A("ex", [128, n_exp], F32) for _ in range(2)]
            ssum = [A("ssum", [128, 1], F32) for _ in range(2)]
            gT = [A("gT", [n_exp, 128], F32) for _ in range(2)]

            def front_gen(ti, row0, rows, cond):
                b = ti % 2
                B0 = 4 * b
                ytk, xtk, xnk, ypk, h2k = (tg + "yt%d" % b, tg + "xt%d" % b, tg + "xn%d" % b,
                                           "+" + tg + "ypT%d" % b, "+" + tg + "h2f%d" % b)
                xak = tg + "XA%d" % ti
                R = slice(0, rows)
                if "ypre_cands" in dr:
                    cands = dr["ypre_cands"](row0, rows)
                    for kq, cap in enumerate(cands):
                        s.dma("sp", cand[b][kq][R, :, :], cap, reads=dr.get("ypre_keys", []), writes=[tg + "cand%d_%d" % (b, kq)])
                    s.op("dve", lambda e, b=b, R=R: e.tensor_scalar(
                        out=yt[b][R, :], in0=cand[b][0][R, :, :].rearrange("p g c -> p (g c)"), scalar1=sel[R, 0:1],
                        scalar2=None, op0=ALU.mult), reads=[tg + "cand%d_0" % b, tg + "sel"], writes=[ytk])
                    yield
                    for kq in range(1, 4):
                        s.op("dve", lambda e, b=b, R=R, kq=kq: e.scalar_tensor_tensor(
                            out=yt[b][R, :], in0=cand[b][kq][R, :, :].rearrange("p g c -> p (g c)"),
                            scalar=sel[R, kq:kq + 1], in1=yt[b][R, :], op0=ALU.mult, op1=ALU.add),
                            reads=[tg + "cand%d_%d" % (b, kq), tg + "sel", ytk], writes=[ytk])
                        yield
                else:
                    s.dma("sp", yt[b][R, :], dr["ypre"][row0:row0 + rows, :], writes=[ytk])
                xsrc = dr["xres_tile"](row0, rows) if "xres_tile" in dr else dr["xres"][row0:row0 + rows, :]
                s.dma("sp", xt[b][R, :], xsrc, writes=[xtk])
                emit_transpose8(cx, yt[b], rows, B0, B0 + 1, ytk)
                yield
                s.op("act", lambda e, b=b, rows=rows: e.copy(
                    out=ypT[b][:, 0:4, 0:rows],
                    in_=cx.ps[B0][:, :].rearrange("p (q t) -> p q t", q=4)[:, :, 0:rows]),
                    reads=["ps%d" % B0], writes=[ypk])
                yield
                s.op("dve", lambda e, b=b, rows=rows: e.tensor_copy(
                    ypT[b][:, 4:8, 0:rows], cx.ps[B0 + 1][:, :].rearrange("p (q t) -> p q t", q=4)[:, :, 0:rows]),
                    reads=["ps%d" % (B0 + 1)], writes=[ypk])
                yield
                for h in range(2):
                    bank = B0 + 2 + h
                    H = slice(h * 512, (h + 1) * 512)

                    def emit(e, b=b, H=H, bank=bank, rows=rows):
                        ins = None
                        for kc in range(KC):
                            ins = e.matmul(cx.ps[bank][0:rows, :], ypT[b][:, kc, 0:rows], wout[:, kc, H],
                                           start=(kc == 0), stop=(kc == KC - 1))
                        return ins
                    s.op("pe", emit, reads=[ypk, tg + "wout"], writes=["ps%d" % bank])
                    yield
                    tk = tg + "tmpf%d_%d" % (b, h)
                    s.op("dve", lambda e, h=h, H=H, bank=bank, R=R, cond=cond: e.tensor_tensor(
                        out=tmpf[b][h][R, :], in0=cx.ps[bank][R, :], in1=g1rep[R, cond, H], op=ALU.mult),
                        reads=["ps%d" % bank, tg + "g1rep"], writes=[tk])
                    yield
                    s.op("pool", lambda e, h=h, H=H, R=R, b=b, ti=ti: e.tensor_tensor(
                        out=XA[R, ti, H], in0=tmpf[b][h][R, :], in1=xt[b][R, H], op=ALU.add),
                        reads=[tk, xtk], writes=[xak])
                    yield
                emit_rstd(cx, XA[R, ti, :], rows, junkf[b], ssf[b], rstdf[b], xak, tg + "f%d" % b)
                yield
                s.op("dve", lambda e, R=R, b=b, ti=ti: e.tensor_scalar(
                    out=xn[b][R, :], in0=XA[R, ti, :], scalar1=rstdf[b][R, :], scalar2=None, op0=ALU.mult),
                    reads=[xak, tg + "f%d" % b + "rstd"], writes=[xnk])
                yield
                emit_transpose8(cx, xn[b], rows, B0, B0 + 1, xnk)
                yield
                for kc in range(KC):
                    bank = B0 + kc // 4
                    q = kc % 4
                    if kc % 2 == 0:
                        s.op("act", lambda e, b=b, kc=kc, q=q, bank=bank, rows=rows, cond=cond: e.activation(
                            out=h2f[b][:, kc, 0:rows], in_=cx.ps[bank][:, q * 128:q * 128 + rows],
                            func=AF.Identity, bias=m2[:, kc, cond:cond + 1], scale=A2[:, kc, cond:cond + 1]),
                            reads=["ps%d" % bank, tg + "A2", "+" + tg + "cols"], writes=[h2k])
                        yield
                    else:
                        s.op("dve", lambda e, b=b, kc=kc, q=q, bank=bank, rows=rows, cond=cond: e.tensor_scalar(
                            out=h2f[b][:, kc, 0:rows], in0=cx.ps[bank][:, q * 128:q * 128 + rows],
                            scalar1=A2[:, kc, cond:cond + 1], scalar2=m2[:, kc, cond:cond + 1],
                            op0=ALU.mult, op1=ALU.add),
                            reads=["ps%d" % bank, tg + "A2", "+" + tg + "cols"], writes=[h2k])
                        yield
                c0 = col0[ti]
                s.op("pool", lambda e, b=b, rows=rows, c0=c0: e.tensor_copy(
                    H2T[:, :, c0:c0 + rows], h2f[b][:, :, 0:rows]), reads=[h2k], writes=[tg + "H2T%d" % ti])
                yield

                def emit_r(e, b=b, rows=rows):
                    for kc in range(KC):
                        e.matmul(cx.ps[B0 + 2][0:rows, 0:n_exp], h2f[b][:, kc, 0:rows], wr[:, kc, :],
                                 start=(kc == 0), stop=False)
                    return e.matmul(cx.ps[B0 + 2][0:rows, 0:n_exp], cx.ones_row[0:1, 0:rows], br[0:1, :],
                                    start=False, stop=True)
                s.op("pe", emit_r, reads=[h2k, tg + "wr", tg + "br", "ones_row"], writes=["ps%d" % (B0 + 2)])
                yield
                s.op("dve", lambda e, R=R: e.tensor_copy(lg[b][R, :], cx.ps[B0 + 2][R, 0:n_exp]),
                     reads=["ps%d" % (B0 + 2)], writes=[tg + "lg%d" % b])
                yield
                s.op("dve", lambda e, R=R: e.max(out=mx8[b][R, :], in_=lg[b][R, :]), reads=[tg + "lg%d" % b], writes=[tg + "mx8%d" % b])
                yield
                s.op("dve", lambda e, R=R: e.tensor_scalar(out=negm[b][R, :], in0=mx8[b][R, 0:1], scalar1=-1.0,
                                                           scalar2=None, op0=ALU.mult),
                     reads=[tg + "mx8%d" % b], writes=[tg + "negm%d" % b])
                yield
                s.op("dve", lambda e, R=R: e.tensor_scalar(out=msk[b][R, :], in0=lg[b][R, :], scalar1=mx8[b][R, 3:4],
                                                           scalar2=None, op0=ALU.is_ge),
                     reads=[tg + "lg%d" % b, tg + "mx8%d" % b], writes=[tg + "msk%d" % b])
                yield
                s.op("act", lambda e, R=R: e.activation(out=ex[b][R, :], in_=lg[b][R, :], func=AF.Exp, bias=negm[b][R, :],
                                                        scale=1.0),
                     reads=[tg + "lg%d" % b, tg + "negm%d" % b], writes=[tg + "ex%d" % b])
                yield
                s.op("dve", lambda e, R=R: e.tensor_tensor(out=ex[b][R, :], in0=ex[b][R, :], in1=msk[b][R, :], op=ALU.mult),
                     reads=[tg + "ex%d" % b, tg + "msk%d" % b], writes=[tg + "ex%d" % b])
                yield
                s.op("dve", lambda e, R=R: e.reduce_sum(out=ssum[b][R, :], in_=ex[b][R, :], axis=AX.X),
                     reads=[tg + "ex%d" % b], writes=[tg + "ssum%d" % b])
                yield
                s.op("dve", lambda e, R=R: e.reciprocal(out=ssum[b][R, :], in_=ssum[b][R, :]), reads=[tg + "ssum%d" % b],
                     writes=[tg + "ssum%d" % b])
                yield
                gk = tg + "gates%d" % ti
                s.op("dve", lambda e, R=R, ti=ti: e.tensor_scalar(out=gates[R, ti, :], in0=ex[b][R, :],
                                                                  scalar1=ssum[b][R, :], scalar2=None, op0=ALU.mult),
                     reads=[tg + "ex%d" % b, tg + "ssum%d" % b], writes=[gk])
                yield
                s.op("pe", lambda e, R=R, ti=ti, rows=rows: e.transpose(
                    out=cx.ps[B0 + 3][0:n_exp, 0:rows], in_=gates[R, ti, :], identity=cx.ident[R, R]),
                    reads=[gk, "ident"], writes=["ps%d" % (B0 + 3)])
                yield
                s.op("dve", lambda e, rows=rows: e.tensor_copy(gT[b][:, 0:rows], cx.ps[B0 + 3][0:n_exp, 0:rows]),
                     reads=["ps%d" % (B0 + 3)], writes=[tg + "gT%d" % b])
                yield
                for h in range(2):
                    bank = B0 + h
                    H = slice(h * 512, (h + 1) * 512)
                    s.op("pe", lambda e, H=H, bank=bank, rows=rows: e.matmul(
                        cx.ps[bank][0:rows, :], gT[b][:, 0:rows], b2[:, H], start=True, stop=True),
                        reads=[tg + "gT%d" % b, tg + "b2"], writes=["ps%d" % bank])
                    yield
                    tk = tg + "tmpf%d_%d" % (b, h)
                    s.op("dve", lambda e, h=h, H=H, bank=bank, R=R, cond=cond: e.tensor_tensor(
                        out=tmpf[b][h][R, :], in0=cx.ps[bank][R, :], in1=g2rep[R, cond, H], op=ALU.mult),
                        reads=["ps%d" % bank, tg + "g2rep"], writes=[tk])
                    yield
                    s.op("pool", lambda e, h=h, H=H, R=R, ti=ti: e.tensor_tensor(
                        out=XA[R, ti, H], in0=tmpf[b][h][R, :], in1=XA[R, ti, H], op=ALU.add),
                        reads=[tk, xak], writes=[xak])
                    yield

            for ti0 in range(0, len(tiles), 2):
                gens = [front_gen(ti, *tiles[ti]) for ti in range(ti0, min(ti0 + 2, len(tiles)))]
                alive = list(gens)
                while alive:
                    for g_ in list(alive):
                        try:
                            next(g_)
                        except StopIteration:
                            alive.remove(g_)

        with cx.scope():
            ACTT = A("ACTT", [128, KC, T], BF16)
            NW1 = 3
            w1b = [A("w1b", [128, KC, 256], BF16) for _ in range(NW1)]
            w2b = [A("w2b", [128, KC, D], BF16) for _ in range(2)]
            wstage = [A("wstage", [128, 2048], F32) for _ in range(4)]
            gbuf = [A("gbuf", [128, 512], F32) for _ in range(2)]
            sgbuf = [A("sgbuf", [128, 512], F32) for _ in range(2)]
            l0buf = [A("l0buf", [128, 512], F32) for _ in range(2)]
            tbuf = [A("tbuf", [128, 512], F32) for _ in range(2)]
            h2keys = [tg + "H2T%d" % ti for ti in range(nt)]
            wsc = [0]

            def fetch_w1(ci):
                ex_i, j = divmod(ci, KC)
                sb_ = wstage[wsc[0] % len(wstage)]
                sk_w = tg + "wst%d" % (wsc[0] % len(wstage))
                wsc[0] += 1
                s.dma("sp", sb_[:], dr["w1r"][ex_i, j], writes=[sk_w])
                s.op("act", lambda e, wb=w1b[ci % NW1], sb_=sb_: e.copy(
                    out=wb[:, :, :].rearrange("p kc n -> p (kc n)"), in_=sb_[:]),
                    reads=[sk_w], writes=[tg + "w1b%d" % (ci % NW1)])

            def fetch_w2(ex_i, qq):
                sb_ = wstage[wsc[0] % len(wstage)]
                sk_w = tg + "wst%d" % (wsc[0] % len(wstage))
                wsc[0] += 1
                s.dma("sp", sb_[:], dr["w2r"][ex_i][:, qq * 2048:(qq + 1) * 2048], writes=[sk_w])
                s.op("act", lambda e, wb=w2b[ex_i % 2], qq=qq, sb_=sb_: e.copy(
                    out=wb[:, 2 * qq:2 * qq + 2, :].rearrange("p j n -> p (j n)"), in_=sb_[:]),
                    reads=[sk_w], writes=[tg + "w2b%d" % (ex_i % 2)])

            nchunks = n_exp * KC
            fetch_w1(0)
            if nchunks > 1:
                fetch_w1(1)
            it = 0
            st2 = 0
            for ex_i in range(n_exp):
                wb2 = w2b[ex_i % 2]
                w2k = tg + "w2b%d" % (ex_i % 2)
                for j in range(KC):
                    ci = ex_i * KC + j
                    if ci + 2 < nchunks:
                        fetch_w1(ci + 2)
                    if j % 2 == 0:
                        fetch_w2(ex_i, j // 2)
                    wb1 = w1b[ci % NW1]
                    w1k = tg + "w1b%d" % (ci % NW1)
                    bg = b1c[:, ex_i * 16 + j:ex_i * 16 + j + 1]
                    bl1 = b1c1[:, ex_i * 16 + 8 + j:ex_i * 16 + 8 + j + 1]
                    for (g0, gw) in groups:
                        p = it % 2
                        it += 1
                        bg_bank, bl_bank = 2 * p, 2 * p + 1

                        def emit1(e, wb1=wb1, g0=g0, gw=gw, bg_bank=bg_bank, bl_bank=bl_bank):
                            ins = None
                            for kc in range(KC):
                                ins = e.matmul(cx.ps[bg_bank][:, 0:gw], wb1[:, kc, 0:128], H2T[:, kc, g0:g0 + gw],
                                               start=(kc == 0), stop=(kc == KC - 1))
                            for kc in range(KC):
                                ins = e.matmul(cx.ps[bl_bank][:, 0:gw], wb1[:, kc, 128:256], H2T[:, kc, g0:g0 + gw],
                                               start=(kc == 0), stop=(kc == KC - 1))
                            return ins
                        s.op("pe", emit1, reads=[w1k] + h2keys, writes=["ps%d" % bg_bank, "ps%d" % bl_bank])
                        gk_, sk_, l0k, tk_ = (tg + "gb%d" % p, tg + "sb%d" % p, tg + "l0%d" % p, tg + "tb%d" % p)
                        W = slice(0, gw)
                        s.op("dve", lambda e, p=p, W=W, bg=bg, bank=bg_bank: e.tensor_scalar(
                            out=gbuf[p][:, W], in0=cx.ps[bank][:, W], scalar1=bg, scalar2=7.0,
                            op0=ALU.add, op1=ALU.min), reads=["ps%d" % bg_bank, tg + "b1c"], writes=[gk_])
                        s.op("act", lambda e, p=p, W=W: e.activation(
                            out=sgbuf[p][:, W], in_=gbuf[p][:, W], func=AF.Sigmoid, scale=1.702),
                            reads=[gk_], writes=[sk_])
                        s.op("dve", lambda e, p=p, W=W, bl1=bl1, bank=bl_bank: e.tensor_scalar(
                            out=l0buf[p][:, W], in0=cx.ps[bank][:, W], scalar1=bl1, scalar2=8.0,
                            op0=ALU.add, op1=ALU.min), reads=["ps%d" % bl_bank, tg + "b1c1"], writes=[l0k])
                        s.op("pool", lambda e, p=p, W=W: e.tensor_tensor(
                            out=tbuf[p][:, W], in0=gbuf[p][:, W], in1=sgbuf[p][:, W], op=ALU.mult),
                            reads=[gk_, sk_], writes=[tk_])
                        s.op("dve", lambda e, p=p, W=W, j=j, g0=g0, gw=gw: e.scalar_tensor_tensor(
                            out=ACTT[:, j, g0:g0 + gw], in0=l0buf[p][:, W], scalar=-6.0, in1=tbuf[p][:, W],
                            op0=ALU.max, op1=ALU.mult),
                            reads=[tk_, l0k], writes=[tg + "ACTT%d_%d" % (j, g0)])
                for ti, (row0, rows, cond) in enumerate(tiles):
                    c0 = col0[ti]
                    gs = [g0 for (g0, gw) in groups if g0 < c0 + rows and c0 < g0 + gw]
                    akeys = [tg + "ACTT%d_%d" % (j, g0) for j in range(KC) for g0 in gs]
                    xak = tg + "XA%d" % ti
                    R = slice(0, rows)
                    for h in range(2):
                        bank = 4 + (st2 % 4)
                        pp = st2 % 2
                        st2 += 1
                        H = slice(h * 512, (h + 1) * 512)

                        def emit2(e, c0=c0, rows=rows, H=H, bank=bank, wb2=wb2):
                            ins = None
                            for j in range(KC):
                                ins = e.matmul(cx.ps[bank][0:rows, :], ACTT[:, j, c0:c0 + rows], wb2[:, j, H],
                                               start=(j == 0), stop=(j == KC - 1))
                            return ins
                        s.op("pe", emit2, reads=akeys + [w2k], writes=["ps%d" % bank])
                        tk = tg + "tmpa%d" % pp
                        s.op("dve", lambda e, H=H, bank=bank, R=R, cond=cond, pp=pp: e.tensor_tensor(
                            out=tmpa[pp][R, :], in0=cx.ps[bank][R, :], in1=g2rep[R, cond, H], op=ALU.mult),
                            reads=["ps%d" % bank, tg + "g2rep"], writes=[tk])
                        s.op("dve", lambda e, H=H, R=R, ti=ti, pp=pp, ex_i=ex_i: e.scalar_tensor_tensor(
                            out=XA[R, ti, H], in0=tmpa[pp][R, :], scalar=gates[R, ti, ex_i:ex_i + 1],
                            in1=XA[R, ti, H], op0=ALU.mult, op1=ALU.add),
                            reads=[tk, tg + "gates%d" % ti, xak], writes=[xak])

        for ti, (row0, rows, cond) in enumerate(tiles):
            xak = tg + "XA%d" % ti
            R = slice(0, rows)
            if final_norm:
                emit_rstd(cx, XA[R, ti, :], rows, junk, ss, rstd, xak, tg)
                s.op("dve", lambda e, R=R, ti=ti: e.scalar_tensor_tensor(
                    out=XA[R, ti, :], in0=XA[R, ti, :], scalar=rstd[R, :], in1=finrep[R, :],
                    op0=ALU.mult, op1=ALU.mult), reads=[xak, tg + "rstd", tg + "finrep"], writes=[xak])
            xdst = dr["xout_tile"](row0, rows) if "xout_tile" in dr else dr["xout"][row0:row0 + rows, :]
            s.dma("sp", xdst, XA[R, ti, :], reads=[xak], writes=[tg + "xout%d" % ti])
        if "h1" in dr:
            h1 = dr["h1"]
            with cx.scope():
                nA1, nm1 = emit_mod1(cx, h1, ncond, tg + "n")
                hxn = [A("hxn", [128, D], F32) for _ in range(2)]
                hTf = [A("hTf", [128, KC, 128], BF16) for _ in range(2)]
                for ti, (row0, rows, cond) in enumerate(tiles):
                    b = ti % 2
                    emit_norm_T(cx, XA[:, ti, :], rows, junk, ss, rstd, hxn[b], tg + "XA%d" % ti, tg + "hxn%d" % b, tg)
                    emit_evac_hT(cx, rows, hTf[b], nA1, nm1, cond, "+" + tg + "hTf%d" % b, tg + "n")
                    s.dma("sp", h1["tile_dst"](row0, rows).rearrange("p (kc t) -> p kc t", kc=KC),
                          hTf[b][:, :, 0:rows], reads=["+" + tg + "hTf%d" % b], writes=[tg + "h1o%d" % ti])


def lay_w1(w1):
    E = w1.shape[0]
    v = w1.reshape(E, 8, 128, 2, 8, 128)
    v = v.transpose(0, 4, 2, 1, 3, 5)
    return np.ascontiguousarray(v).reshape(E, 8, 128, 2048)


def lay_b1(b1):
    E = b1.shape[0]
    return np.ascontiguousarray(b1.reshape(E, 16, 128).transpose(2, 0, 1)).reshape(128, E * 16)


def lay_w2(w2):
    E = w2.shape[0]
    return np.ascontiguousarray(w2.reshape(E, 8, 128, 1024).transpose(0, 2, 1, 3)).reshape(E, 128, 8192)


def lay_cols(v):
    v = np.asarray(v, dtype=np.float32).reshape(-1, 8, 128)
    return np.ascontiguousarray(v.transpose(2, 1, 0))


def dft_consts():
    def cs(n, scale):
        k = np.arange(n)
        ang = 2 * np.pi * np.outer(k, k) / n
        return np.cos(ang) * scale, np.sin(ang) * scale
    cch, sch = cs(256, 1 / 16.0)
    cr, sr = cs(128, 1 / np.sqrt(128.0))
    cc, sc = cs(64, 1 / 8.0)
    c256, s256 = cs(256, 1 / 16.0)
    bdc = np.zeros((128, 128))
    bds = np.zeros((128, 128))
    for i in range(2):
        bdc[i * 64:(i + 1) * 64, i * 64:(i + 1) * 64] = cc
        bds[i * 64:(i + 1) * 64, i * 64:(i + 1) * 64] = -sc
    f = lambda a: np.ascontiguousarray(a, dtype=np.float32)
    return dict(cs_ch=f(np.concatenate([cch, sch], 1)),
                rs1=f(np.concatenate([cr, sr], 1)),
                rs2=f(np.concatenate([-sr, cr], 1)),
                bdc=f(bdc), bds=f(bds),
                c256=f(c256), s256n=f(-s256))


def emit_norm_T(cx, src, rows, junk, ss, rstd, xn, srckey, xnkey, tag, banks=(0, 1)):
    s = cx.s
    R = slice(0, rows)
    emit_rstd(cx, src[R, :], rows, junk, ss, rstd, srckey, tag)
    s.op("dve", lambda e: e.tensor_scalar(out=xn[R, :], in0=src[R, :], scalar1=rstd[R, :], scalar2=None,
                                          op0=ALU.mult), reads=[srckey, tag + "rstd"], writes=[xnkey])
    emit_transpose8(cx, xn, rows, banks[0], banks[1], xnkey)


def emit_evac_hT(cx, rows, hT, A1, B1, cond, hkey, tag, banks=(0, 1)):
    s = cx.s
    for kc in range(KC):
        bank, q = banks[kc // 4], kc % 4
        if kc // 4 == 0:
            s.op("act", lambda e, kc=kc, q=q, bank=bank: e.activation(
                out=hT[:, kc, 0:rows], in_=cx.ps[bank][:, q * 128:q * 128 + rows], func=AF.Identity,
                bias=B1[:, kc, cond:cond + 1], scale=A1[:, kc, cond:cond + 1]),
                reads=["ps%d" % bank, tag + "A1", "+" + tag + "cols"], writes=[hkey])
        else:
            s.op("dve", lambda e, kc=kc, q=q, bank=bank: e.tensor_scalar(
                out=hT[:, kc, 0:rows], in0=cx.ps[bank][:, q * 128:q * 128 + rows],
                scalar1=A1[:, kc, cond:cond + 1], scalar2=B1[:, kc, cond:cond + 1], op0=ALU.mult, op1=ALU.add),
                reads=["ps%d" % bank, tag + "A1", "+" + tag + "cols"], writes=[hkey])


def emit_norm_to_hT(cx, src, rows, hT, A1, B1, cond, junk, ss, rstd, xn, srckey, xnkey, hkey, tag):
    emit_norm_T(cx, src, rows, junk, ss, rstd, xn, srckey, xnkey, tag)
    emit_evac_hT(cx, rows, hT, A1, B1, cond, hkey, tag)


def emit_mod1(cx, dr, ncond, tag):
    s, A = cx.s, cx.A
    m1 = A("m1cols", [128, 16, ncond], F32)
    A1 = A("A1", [128, KC, ncond], F32)
    with cx.scope():
        modb_row = A("modb", [1, 6 * D], F32)
        s.dma("sp", modb_row[:], dr["modb"], writes=[tag + "modb"])
        silu_cols, _ = emit_silu_cond(cx, dr["cond_cols"], ncond, tag, False)
        emit_mod_cols(cx, dr["modw"], modb_row, silu_cols, ncond, list(range(0, 16)), m1, tag)
        n1g = A("n1g", [128, KC], F32)
        s.dma("sp", n1g[:], dr["n1g_col"], writes=[tag + "n1g"])
        for c in range(ncond):
            s.op("dve", lambda e, c=c: e.scalar_tensor_tensor(
                out=A1[:, :, c], in0=m1[:, 8:16, c], scalar=1.0, in1=n1g[:, :], op0=ALU.add, op1=ALU.mult),
                reads=["+" + tag + "cols", tag + "n1g"], writes=[tag + "A1"])
    return A1, m1


def emit_fourier(cx, dr, tag="F"):
    s, A = cx.s, cx.A
    tg = tag
    with cx.scope():
        A1, m1 = emit_mod1(cx, dr, 2, tg)
        WW = A("WW", [128, KC, 512], BF16)
        rs1 = A("rs1", [128, 256], BF16)
        rs2 = A("rs2", [128, 256], BF16)
        bdc = A("bdc", [128, 128], BF16)
        bds = A("bds", [128, 128], BF16)
        c256 = A("c256", [128, 2, 256], BF16)
        s256 = A("s256", [128, 2, 256], BF16)
        s.dma("pool", rs1[:], dr["rs1"], writes=[tg + "rs1"])
        s.dma("pool", rs2[:], dr["rs2"], writes=[tg + "rs2"])
        s.dma("pool", bdc[:], dr["bdc"], writes=[tg + "bdc"])
        s.dma("pool", bds[:], dr["bds"], writes=[tg + "bds"])
        s.dma("pool", c256[:], dr["c256"].rearrange("(k p) n -> p k n", p=128), writes=[tg + "c256"])
        s.dma("pool", s256[:], dr["s256n"].rearrange("(k p) n -> p k n", p=128), writes=[tg + "s256"])
        with cx.scope():
            wT = A("wT", [128, 2, D], F32)
            csch = A("csch", [128, 2, 512], F32)
            s.dma("sp", wT[:], dr["winT"].rearrange("(k p) n -> p k n", p=128), writes=[tg + "wT"])
            s.dma("sp", csch[:], dr["cs_ch"].rearrange("(k p) n -> p k n", p=128), writes=[tg + "csch"])
            for ic in range(KC):
                bank = 4 + ic % 4

                def emit(e, ic=ic, bank=bank):
                    e.matmul(cx.ps[bank][:, :], wT[:, 0, ic * 128:(ic + 1) * 128], csch[:, 0, :], start=True, stop=False)
                    return e.matmul(cx.ps[bank][:, :], wT[:, 1, ic * 128:(ic + 1) * 128], csch[:, 1, :],
                                    start=False, stop=True)
                s.op("pe", emit, reads=[tg + "wT", tg + "csch"], writes=["ps%d" % bank])
                s.op("act", lambda e, ic=ic, bank=bank: e.copy(out=WW[:, ic, :], in_=cx.ps[bank][:, :]),
                     reads=["ps%d" % bank], writes=["+" + tg + "WW"])

        xt = [A("xt", [128, D], F32) for _ in range(2)]
        xn = [A("xn", [128, D], F32) for _ in range(2)]
        hT = [A("hT", [128, KC, 128], BF16) for _ in range(2)]
        junk = A("junk", [128, D], F32)
        ss = A("ss", [128, 1], F32)
        rstd = A("rstd", [128, 1], F32)
        odt = dr.get("out_dt", F32)
        yo = [A("yo", [128, 512], odt) for _ in range(2)]
        lat_chunks = dr["ylat_chunks"]
        gr_per_chunk = 128 // len(lat_chunks)

        with cx.scope():
            PQc = A("PQc", [128, 2, 512], BF16)
            for m in range(2):
                b = m % 2
                s.dma("sp", xt[b][:], dr["ctx"][m * 128:(m + 1) * 128, :], writes=[tg + "xt%d" % b])
                emit_norm_to_hT(cx, xt[b], 128, hT[b], A1, m1, 0, junk, ss, rstd, xn[b],
                                tg + "xt%d" % b, tg + "xn%d" % b, "+" + tg + "hT%d" % b, tg)

                def emit(e, b=b):
                    ins = None
                    for kc in range(KC):
                        ins = e.matmul(cx.ps[2][:, :], hT[b][:, kc, :], WW[:, kc, :], start=(kc == 0),
                                       stop=(kc == KC - 1))
                    return ins
                s.op("pe", emit, reads=["+" + tg + "hT%d" % b, "+" + tg + "WW"], writes=["ps2"])
                s.op("act", lambda e, m=m: e.copy(out=PQc[:, m, :], in_=cx.ps[2][:, :]), reads=["ps2"],
                     writes=["+" + tg + "PQc"])
            for m in range(2):
                def emit(e, m=m):
                    M = slice(m * 128, (m + 1) * 128)
                    e.matmul(cx.ps[3][:, 0:256], c256[:, 0, M], PQc[:, 0, 0:256], start=True, stop=False)
                    e.matmul(cx.ps[3][:, 0:256], c256[:, 1, M], PQc[:, 1, 0:256], start=False, stop=False)
                    e.matmul(cx.ps[3][:, 0:256], s256[:, 0, M], PQc[:, 0, 256:512], start=False, stop=False)
                    return e.matmul(cx.ps[3][:, 0:256], s256[:, 1, M], PQc[:, 1, 256:512], start=False, stop=True)
                s.op("pe", emit, reads=["+" + tg + "PQc", tg + "c256", tg + "s256"], writes=["ps3"])
                s.op("dve", lambda e, m=m: e.tensor_copy(yo[m][:, 0:256], cx.ps[3][:, 0:256]), reads=["ps3"],
                     writes=[tg + "yo%d" % m])
                s.dma("sp", dr["yctx"][m * 128:(m + 1) * 128, :], yo[m][:, 0:256], reads=[tg + "yo%d" % m],
                      writes=["+yctxo"])
            if "after_ctx" in dr:
                dr["after_ctx"]()

        PQ = A("PQ", [128, 2, 128, 2, 64], BF16)
        AB = A("AB", [128, 2, 128, 128], BF16)
        xv = dr["x"].rearrange("(r c) d -> c r d", c=64)
        xt3 = xt + [A("xt", [128, D], F32)]

        def stL(c):
            s.dma("sp", xt3[c % 3][:], xv[c], writes=[tg + "xl%d" % (c % 3)])

        def stN(c):
            banks = (0, 1) if c % 2 == 0 else (4, 5)
            src_, skey, xnk = xt3[c % 3], tg + "xl%d" % (c % 3), tg + "xn%d" % (c % 2)
            emit_rstd(cx, src_[:, :], 128, junk, ss, rstd, skey, tg)
            yield
            s.op("dve", lambda e: e.tensor_scalar(out=xn[c % 2][:, :], in0=src_[:, :], scalar1=rstd[:, :], scalar2=None,
                                                  op0=ALU.mult), reads=[skey, tg + "rstd"], writes=[xnk])
            yield
            emit_transpose8(cx, xn[c % 2], 128, banks[0], banks[1], xnk)
            yield

        def stE(c):
            b = c % 2
            banks = (0, 1) if c % 2 == 0 else (4, 5)
            hkey = "+" + tg + "hT%d" % b
            for kc in range(KC):
                bank, q = banks[kc // 4], kc % 4
                if kc // 4 == 0:
                    s.op("act", lambda e, kc=kc, q=q, bank=bank: e.activation(
                        out=hT[b][:, kc, :], in_=cx.ps[bank][:, q * 128:(q + 1) * 128], func=AF.Identity,
                        bias=m1[:, kc, 1:2], scale=A1[:, kc, 1:2]),
                        reads=["ps%d" % bank, tg + "A1", "+" + tg + "cols"], writes=[hkey])
                else:
                    s.op("dve", lambda e, kc=kc, q=q, bank=bank: e.tensor_scalar(
                        out=hT[b][:, kc, :], in0=cx.ps[bank][:, q * 128:(q + 1) * 128],
                        scalar1=A1[:, kc, 1:2], scalar2=m1[:, kc, 1:2], op0=ALU.mult, op1=ALU.add),
                        reads=["ps%d" % bank, tg + "A1", "+" + tg + "cols"], writes=[hkey])
                if kc % 2 == 1:
                    yield
            bank = 2 + c % 2

            def emit(e, b=b, bank=bank):
                ins = None
                for kc in range(KC):
                    ins = e.matmul(cx.ps[bank][:, :], hT[b][:, kc, :], WW[:, kc, :], start=(kc == 0),
                                   stop=(kc == KC - 1))
                return ins
            s.op("pe", emit, reads=[hkey, "+" + tg + "WW"], writes=["ps%d" % bank])
            yield
            for pq in range(2):
                src2 = cx.ps[bank][:, pq * 256:(pq + 1) * 256].rearrange("p (l h) -> p l h", l=2)
                dst = PQ[:, pq, :, :, c].rearrange("p h l -> p l h")
                if c % 2 == 0:
                    s.op("act", lambda e, src2=src2, dst=dst: e.copy(out=dst, in_=src2), reads=["ps%d" % bank],
                         writes=["+" + tg + "PQ"])
                else:
                    s.op("dve", lambda e, src2=src2, dst=dst: e.tensor_copy(dst, src2), reads=["ps%d" % bank],
                         writes=["+" + tg + "PQ"])
                yield

        def run2(gens):
            alive = [g for g in gens if g is not None]
            while alive:
                for g in list(alive):
                    try:
                        next(g)
                    except StopIteration:
                        alive.remove(g)

        stL(0)
        stL(1)
        run2([stN(0)])
        for c in range(64):
            if c + 2 < 64:
                stL(c + 2)
            run2([stN(c + 1) if c + 1 < 64 else None, stE(c)])
        for hh in range(128):
            bank = 4 + hh % 4

            def emit(e, hh=hh, bank=bank):
                e.matmul(cx.ps[bank][:, 0:256], PQ[:, 0, hh, :, :].rearrange("p l c -> p (l c)"), rs1[:, :],
                         start=True, stop=False)
                return e.matmul(cx.ps[bank][:, 0:256], PQ[:, 1, hh, :, :].rearrange("p l c -> p (l c)"), rs2[:, :],
                                start=False, stop=True)
            s.op("pe", emit, reads=["+" + tg + "PQ", tg + "rs1", tg + "rs2"], writes=["ps%d" % bank])
            src = cx.ps[bank][:, 0:256].rearrange("p (a r) -> p a r", a=2)
            dst = AB[:, :, :, hh]
            if hh % 2 == 0:
                s.op("act", lambda e, src=src, dst=dst: e.copy(out=dst, in_=src), reads=["ps%d" % bank],
                     writes=["+" + tg + "AB"])
            else:
                s.op("dve", lambda e, src=src, dst=dst: e.tensor_copy(dst, src), reads=["ps%d" % bank],
                     writes=["+" + tg + "AB"])
        yvs = [ch.rearrange("(r c) n -> c r n", c=64) for ch in lat_chunks]
        for r4 in range(32):
            yv = yvs[(r4 * 4) // gr_per_chunk]
            rr0 = (r4 * 4) % gr_per_chunk
            bank = r4 % 2
            b = r4 % 2

            def emit(e, r4=r4, bank=bank):
                e.matmul(cx.ps[bank][:, :], bdc[:, :], AB[:, 0, r4 * 4:(r4 + 1) * 4, :].rearrange("p r h -> p (r h)"),
                         start=True, stop=False)
                return e.matmul(cx.ps[bank][:, :], bds[:, :],
                                AB[:, 1, r4 * 4:(r4 + 1) * 4, :].rearrange("p r h -> p (r h)"), start=False, stop=True)
            s.op("pe", emit, reads=["+" + tg + "AB", tg + "bdc", tg + "bds"], writes=["ps%d" % bank])
            if r4 % 2 == 0:
                s.op("act", lambda e, b=b, bank=bank: e.copy(out=yo[b][:, :], in_=cx.ps[bank][:, :]),
                     reads=["ps%d" % bank], writes=[tg + "yo%d" % b])
            else:
                s.op("dve", lambda e, b=b, bank=bank: e.tensor_copy(yo[b][:, :], cx.ps[bank][:, :]),
                     reads=["ps%d" % bank], writes=[tg + "yo%d" % b])
            for lo in range(2):
                s.dma("sp", yv[:, rr0:rr0 + 4, lo * 128:(lo + 1) * 128],
                      yo[b][lo * 64:(lo + 1) * 64, :].rearrange("p (r h) -> p r h", r=4),
                      reads=[tg + "yo%d" % b], writes=["+ylatc%d" % ((r4 * 4) // gr_per_chunk)])
            if "after_out" in dr and (r4 * 4 + 4) % gr_per_chunk == 0:
                dr["after_out"]((r4 * 4) // gr_per_chunk)


def hgrn_consts():
    t = np.arange(128)
    same = (t[:, None] // 64) == (t[None, :] // 64)
    tri_fw = (same & (t[:, None] <= t[None, :])).astype(np.float32)
    tri_bw = (same & (t[:, None] >= t[None, :])).astype(np.float32)
    su_fw = (same & (t[:, None] > t[None, :])).astype(np.float32)
    su_bw = (same & (t[:, None] < t[None, :])).astype(np.float32)
    return dict(tri_fw=tri_fw, tri_bw=tri_bw, su_fw=su_fw, su_bw=su_bw)


def emit_hgrn(cx, dr, tag="H", n_lat_tiles=64, n_ctx_tiles=2):
    s, A, nc = cx.s, cx.A, cx.nc
    tg = tag
    NL = n_lat_tiles
    pass
    with cx.scope():
        if "h_tile" not in dr:
            A1, m1 = emit_mod1(cx, dr, 2, tg)
        W = A("W5", [128, KC, 1280], BF16)
        s.dma("pool", W[:], dr["win5"].rearrange("(kc p) n -> p kc n", p=128), writes=[tg + "W"])
        tri = [A("tri", [128, 128], F32) for _ in range(2)]
        su = [A("su", [128, 128], F32) for _ in range(2)]
        for d, nm in enumerate(("fw", "bw")):
            s.dma("sp", tri[d][:], dr["tri_" + nm], writes=[tg + "tri%d" % d])
            s.dma("sp", su[d][:], dr["su_" + nm], writes=[tg + "su%d" % d])
        hng = A("hng", [128, 256], F32)
        s.dma("sp", hng[:], dr["hng_rep"], writes=[tg + "hng"])
        lb = A("lb", [128, 2, 256], F32)
        oml = A("oml", [128, 2, 256], F32)
        with cx.scope():
            lbr = A("lbr", [128, 2, 2, 256], F32)
            s.dma("sp", lbr[:], dr["lbrep"], writes=[tg + "lbr"])
            s.op("dve", lambda e: e.tensor_tensor(out=lb[:], in0=lbr[:, 1], in1=lbr[:, 0], op=ALU.subtract),
                 reads=[tg + "lbr"], writes=[tg + "lb"])
            s.op("act", lambda e: e.activation(out=lb[:], in_=lb[:], func=AF.Exp, scale=-1.0),
                 reads=[tg + "lb"], writes=[tg + "lb"])
            s.op("dve", lambda e: e.tensor_scalar(out=lb[:], in0=lb[:], scalar1=1.0, scalar2=None, op0=ALU.add),
                 reads=[tg + "lb"], writes=[tg + "lb"])
            s.op("dve", lambda e: e.reciprocal(out=lb[:], in_=lb[:]), reads=[tg + "lb"], writes=[tg + "lb"])
            s.op("dve", lambda e: e.tensor_scalar(out=oml[:], in0=lb[:], scalar1=-1.0, scalar2=1.0, op0=ALU.mult,
                                                  op1=ALU.add), reads=[tg + "lb"], writes=[tg + "oml"])

        OFW = A("OFW", [128, max(NL, 1), 256], F32)
        xt = [A("xt", [128, D], F32) for _ in range(2)]
        xn = [A("xn", [128, D], F32) for _ in range(2)]
        hT = [A("hT", [128, KC, 128], BF16) for _ in range(2)]
        junk = A("junk", [128, D], F32)
        ss = A("ss", [128, 1], F32)
        rstd = A("rstd", [128, 1], F32)
        NB = 2
        EA = [A("EA", [128, 768], F32) for _ in range(NB)]
        fg = [A("fg", [128, 256], F32) for _ in range(NB)]
        logf = [A("logf", [128, 256], F32) for _ in range(NB)]
        kf = [A("kf", [128, 256], F32) for _ in range(NB)]
        kh = [A("kh", [128, 256], BF16) for _ in range(NB)]
        kh2 = [A("kh2", [128, 256], BF16) for _ in range(NB)]
        rmask = A("rmask", [128, 2], F32)
        s.op("dve", lambda e: e.memset(rmask[:], 0.0), writes=[tg + "rmask"])
        s.op("dve", lambda e: e.memset(rmask[0:64, 0:1], 1.0), writes=[tg + "rmask"])
        s.op("dve", lambda e: e.memset(rmask[64:128, 1:2], 1.0), writes=[tg + "rmask"])
        vv = [A("vv", [128, 256], BF16) for _ in range(NB)]
        qs = [A("qs", [128, 256], F32) for _ in range(NB)]
        gg = [A("gg", [128, 256], F32) for _ in range(NB)]
        ec = [A("ec", [128, 256], F32) for _ in range(NB)]
        ebT = [[A("ebT", [128, 128], F32) for _ in range(NB)] for _ in range(2)]
        enbT = [[A("enbT", [128, 128], F32) for _ in range(NB)] for _ in range(2)]
        Z = [[A("Z", [128, 256], BF16) for _ in range(NB)] for _ in range(2)]
        ktT = [[A("ktT", [128, 128], BF16) for _ in range(NB)] for _ in range(2)]
        scm = [[A("scm", [128, 128], BF16) for _ in range(NB)] for _ in range(2)]
        S = [[A("S", [128, 128], F32) for _ in range(3)] for _ in range(2)]
        Sb = [[A("Sb", [128, 128], BF16) for _ in range(4)] for _ in range(2)]
        obuf = [A("obuf", [128, 256], F32) for _ in range(NB)]
        yout = [A("yout", [128, 256], dr.get("out_dt", F32)) for _ in range(NB)]
        ssq = A("ssq", [128, 2], F32)
        rsq = A("rsq", [128, 2], F32)
        junkB = A("junkB", [128, 256], F32)
        for hd in range(2):
            for b in range(NB):
                s.op("pool", lambda e, hd=hd, b=b: e.memset(Z[hd][b][:], 0.0), writes=[tg + "Z%d_%d" % (hd, b)])

        steps = []
        for d in range(2):
            if d == 0:
                order = [("ctx", i) for i in range(n_ctx_tiles)] + [("lat", i) for i in range(NL)]
            else:
                order = [("ctx", i) for i in reversed(range(n_ctx_tiles))] + [("lat", i) for i in reversed(range(NL))]
            for idx, (kind, i) in enumerate(order):
                steps.append((d, kind, i, idx == 0))
        scur = [0, 0]
        sbc = [0, 0]

        xt4 = xt + [A("xt", [128, D], F32) for _ in range(2)]
        hT4 = [A("hT4", [128, KC, 128], BF16) for _ in range(4)]
        hscr_t = nc.dram_tensor(cx.name("hscr"), [n_ctx_tiles + max(NL, 1), 128, KC * 128], BF16)
        hscr = [hscr_t.ap()[j_] for j_ in range(n_ctx_tiles + max(NL, 1))]

        def stageL(n):
            d, kind, i, first_of_sweep = steps[n]
            if "h_tile" in dr:
                for (dst_cols, hsrc_ap) in dr["h_tile"](kind, i):
                    s.dma("sp", hT4[n % 4][:, :, dst_cols], hsrc_ap.rearrange("p (kc t) -> p kc t", kc=KC),
                          writes=["+" + tg + "hl%d" % (n % 4)])
                return
            if d == 1:
                slot = i if kind == "ctx" else n_ctx_tiles + i
                s.dma("sp", hT4[n % 4][:], hscr[slot].rearrange("p (kc t) -> p kc t", kc=KC),
                      reads=["hscr%d" % slot], writes=["+" + tg + "hl%d" % (n % 4)])
                return
            if "x_tile" in dr:
                src = dr["x_tile"](kind, i)
            else:
                src = dr["x"][i * 128:(i + 1) * 128, :] if kind == "lat" else dr["xctx"][i * 128:(i + 1) * 128, :]
            s.dma("sp", xt4[n % 4][:], src, writes=[tg + "xl%d" % (n % 4)])

        def stageA(n):
            d, kind, i, first_of_sweep = steps[n]
            lat = kind == "lat"
            b = n % 2
            k = lambda nm: tg + nm + "%d" % b
            if d == 0 and "h_tile" not in dr:
                emit_norm_T(cx, xt4[n % 4], 128, junk, ss, rstd, xn[b], tg + "xl%d" % (n % 4), k("xn"), tg)
                yield
                emit_evac_hT(cx, 128, hT[b], A1, m1, 1 if lat else 0, "+" + k("hT"), tg)
                slot = i if kind == "ctx" else n_ctx_tiles + i
                s.dma("sp", hscr[slot].rearrange("p (kc t) -> p kc t", kc=KC), hT[b][:], reads=["+" + k("hT")],
                      writes=["hscr%d" % slot])
                hsrc, hkey_ = hT[b], "+" + k("hT")
            else:
                hsrc, hkey_ = hT4[n % 4], "+" + tg + "hl%d" % (n % 4)
            c0 = 256 if d == 0 else 512

            def emit_p(e, hsrc=hsrc, c0=c0):
                ins = None
                for kc in range(KC):
                    ins = e.matmul(cx.ps[2][:, :], hsrc[:, kc, :], W[:, kc, c0:c0 + 512], start=(kc == 0),
                                   stop=(kc == KC - 1))
                return ins
            s.op("pe", emit_p, reads=[hkey_, tg + "W"], writes=["ps2"])
            yield
            if lat:
                def emit_q(e, hsrc=hsrc, d=d):
                    ins = None
                    if d == 1:
                        for kc in range(KC):
                            ins = e.matmul(cx.ps[3][:, 0:256], hsrc[:, kc, :], W[:, kc, 1024:1280],
                                           start=(kc == 0), stop=(kc == KC - 1))
                    for hd in range(2):
                        for kc in range(KC):
                            ins = e.matmul(cx.ps[3][:, 256 + hd * 128:384 + hd * 128],
                                           W[:, kc, hd * 128:(hd + 1) * 128], hsrc[:, kc, :],
                                           start=(kc == 0), stop=(kc == KC - 1))
                    return ins
                s.op("pe", emit_q, reads=[hkey_, tg + "W"], writes=["ps3"])
                yield
            fsl = cx.ps[2][:, 0:256] if d == 0 else cx.ps[2][:, 256:512]
            isl = cx.ps[2][:, 256:512] if d == 0 else cx.ps[2][:, 0:256]
            yield
            EAb = EA[b]
            s.op("act", lambda e, EAb=EAb, fsl=fsl: e.activation(out=EAb[:, 0:256], in_=fsl, func=AF.Exp, scale=-1.0),
                 reads=["ps2"], writes=[k("EA")])
            yield
            s.op("act", lambda e, b=b, isl=isl: e.copy(out=vv[b][:], in_=isl), reads=["ps2"], writes=[k("vv")])
            yield
            if lat and d == 1:
                s.op("act", lambda e, EAb=EAb: e.activation(out=EAb[:, 256:768], in_=cx.ps[3][:, 0:512], func=AF.Exp,
                                                            scale=-1.0), reads=["ps3", k("EA")], writes=[k("EA")])
                yield
                reg = EAb[:, 0:768]
            elif lat:
                s.op("act", lambda e, EAb=EAb: e.activation(out=EAb[:, 512:768], in_=cx.ps[3][:, 256:512], func=AF.Exp,
                                                            scale=-1.0), reads=["ps3", k("EA")], writes=[k("EA")])
                yield
                reg = EAb[:, :].rearrange("p (a x) -> p a x", a=3)[:, 0:3:2, :]
            else:
                reg = EAb[:, 0:256]
            s.op("act", lambda e, reg=reg: e.activation(out=reg, in_=reg, func=AF.Ln, bias=cx.one_col[:, :], scale=1.0),
                 reads=[k("EA"), "one_col"], writes=[k("EA")])
            yield
            s.op("act", lambda e, reg=reg: e.activation(out=reg, in_=reg, func=AF.Exp, scale=-1.0),
                 reads=[k("EA")], writes=[k("EA")])
            yield
            s.op("dve", lambda e, b=b, d=d, EAb=EAb: e.tensor_tensor(out=fg[b][:], in0=EAb[:, 0:256], in1=oml[:, d, :],
                                                                     op=ALU.mult),
                 reads=[k("EA"), tg + "oml"], writes=[k("fg")])
            yield
            s.op("dve", lambda e, b=b, d=d: e.tensor_tensor(out=fg[b][:], in0=fg[b][:], in1=lb[:, d, :],
                                                            op=ALU.add),
                 reads=[k("fg"), tg + "lb"], writes=[k("fg")])
            yield
            s.op("act", lambda e, b=b: e.activation(out=logf[b][:], in_=fg[b][:], func=AF.Ln),
                 reads=[k("fg")], writes=[k("logf")])
            yield
            s.op("pool", lambda e, b=b: e.tensor_scalar(out=kf[b][:], in0=fg[b][:], scalar1=-1.0, scalar2=1.0,
                                                        op0=ALU.mult, op1=ALU.add),
                 reads=[k("fg")], writes=[k("kf")])
            yield
            yield
            if lat:
                qsl = cx.ps[3][:, 256:512]
                s.op("dve", lambda e, b=b, qsl=qsl, EAb=EAb: e.tensor_tensor(out=qs[b][:], in0=EAb[:, 512:768], in1=qsl,
                                                                             op=ALU.mult),
                     reads=[k("EA"), "ps3"], writes=[k("qs")])
                yield
                if d == 1:
                    gsl = cx.ps[3][:, 0:256]
                    s.op("dve", lambda e, b=b, gsl=gsl, EAb=EAb: e.tensor_tensor(out=gg[b][:], in0=EAb[:, 256:512],
                                                                                 in1=gsl, op=ALU.mult),
                         reads=[k("EA"), "ps3"], writes=[k("gg")])
                    yield
                    s.op("pool", lambda e, b=b: e.tensor_tensor(out=gg[b][:], in0=gg[b][:], in1=hng[:],
                                                                op=ALU.mult),
                         reads=[k("gg"), tg + "hng"], writes=[k("gg")])
                    yield

        def stageB(n):
            d, kind, i, first_of_sweep = steps[n]
            lat = kind == "lat"
            b = n % 2
            k = lambda nm: tg + nm + "%d" % b
            if first_of_sweep:
                scur[0] = scur[1] = 0
                sbc[0] = sbc[1] = 0
                for hd in range(2):
                    s.op("dve", lambda e, hd=hd: e.memset(S[hd][0][:], 0.0), writes=[tg + "S%d_0" % hd])
                    yield
            s.op("pe", lambda e, b=b, d=d: e.matmul(cx.ps[6][:, 0:256], su[d][:, :], logf[b][:, :],
                                                    start=True, stop=True),
                 reads=[k("logf"), tg + "su%d" % d], writes=["ps6"])
            yield
            s.op("act", lambda e, b=b: e.activation(out=ec[b][:], in_=cx.ps[6][:, 0:256], func=AF.Exp),
                 reads=["ps6"], writes=[k("ec")])
            yield
            s.op("dve", lambda e, b=b: e.scalar_tensor_tensor(
                out=kh[b][:], in0=kf[b][:], scalar=rmask[:, 0:1], in1=ec[b][:], op0=ALU.mult, op1=ALU.mult),
                reads=[k("kf"), k("ec"), tg + "rmask"], writes=["+" + k("kh")])
            yield
            s.op("dve", lambda e, b=b: e.scalar_tensor_tensor(
                out=kh2[b][:], in0=kf[b][:], scalar=rmask[:, 1:2], in1=ec[b][:], op0=ALU.mult, op1=ALU.mult),
                reads=[k("kf"), k("ec"), tg + "rmask"], writes=["+" + k("kh")])
            yield
            first, second = (0, 1) if d == 0 else (1, 0)
            yield

            def head_gen(hd):
                H = slice(hd * 128, (hd + 1) * 128)
                kb = lambda nm: tg + nm + "%d_%d" % (hd, b)
                hb = 4 + hd
                hbk = "ps%d" % hb
                BT, KT, SC, OO = slice(0, 128), slice(128, 256), slice(256, 384), slice(384, 512)

                def emit_bt(e, b=b, d=d, H=H, hb=hb, lat=lat):
                    ins = e.matmul(cx.ps[hb][:, BT], logf[b][:, H], tri[d][:, :], start=True, stop=True)
                    if lat:
                        ins = e.transpose(out=cx.ps[hb][:, KT], in_=kf[b][:, H], identity=cx.ident[:, :])
                    return ins
                s.op("pe", emit_bt, reads=[k("logf"), tg + "tri%d" % d, k("kf"), "ident"], writes=[hbk])
                yield
                s.op("act", lambda e, b=b, hd=hd, hb=hb: e.activation(out=ebT[hd][b][:], in_=cx.ps[hb][:, BT],
                                                                      func=AF.Exp),
                     reads=[hbk], writes=[kb("ebT")])
                yield
                if lat:
                    s.op("dve", lambda e, b=b, hd=hd, hb=hb: e.tensor_scalar(
                        out=enbT[hd][b][:], in0=cx.ps[hb][:, BT], scalar1=-87.0, scalar2=None, op0=ALU.max),
                        reads=[hbk], writes=[kb("enbT")])
                    yield
                    s.op("act", lambda e, b=b, hd=hd: e.activation(
                        out=enbT[hd][b][:], in_=enbT[hd][b][:], func=AF.Exp, scale=-1.0),
                        reads=[kb("enbT")], writes=[kb("enbT")])
                    yield
                    Zv = Z[hd][b][:, :].rearrange("p (a x) -> p a x", a=4)[:, 0:4:3, :]
                    s.op("dve", lambda e, b=b, hd=hd, H=H, Zv=Zv: e.tensor_tensor(
                        out=Zv, in0=qs[b][:, H].rearrange("p (a x) -> p a x", a=2),
                        in1=ebT[hd][b][:, :].rearrange("p (a x) -> p a x", a=2), op=ALU.mult),
                        reads=[k("qs"), kb("ebT")], writes=[kb("Z")])
                    yield
                    s.op("dve", lambda e, b=b, hd=hd, hb=hb: e.tensor_tensor(
                        out=ktT[hd][b][:], in0=cx.ps[hb][:, KT], in1=enbT[hd][b][:], op=ALU.mult),
                        reads=[hbk, kb("enbT")], writes=[kb("ktT")])
                    yield
                    s.op("pe", lambda e, b=b, hd=hd, hb=hb, Zv=Zv: e.matmul(
                        cx.ps[hb][:, SC].rearrange("p (a x) -> p a x", a=2), ktT[hd][b][:, :], Zv,
                        start=True, stop=True),
                        reads=[kb("ktT"), kb("Z")], writes=[hbk])
                    yield
                    s.op("dve", lambda e, b=b, hd=hd, hb=hb, d=d: e.tensor_tensor(
                        out=scm[hd][b][:], in0=cx.ps[hb][:, SC], in1=tri[d][:, :], op=ALU.mult),
                        reads=[hbk, tg + "tri%d" % d], writes=[kb("scm")])
                    yield
                yield
                kvb = 6 if hd == 0 else 7
                kvk = "ps%d" % kvb
                o0 = 256 if hd == 0 else 0
                kv1 = slice(o0, o0 + 128)
                kv2 = slice(o0 + 128, o0 + 256)
                P1 = slice(first * 64, first * 64 + 64)
                P2 = slice(second * 64, second * 64 + 64)

                khf = kh if first == 0 else kh2
                khs = kh2 if first == 0 else kh

                def emit_kv(e, b=b, H=H, kv1=kv1, kv2=kv2, khf=khf, khs=khs, kvb=kvb):
                    e.matmul(cx.ps[kvb][:, kv1], khf[b][:, H], vv[b][:, H], start=True, stop=True)
                    return e.matmul(cx.ps[kvb][:, kv2], khs[b][:, H], vv[b][:, H], start=True, stop=True)
                s.op("pe", emit_kv, reads=["+" + k("kh"), k("vv")], writes=[kvk])
                yield
                if d == 0:
                    c1, c2 = 63, 127
                else:
                    c1, c2 = 64, 0
                si, sm, so = scur[hd] % 3, (scur[hd] + 1) % 3, (scur[hd] + 2) % 3
                scur[hd] += 2
                kS = lambda j: tg + "S%d_%d" % (hd, j)
                if lat:
                    bi, bm = sbc[hd] % 4, (sbc[hd] + 1) % 4
                    sbc[hd] += 2
                    kSb = lambda j: tg + "Sb%d_%d" % (hd, j)
                    s.op("pool", lambda e, hd=hd, si=si, bi=bi: e.tensor_copy(Sb[hd][bi][:], S[hd][si][:]),
                         reads=[kS(si)], writes=[kSb(bi)])
                    yield
                s.op("dve", lambda e, hd=hd, b=b, si=si, sm=sm, c1=c1, kv1=kv1, kvb=kvb: e.scalar_tensor_tensor(
                    out=S[hd][sm][:], in0=S[hd][si][:], scalar=ebT[hd][b][:, c1:c1 + 1], in1=cx.ps[kvb][:, kv1],
                    op0=ALU.mult, op1=ALU.add),
                    reads=[kS(si), kb("ebT"), kvk], writes=[kS(sm)])
                yield
                if lat:
                    s.op("pool", lambda e, hd=hd, sm=sm, bm=bm: e.tensor_copy(Sb[hd][bm][:], S[hd][sm][:]),
                         reads=[kS(sm)], writes=[kSb(bm)])
                    yield
                s.op("dve", lambda e, hd=hd, b=b, sm=sm, so=so, c2=c2, kv2=kv2, kvb=kvb: e.scalar_tensor_tensor(
                    out=S[hd][so][:], in0=S[hd][sm][:], scalar=ebT[hd][b][:, c2:c2 + 1], in1=cx.ps[kvb][:, kv2],
                    op0=ALU.mult, op1=ALU.add),
                    reads=[kS(sm), kb("ebT"), kvk], writes=[kS(so)])
                yield
                if lat:
                    Zf = Z[hd][b][:, first * 128:(first + 1) * 128]
                    Zs = Z[hd][b][:, second * 128:(second + 1) * 128]

                    def emit_o(e, b=b, hd=hd, H=H, Zf=Zf, Zs=Zs, bi=bi, bm=bm, hb=hb):
                        e.matmul(cx.ps[hb][:, OO], scm[hd][b][:, :], vv[b][:, H], start=True, stop=False)
                        e.matmul(cx.ps[hb][:, OO], Zf, Sb[hd][bi][:, :], start=False, stop=False)
                        return e.matmul(cx.ps[hb][:, OO], Zs, Sb[hd][bm][:, :], start=False, stop=True)
                    s.op("pe", emit_o, reads=[kb("scm"), k("vv"), kb("Z"), kSb(bi), kSb(bm)], writes=[hbk])
                    yield
                    if d == 0:
                        s.op("act", lambda e, i=i, H=H, hb=hb: e.copy(out=OFW[:, i, H], in_=cx.ps[hb][:, OO]),
                             reads=[hbk], writes=[tg + "OFW%d_%d" % (i, hd)])
                        yield
                    else:
                        s.op("dve", lambda e, b=b, i=i, H=H, hb=hb: e.tensor_tensor(
                            out=obuf[b][:, H], in0=cx.ps[hb][:, OO], in1=OFW[:, i, H], op=ALU.add),
                            reads=[hbk, tg + "OFW%d_%d" % (i, hd)], writes=["+" + k("obuf")])
                        yield
                        s.op("act", lambda e, b=b, hd=hd, H=H: e.activation(
                            out=junk[:, H], in_=obuf[b][:, H], func=AF.Square, accum_out=ssq[:, hd:hd + 1]),
                            reads=["+" + k("obuf")], writes=[tg + "junk", "+" + tg + "ssq"])
                        yield

            yield from interleave_gen([head_gen(0), head_gen(1)])

            if lat and d == 1:
                s.op("act", lambda e: e.activation(out=rsq[:], in_=ssq[:], func=AF.Ln, bias=cx.eps_col[:, :],
                                                   scale=1.0 / 128),
                     reads=["+" + tg + "ssq", "eps_col"], writes=[tg + "rsq"])
                s.op("act", lambda e: e.activation(out=rsq[:], in_=rsq[:], func=AF.Exp, scale=-0.5),
                     reads=[tg + "rsq"], writes=[tg + "rsq"])
                for hd in range(2):
                    H = slice(hd * 128, (hd + 1) * 128)
                    s.op("dve", lambda e, b=b, hd=hd, H=H: e.scalar_tensor_tensor(
                        out=yout[b][:, H], in0=obuf[b][:, H], scalar=rsq[:, hd:hd + 1], in1=gg[b][:, H],
                        op0=ALU.mult, op1=ALU.mult),
                        reads=["+" + k("obuf"), tg + "rsq", k("gg")], writes=["+" + k("yout")])
                ydst = dr["ypre_tile"](i) if "ypre_tile" in dr else dr["ypre"][i * 128:(i + 1) * 128, :]
                ykey = dr["ypre_key"](i) if "ypre_key" in dr else tg + "ypre%d" % i
                s.dma("sp", ydst, yout[b][:], reads=["+" + k("yout")], writes=[ykey])
                if "after_out" in dr:
                    dr["after_out"](i)

        def interleave_gen(gens):
            alive = list(gens)
            while alive:
                for g in list(alive):
                    try:
                        next(g)
                    except StopIteration:
                        alive.remove(g)
                yield

        def run_interleaved(gens):
            alive = [g for g in gens if g is not None]
            while alive:
                for g in list(alive):
                    try:
                        next(g)
                    except StopIteration:
                        alive.remove(g)

        for n in range(min(3, len(steps))):
            stageL(n)
        run_interleaved([stageA(0)])
        for n in range(len(steps)):
            run_interleaved([stageA(n + 1) if n + 1 < len(steps) else None, stageB(n)])
            if n + 3 < len(steps):
                stageL(n + 3)


def lay_win5(w_in, hp):
    sec = lambda j: w_in[:, j * D + hp * 256: j * D + (hp + 1) * 256]
    return np.ascontiguousarray(np.concatenate([sec(0), sec(1), sec(3), sec(2), sec(4)], axis=1))


def _dt(nc, name, shape, kind="ExternalInput", dtype=F32):
    return nc.dram_tensor(name, list(shape), dtype, kind=kind).ap()


def build_fourier_prog():
    nc = bass.Bass("TRN2", target_bir_lowering=False)
    dr = dict(x=_dt(nc, "x", [8192, D]), ctx=_dt(nc, "ctx", [256, D]),
              yctx=_dt(nc, "yctx", [256, 256], "ExternalOutput"),
              modw=_dt(nc, "modw", [D, 6 * D]), modb=_dt(nc, "modb", [1, 6 * D]),
              cond_cols=_dt(nc, "cond_cols", [128, 8, 2]), n1g_col=_dt(nc, "n1g_col", [128, 8]),
              winT=_dt(nc, "winT", [256, D]), cs_ch=_dt(nc, "cs_ch", [256, 512]), rs1=_dt(nc, "rs1", [128, 256]),
              rs2=_dt(nc, "rs2", [128, 256]), bdc=_dt(nc, "bdc", [128, 128]), bds=_dt(nc, "bds", [128, 128]),
              c256=_dt(nc, "c256", [256, 256]), s256n=_dt(nc, "s256n", [256, 256]))
    dr["ylat_chunks"] = [_dt(nc, "ylat", [8192, 256], "ExternalOutput")]
    ident = _dt(nc, "ident", [128, 128])
    cx = Ctx(nc)
    load_consts(cx, ident)
    emit_fourier(cx, dr)
    cx.s.barrier(["sp"])
    return nc


def build_hgrn_prog():
    nc = bass.Bass("TRN2", target_bir_lowering=False)
    dr = dict(x=_dt(nc, "x", [8192, D]), xctx=_dt(nc, "xctx", [256, D]),
              ypre=_dt(nc, "ypre", [8192, 256], "ExternalOutput"),
              modw=_dt(nc, "modw", [D, 6 * D]), modb=_dt(nc, "modb", [1, 6 * D]),
              cond_cols=_dt(nc, "cond_cols", [128, 8, 2]), n1g_col=_dt(nc, "n1g_col", [128, 8]),
              win5=_dt(nc, "win5", [D, 1280]), lbrep=_dt(nc, "lbrep", [128, 2, 2, 256]),
              hng_rep=_dt(nc, "hng_rep", [128, 256]), tri_fw=_dt(nc, "tri_fw", [128, 128]),
              tri_bw=_dt(nc, "tri_bw", [128, 128]), su_fw=_dt(nc, "su_fw", [128, 128]),
              su_bw=_dt(nc, "su_bw", [128, 128]))
    ident = _dt(nc, "ident", [128, 128])
    cx = Ctx(nc)
    load_consts(cx, ident)
    emit_hgrn(cx, dr)
    cx.s.barrier(["sp"])
    return nc


def post_tiles(with_ctx):
    p0 = [(i * 128, 128, 1) for i in range(0, 8)]
    p1 = [(i * 128, 128, 1) for i in range(8, 16)]
    if with_ctx:
        p0 = p0 + [(2048, 64, 0)]
    return [p0, p1]


def build_post_prog(with_ctx, final_norm):
    nc = bass.Bass("TRN2", target_bir_lowering=False)
    T = 2112 if with_ctx else 2048
    dr = dict(xres=_dt(nc, "xres", [T, D]), ypre=_dt(nc, "ypre", [T, D]), xout=_dt(nc, "xout", [T, D], "ExternalOutput"),
              wout=_dt(nc, "wout", [D, D]), modw=_dt(nc, "modw", [D, 6 * D]), modb=_dt(nc, "modb", [1, 6 * D]),
              cond_cols=_dt(nc, "cond_cols", [128, 8, 2]), n2g_col=_dt(nc, "n2g_col", [128, 8]),
              wr=_dt(nc, "wr", [D, NE]), br=_dt(nc, "br", [1, NE]), w1r=_dt(nc, "w1r", [NE, 8, 128, 2048]),
              b1c=_dt(nc, "b1c", [128, NE * 16]), w2r=_dt(nc, "w2r", [NE, 128, 8192]), b2=_dt(nc, "b2", [NE, D]),
              fin_rep=_dt(nc, "fin_rep", [128, D]))
    ident = _dt(nc, "ident", [128, 128])
    cx = Ctx(nc)
    load_consts(cx, ident)
    for pi, tiles in enumerate(post_tiles(with_ctx)):
        emit_post(cx, tiles, 2, dr, final_norm, tagp="P%d" % pi)
    cx.s.barrier(["sp"])
    return nc


def _run(nc, in_maps):
    res = run_bass_kernel_spmd(nc, in_maps, core_ids=list(range(NCORES)))
    return res.results


_DEBUG = None


def kernel_unfused(x, c, ctx, c_ctx, mod_w, mod_b, norm1_g, norm2_g, fourier_w_in, fourier_w_out,
           hgrn_w_in, hgrn_lower_bounds, hgrn_norm_g, hgrn_w_out, router_w, router_b,
           expert_w1, expert_b1, expert_w2, expert_b2, final_norm_g):
    dbg = _DEBUG
    f32 = lambda a: np.ascontiguousarray(np.asarray(a, dtype=np.float32))
    x, c, ctx, c_ctx = f32(x), f32(c), f32(ctx), f32(c_ctx)
    mod_w, mod_b = f32(mod_w), f32(mod_b)
    norm1_g, norm2_g = f32(norm1_g), f32(norm2_g)
    ident = np.eye(128, dtype=np.float32)
    B = x.shape[0]
    cond_cols = [lay_cols(np.stack([c_ctx, c[b]])) for b in range(B)]

    fw_in = f32(fourier_w_in)[0]
    consts = dft_consts()
    ims = []
    for j in range(NCORES):
        b, g = j // 4, j % 4
        m = dict(x=x[b], ctx=ctx[b], modw=mod_w[0], modb=mod_b[0][None, :], cond_cols=cond_cols[b],
                 n1g_col=lay_cols(norm1_g[0])[:, :, 0], winT=np.ascontiguousarray(fw_in[:, g * 256:(g + 1) * 256].T),
                 ident=ident)
        m.update(consts)
        ims.append(m)
    r = _run(build_fourier_prog(), ims)
    y_lat = np.empty((B, 8192, D), np.float32)
    y_ctx = np.empty((B, 256, D), np.float32)
    for j in range(NCORES):
        b, g = j // 4, j % 4
        y_lat[b][:, g * 256:(g + 1) * 256] = r[j]["ylat"]
        y_ctx[b][:, g * 256:(g + 1) * 256] = r[j]["yctx"]

    def run_post(layer, xl, xc, yl, yc, wout, with_ctx, final_norm):
        w1r, w2r = lay_w1(f32(expert_w1[layer])), lay_w2(f32(expert_w2[layer]))
        b1c, b2 = lay_b1(f32(expert_b1[layer])), f32(expert_b2[layer])
        xl_f, yl_f = xl.reshape(-1, D), yl.reshape(-1, D)
        ims = []
        for j in range(NCORES):
            b = j // 4
            xr, yp = xl_f[j * 2048:(j + 1) * 2048], yl_f[j * 2048:(j + 1) * 2048]
            if with_ctx:
                xr = np.concatenate([xr, xc.reshape(-1, D)[j * 64:(j + 1) * 64]], 0)
                yp = np.concatenate([yp, yc.reshape(-1, D)[j * 64:(j + 1) * 64]], 0)
            ims.append(dict(xres=np.ascontiguousarray(xr), ypre=np.ascontiguousarray(yp), wout=wout, modw=mod_w[layer],
                            modb=mod_b[layer][None, :], cond_cols=cond_cols[b],
                            n2g_col=lay_cols(norm2_g[layer])[:, :, 0], wr=f32(router_w[layer]),
                            br=f32(router_b[layer])[None, :], w1r=w1r, b1c=b1c, w2r=w2r, b2=b2,
                            fin_rep=np.ascontiguousarray(np.broadcast_to(f32(final_norm_g), (128, D))), ident=ident))
        r = _run(build_post_prog(with_ctx, final_norm), ims)
        xo = np.concatenate([r[j]["xout"][0:2048] for j in range(NCORES)], 0).reshape(B, 8192, D)
        xco = None
        if with_ctx:
            xco = np.concatenate([r[j]["xout"][2048:2112] for j in range(NCORES)], 0).reshape(B, 256, D)
        return xo, xco

    if dbg is not None:
        dbg["y_lat"], dbg["y_ctx"] = y_lat, y_ctx
    x1_lat, x1_ctx = run_post(0, x, ctx, y_lat, y_ctx, f32(fourier_w_out)[0], True, False)
    if dbg is not None:
        dbg["x1_lat"], dbg["x1_ctx"] = x1_lat, x1_ctx

    hw_in = f32(hgrn_w_in)[0]
    lbr = f32(hgrn_lower_bounds)
    hng = f32(hgrn_norm_g)[0]
    hc = hgrn_consts()
    ims = []
    for j in range(NCORES):
        b, hp = j // 4, j % 4
        cols = slice(hp * 256, (hp + 1) * 256)
        m = dict(x=x1_lat[b], xctx=x1_ctx[b], modw=mod_w[1], modb=mod_b[1][None, :], cond_cols=cond_cols[b],
                 n1g_col=lay_cols(norm1_g[1])[:, :, 0], win5=lay_win5(hw_in, hp),
                 lbrep=np.ascontiguousarray(np.broadcast_to(lbr[:, :, cols], (128, 2, 2, 256))),
                 hng_rep=np.ascontiguousarray(np.broadcast_to(hng[cols], (128, 256))), ident=ident)
        m.update(hc)
        ims.append(m)
    r = _run(build_hgrn_prog(), ims)
    y1 = np.empty((B, 8192, D), np.float32)
    for j in range(NCORES):
        b, hp = j // 4, j % 4
        y1[b][:, hp * 256:(hp + 1) * 256] = r[j]["ypre"]

    if dbg is not None:
        dbg["y1"] = y1
    out, _ = run_post(1, x1_lat, None, y1, None, f32(hgrn_w_out)[0], False, True)
    return out


GROUPS = [[0, 1, 2, 3], [4, 5, 6, 7]]


def build_fused_prog():
    nc = bass.Bass("TRN2", target_bir_lowering=False)
    E = lambda name, shape: _dt(nc, name, shape)
    I = lambda name, shape, dt=F32: nc.dram_tensor(name, list(shape), dt)
    ext = dict(
        x=E("x", [8192, D]), ctx=E("ctx", [256, D]), xres0=E("xres0", [2112, D]),
        cond_cols=E("cond_cols", [128, 8, 2]), sel=E("sel", [128, 4]), ident=E("ident", [128, 128]),
        modw0=E("modw0", [D, 6 * D]), modb0=E("modb0", [1, 6 * D]), modw1=E("modw1", [D, 6 * D]),
        modb1=E("modb1", [1, 6 * D]),
        n1g0=E("n1g0", [128, 8]), n1g1=E("n1g1", [128, 8]), n2g0=E("n2g0", [128, 8]), n2g1=E("n2g1", [128, 8]),
        winT=E("winT", [256, D]), fwout=E("fwout", [D, D]), hwout=E("hwout", [D, D]),
        win5=E("win5", [D, 1280]), lbrep=E("lbrep", [128, 2, 2, 256]), hng_rep=E("hng_rep", [128, 256]),
        fin_rep=E("fin_rep", [128, D]))
    for nm, shp in (("cs_ch", [256, 512]), ("rs1", [128, 256]), ("rs2", [128, 256]), ("bdc", [128, 128]),
                    ("bds", [128, 128]), ("c256", [256, 256]), ("s256n", [256, 256]), ("tri_fw", [128, 128]),
                    ("tri_bw", [128, 128]), ("su_fw", [128, 128]), ("su_bw", [128, 128])):
        ext[nm] = E(nm, shp)
    for l in range(2):
        ext["wr%d" % l] = E("wr%d" % l, [D, NE])
        ext["br%d" % l] = E("br%d" % l, [1, NE])
        ext["w1r%d" % l] = E("w1r%d" % l, [NE, 8, 128, 2048])
        ext["b1c%d" % l] = E("b1c%d" % l, [128, NE * 16])
        ext["w2r%d" % l] = E("w2r%d" % l, [NE, 128, 8192])
        ext["b2%d" % l] = E("b2%d" % l, [NE, D])
    xout = _dt(nc, "xout", [2048, D], "ExternalOutput")

    yF = [I("yF%d" % c, [2048, 256], BF16) for c in range(4)]
    yFc = I("yFc", [256, 256], BF16)
    G1 = [I("G1_%d" % c, [4 * 2048, 256], BF16) for c in range(4)]
    G1c = I("G1c", [4 * 256, 256], BF16)
    x1 = [I("x1_%d" % c, [256, D]) for c in range(8)]
    x1c = I("x1c", [64, D])
    h1s = [I("h1s_%d" % c, [4 * 128, KC * 128], BF16) for c in range(4)]
    h1sc = I("h1sc", [128, KC * 64], BF16)
    G2 = [I("G2_%d" % c, [4 * 512, KC * 128], BF16) for c in range(4)]
    G2c = I("G2c", [4 * 128, KC * 64], BF16)
    yH = [I("yH%d" % c, [2048, 256], BF16) for c in range(4)]
    G3 = [I("G3_%d" % c, [4 * 2048, 256], BF16) for c in range(4)]

    cx = Ctx(nc)
    s = cx.s
    load_consts(cx, ext["ident"])

    drF = dict(x=ext["x"], ctx=ext["ctx"], yctx=yFc.ap(), ylat_chunks=[t.ap() for t in yF], out_dt=BF16,
               modw=ext["modw0"], modb=ext["modb0"], cond_cols=ext["cond_cols"], n1g_col=ext["n1g0"],
               winT=ext["winT"])
    for nm in ("cs_ch", "rs1", "rs2", "bdc", "bds", "c256", "s256n"):
        drF[nm] = ext[nm]
    drF["after_out"] = lambda c: s.collective("AllGather", [yF[c].ap().opt()], [G1[c].ap().opt()], GROUPS,
                                              reads=["+ylatc%d" % c])
    drF["after_ctx"] = lambda: s.collective("AllGather", [yFc.ap().opt()], [G1c.ap().opt()], GROUPS,
                                            reads=["+yctxo"])
    emit_fourier(cx, drF)
    s.barrier()

    def x1_tile(row0, rows):
        if row0 >= 2048:
            return x1c.ap()[0:rows, :]
        return x1[row0 // 256].ap()[row0 % 256:row0 % 256 + rows, :]

    def cands0(row0, rows):
        if row0 >= 2048:
            v = G1c.ap().rearrange("(g r) c -> r g c", g=4)
            return [v[64 * k:64 * k + rows] for k in range(4)]
        return [G1[k].ap().rearrange("(g r) c -> r g c", g=4)[row0:row0 + rows] for k in range(4)]

    def post_dr(l, wout, cands, xres_tile, xout_tile):
        return dict(ypre_cands=cands, sel=ext["sel"], xres_tile=xres_tile, xout_tile=xout_tile, wout=wout,
                    modw=ext["modw%d" % l], modb=ext["modb%d" % l], cond_cols=ext["cond_cols"],
                    n2g_col=ext["n2g%d" % l], wr=ext["wr%d" % l], br=ext["br%d" % l], w1r=ext["w1r%d" % l],
                    b1c=ext["b1c%d" % l], w2r=ext["w2r%d" % l], b2=ext["b2%d" % l], fin_rep=ext["fin_rep"])

    def h1_dst(row0, rows):
        if row0 >= 2048:
            return h1sc.ap()[:, :]
        lt = row0 // 128
        return h1s[lt // 4].ap()[(lt % 4) * 128:(lt % 4) * 128 + 128, :]

    dr0 = post_dr(0, ext["fwout"], cands0, lambda row0, rows: ext["xres0"][row0:row0 + rows, :], x1_tile)
    dr0["h1"] = dict(modw=ext["modw1"], modb=ext["modb1"], cond_cols=ext["cond_cols"], n1g_col=ext["n1g1"],
                     tile_dst=h1_dst)
    with cx.scope():
        shared0 = emit_post_params(cx, dr0, 2, "L0")
        for pi, tiles in enumerate(post_tiles(True)):
            emit_post(cx, tiles, 2, dr0, False, tagp="P0%d" % pi, shared=shared0)
            for c in range(2 * pi, 2 * pi + 2):
                s.collective("AllGather", [h1s[c].ap().opt()], [G2[c].ap().opt()], GROUPS)
            if pi == 0:
                s.collective("AllGather", [h1sc.ap().opt()], [G2c.ap().opt()], GROUPS)
    s.barrier()

    def hx_tile(kind, i):
        if kind == "ctx":
            return [(slice(64 * q, 64 * q + 64), G2c.ap()[(2 * i + q) * 128:(2 * i + q) * 128 + 128, :])
                    for q in range(2)]
        r, lt = i // 16, i % 16
        return [(slice(0, 128), G2[lt // 4].ap()[r * 512 + (lt % 4) * 128:r * 512 + (lt % 4) * 128 + 128, :])]

    drH = dict(h_tile=hx_tile, ypre_tile=lambda i: yH[i // 16].ap()[(128 * i) % 2048:(128 * i) % 2048 + 128, :],
               out_dt=BF16, modw=ext["modw1"], modb=ext["modb1"], cond_cols=ext["cond_cols"], n1g_col=ext["n1g1"],
               win5=ext["win5"], lbrep=ext["lbrep"], hng_rep=ext["hng_rep"])
    for nm in ("tri_fw", "tri_bw", "su_fw", "su_bw"):
        drH[nm] = ext[nm]
    drH["ypre_key"] = lambda i: "+yHc%d" % (i // 16)

    def after_h(i):
        if i % 16 == 0:
            s.collective("AllGather", [yH[i // 16].ap().opt()], [G3[i // 16].ap().opt()], GROUPS,
                         reads=["+yHc%d" % (i // 16)])
    drH["after_out"] = after_h
    emit_hgrn(cx, drH)
    s.barrier()

    cands1 = lambda row0, rows: [G3[k].ap().rearrange("(g r) c -> r g c", g=4)[row0:row0 + rows] for k in range(4)]
    dr1 = post_dr(1, ext["hwout"], cands1, x1_tile, lambda row0, rows: xout[row0:row0 + rows, :])
    with cx.scope():
        shared1 = emit_post_params(cx, dr1, 2, "L1")
        for pi, tiles in enumerate(post_tiles(False)):
            emit_post(cx, tiles, 2, dr1, True, tagp="P1%d" % pi, shared=shared1)
    s.barrier(["sp"])
    return nc


def kernel(x, c, ctx, c_ctx, mod_w, mod_b, norm1_g, norm2_g, fourier_w_in, fourier_w_out,
           hgrn_w_in, hgrn_lower_bounds, hgrn_norm_g, hgrn_w_out, router_w, router_b,
           expert_w1, expert_b1, expert_w2, expert_b2, final_norm_g):
    f32 = lambda a: np.ascontiguousarray(np.asarray(a, dtype=np.float32))
    x, c, ctx, c_ctx = f32(x), f32(c), f32(ctx), f32(c_ctx)
    mod_w, mod_b = f32(mod_w), f32(mod_b)
    norm1_g, norm2_g = f32(norm1_g), f32(norm2_g)
    B = x.shape[0]
    shared = dict(ident=np.eye(128, dtype=np.float32),
                  modw0=mod_w[0], modb0=mod_b[0][None, :], modw1=mod_w[1], modb1=mod_b[1][None, :],
                  n1g0=lay_cols(norm1_g[0])[:, :, 0], n1g1=lay_cols(norm1_g[1])[:, :, 0],
                  n2g0=lay_cols(norm2_g[0])[:, :, 0], n2g1=lay_cols(norm2_g[1])[:, :, 0],
                  fwout=f32(fourier_w_out)[0], hwout=f32(hgrn_w_out)[0],
                  fin_rep=np.ascontiguousarray(np.broadcast_to(f32(final_norm_g), (128, D))))
    shared.update(dft_consts())
    shared.update(hgrn_consts())
    for l in range(2):
        shared["wr%d" % l] = f32(router_w[l])
        shared["br%d" % l] = f32(router_b[l])[None, :]
        shared["w1r%d" % l] = lay_w1(f32(expert_w1[l]))
        shared["b1c%d" % l] = lay_b1(f32(expert_b1[l]))
        shared["w2r%d" % l] = lay_w2(f32(expert_w2[l]))
        shared["b2%d" % l] = f32(expert_b2[l])
    fw_in = f32(fourier_w_in)[0]
    hw_in = f32(hgrn_w_in)[0]
    lbr = f32(hgrn_lower_bounds)
    hng = f32(hgrn_norm_g)[0]
    x_f, ctx_f = x.reshape(-1, D), ctx.reshape(-1, D)
    ims = []
    for j in range(NCORES):
        b, g = j // 4, j % 4
        cols = slice(g * 256, (g + 1) * 256)
        sel = np.zeros((128, 4), np.float32)
        sel[:, g] = 1.0
        m = dict(shared)
        m.update(x=x[b], ctx=ctx[b],
                 xres0=np.ascontiguousarray(np.concatenate([x_f[j * 2048:(j + 1) * 2048], ctx_f[j * 64:(j + 1) * 64]], 0)),
                 cond_cols=lay_cols(np.stack([c_ctx, c[b]])), sel=sel,
                 winT=np.ascontiguousarray(fw_in[:, cols].T), win5=lay_win5(hw_in, g),
                 lbrep=np.ascontiguousarray(np.broadcast_to(lbr[:, :, cols], (128, 2, 2, 256))),
                 hng_rep=np.ascontiguousarray(np.broadcast_to(hng[cols], (128, 256))))
        ims.append(m)
    r = _run(build_fused_prog(), ims)
    return np.concatenate([r[j]["xout"] for j in range(NCORES)], 0).reshape(B, 8192, D)
```

```python
import numpy as np
from contextlib import ExitStack, contextmanager
import concourse.bass as bass
import concourse.mybir as mybir
from concourse.bass_utils import run_bass_kernel_spmd

F32 = mybir.dt.float32
BF16 = mybir.dt.bfloat16
AF = mybir.ActivationFunctionType
ALU = mybir.AluOpType
AX = mybir.AxisListType

D = 1024
KC = 8
NE = 32
EPS = 1e-6
NCORES = 8


class Sched:
    ENG = ("pe", "dve", "act", "pool", "sp")

    def __init__(self, nc, n_dma_sems=48):
        self.nc = nc
        self.e = {"pe": nc.tensor, "dve": nc.vector, "act": nc.scalar,
                  "pool": nc.gpsimd, "sp": nc.sync}
        self.sem = {k: nc.alloc_semaphore("sem_" + k) for k in self.ENG}
        self.cnt = {k: 0 for k in self.ENG}
        self.dsem = [nc.alloc_semaphore("dsem%d" % i) for i in range(n_dma_sems)]
        self.dval = [0] * n_dma_sems
        self.dnext = 0
        self.n_hw = (n_dma_sems * 3) // 4
        self.dnext_sw = self.n_hw
        self.seen = {k: {} for k in self.ENG}
        self.lastw = {}
        self.readers = {}
        self.multiw = {}
        self.genreaders = {}
        self.pslock = {}
        self.know = {}
        self.ccsems = []
        self.cctoks = []

    def _wait(self, eng, tok):
        key, sem, val = tok
        if key == "pe" and eng == "pe":
            return
        if self.seen[eng].get(key, 0) >= val:
            return
        self.e[eng].wait_ge(sem, val)
        self.seen[eng][key] = val
        snap = self.know.get((key, val))
        if snap:
            mine = self.seen[eng]
            for k2, v2 in snap.items():
                if mine.get(k2, 0) < v2:
                    mine[k2] = v2

    def _deps(self, eng, reads, writes):
        for r in reads:
            if r.startswith("+"):
                for t in self.multiw.get(r, {}).values():
                    self._wait(eng, t)
                continue
            t = self.lastw.get(r)
            if t is not None:
                self._wait(eng, t)
        for w in writes:
            if not w.startswith("+"):
                t = self.lastw.get(w)
                if t is not None:
                    self._wait(eng, t)
            else:
                for t in self.genreaders.get(w, {}).values():
                    self._wait(eng, t)
            for t in self.readers.get(w, {}).values():
                self._wait(eng, t)

    def _record(self, tok, reads, writes):
        for w in writes:
            if w.startswith("+"):
                if self.readers.get(w):
                    self.multiw[w] = {}
                    self.genreaders[w] = dict(self.readers[w])
                self.multiw.setdefault(w, {})[tok[0]] = tok
            else:
                self.lastw[w] = tok
            self.readers[w] = {}
        for r in reads:
            if r in writes:
                continue
            self.readers.setdefault(r, {})[tok[0]] = tok

    def op(self, eng, emit, reads=(), writes=()):
        self._deps(eng, reads, writes)
        for r in reads:
            if r.startswith("ps"):
                t = self.pslock.get(r)
                if t is not None and t[0] != eng:
                    self._wait(eng, t)
        ins = emit(self.e[eng])
        self.cnt[eng] += 1
        ins.then_inc(self.sem[eng], 1)
        tok = (eng, self.sem[eng], self.cnt[eng])
        self.know[(eng, self.cnt[eng])] = dict(self.seen[eng])
        self._record(tok, reads, writes)
        for r in list(reads) + list(writes):
            if r.startswith("ps"):
                self.pslock[r] = tok
        return tok

    def dma(self, q, out, in_, reads=(), writes=()):
        self._deps(q, reads, writes)
        if q == "pool":
            i = self.dnext_sw
            self.dnext_sw = self.n_hw + (i + 1 - self.n_hw) % (len(self.dsem) - self.n_hw)
        else:
            i = self.dnext
            self.dnext = (i + 1) % self.n_hw
        key = "d%d" % i
        if self.dval[i] > 0:
            self._wait(q, (key, self.dsem[i], self.dval[i]))
        self.dval[i] += 16
        self.e[q].dma_start(out=out, in_=in_).then_inc(self.dsem[i], 16)
        tok = (key, self.dsem[i], self.dval[i])
        self.know[(key, self.dval[i])] = dict(self.seen[q])
        self._record(tok, reads, writes)
        return tok

    def collective(self, kind, ins, outs, groups, reads=(), writes=()):
        q = "pool"
        self._deps(q, reads, writes)
        sem = self.nc.alloc_semaphore("ccsem%d" % len(self.ccsems))
        self.ccsems.append(sem)
        self.e[q].collective_compute(kind, ALU.bypass, replica_groups=groups, ins=ins, outs=outs).then_inc(sem, 1)
        tok = ("cc%d" % len(self.ccsems), sem, 1)
        self._record(tok, reads, writes)
        self.cctoks.append(tok)
        return tok

    def barrier(self, engines=None):
        engines = engines or self.ENG
        for eng in engines:
            for f in self.ENG:
                if self.cnt[f] > 0:
                    self._wait(eng, (f, self.sem[f], self.cnt[f]))
            for i, v in enumerate(self.dval):
                if v > 0:
                    self._wait(eng, ("d%d" % i, self.dsem[i], v))
            for tok in self.cctoks:
                self._wait(eng, tok)


def ps_banks(nc):
    return [nc.alloc_psum_tensor("psb%d" % i, [128, 512], F32) for i in range(8)]


class Ctx:
    def __init__(self, nc):
        self.nc = nc
        self.s = Sched(nc)
        self.ps = ps_banks(nc)
        self.uid = 0
        self.stacks = []
        a = lambda name, shape, dt: nc.alloc_sbuf_tensor(name, shape, dt)
        self.ident = a("ident_sb", [128, 128], F32)
        self.ones_row = a("ones_row", [1, 128], F32)
        self.eps_col = a("eps_col", [128, 1], F32)
        self.one_col = a("one_col", [128, 1], F32)

    def name(self, base):
        self.uid += 1
        return "%s_%d" % (base, self.uid)

    @contextmanager
    def scope(self):
        st = ExitStack()
        self.stacks.append(st)
        try:
            yield
        finally:
            self.s.barrier()
            self.s.lastw = {}
            self.s.readers = {}
            self.s.multiw = {}
            self.s.genreaders = {}
            self.s.know = {}
            self.stacks.pop()
            st.close()

    def A(self, name, shape, dt):
        g = self.nc.sbuf_tensor(self.name(name), shape, dt)
        return self.stacks[-1].enter_context(g)


def load_consts(cx, ident_dram):
    s = cx.s
    s.dma("sp", cx.ident[:], ident_dram, writes=["ident"])
    s.op("dve", lambda e: e.memset(cx.ones_row[:], 1.0), writes=["ones_row"])
    s.op("dve", lambda e: e.memset(cx.eps_col[:], EPS), writes=["eps_col"])
    s.op("dve", lambda e: e.memset(cx.one_col[:], 1.0), writes=["one_col"])


def emit_rstd(cx, x_ap, rows, junk, ss, rstd, xkey, tag):
    s = cx.s
    s.op("act", lambda e: e.activation(out=junk[0:rows, :], in_=x_ap, func=AF.Square,
                                       accum_out=ss[0:rows, :]),
         reads=[xkey, "eps_col"], writes=[tag + "junk", tag + "ss"])
    s.op("act", lambda e: e.activation(out=rstd[0:rows, :], in_=ss[0:rows, :], func=AF.Ln,
                                       bias=cx.eps_col[0:rows, :], scale=1.0 / D),
         reads=[tag + "ss", "eps_col"], writes=[tag + "rstd"])
    s.op("act", lambda e: e.activation(out=rstd[0:rows, :], in_=rstd[0:rows, :], func=AF.Exp,
                                       scale=-0.5),
         reads=[tag + "rstd"], writes=[tag + "rstd"])


def emit_transpose8(cx, src, rows, bank_a, bank_b, srckey):
    s = cx.s
    pa, pb = cx.ps[bank_a], cx.ps[bank_b]

    def emit_half(e, bank, k0):
        ins = None
        for q in range(4):
            kc = k0 + q
            ins = e.transpose(out=bank[:, q * 128:q * 128 + rows],
                              in_=src[0:rows, kc * 128:(kc + 1) * 128],
                              identity=cx.ident[0:rows, 0:rows])
        return ins
    s.op("pe", lambda e: emit_half(e, pa, 0), reads=[srckey, "ident"], writes=["ps%d" % bank_a])
    s.op("pe", lambda e: emit_half(e, pb, 4), reads=[srckey, "ident"], writes=["ps%d" % bank_b])


def emit_mod_cols(cx, modw, modb_row, silu_cols, ncond, col_blocks, out_cols, tag):
    s = cx.s
    with cx.scope():
        wbuf = [cx.A("mcw", [128, KC, 128], F32) for _ in range(2)]
        for i, blk in enumerate(col_blocks):
            wb = wbuf[i % 2]
            wk = tag + "mcw%d" % (i % 2)
            s.dma("sp", wb[:], modw[:, blk * 128:(blk + 1) * 128].rearrange("(kc p) n -> p kc n", p=128),
                  writes=[wk])
            bank = 4 + (i % 4)

            def emit(e, wb=wb, blk=blk, bank=bank):
                pt = cx.ps[bank]
                for kc in range(KC):
                    e.matmul(pt[:, 0:ncond], wb[:, kc, :], silu_cols[:, kc, :], start=(kc == 0), stop=False)
                return e.matmul(pt[:, 0:ncond], modb_row[0:1, blk * 128:(blk + 1) * 128],
                                cx.ones_row[0:1, 0:ncond], start=False, stop=True)
            s.op("pe", emit, reads=[wk, tag + "silu", tag + "modb", "ones_row"], writes=["ps%d" % bank])
            s.op("dve", lambda e, i=i, bank=bank: e.tensor_copy(out_cols[:, i, :], cx.ps[bank][:, 0:ncond]),
                 reads=["ps%d" % bank], writes=["+" + tag + "cols"])


def emit_mod_rows(cx, modw, modb_row, silu_rep, ncond, col0, out_rep, tag, okey):
    s = cx.s
    with cx.scope():
        wbuf = [cx.A("mrw", [128, KC, 512], F32) for _ in range(2)]
        for h in range(2):
            wb = wbuf[h]
            wk = tag + "mrw%d" % h
            c0 = col0 + h * 512
            s.dma("sp", wb[:], modw[:, c0:c0 + 512].rearrange("(kc p) n -> p kc n", p=128), writes=[wk])
            for c in range(ncond):
                bank = 4 + ((2 * h + c) % 4)

                def emit(e, wb=wb, c=c, c0=c0, bank=bank):
                    pt = cx.ps[bank]
                    for kc in range(KC):
                        e.matmul(pt[:, :], silu_rep[:, c, kc, :], wb[:, kc, :], start=(kc == 0), stop=False)
                    return e.matmul(pt[:, :], cx.ones_row[0:1, :], modb_row[0:1, c0:c0 + 512],
                                    start=False, stop=True)
                s.op("pe", emit, reads=[wk, tag + "silurep", tag + "modb", "ones_row"], writes=["ps%d" % bank])
                s.op("dve", lambda e, c=c, h=h, bank=bank: e.tensor_copy(
                    out_rep[:, c, h * 512:(h + 1) * 512], cx.ps[bank][:, :]),
                    reads=["ps%d" % bank], writes=[okey])


def emit_silu_cond(cx, cond_cols_dram, ncond, tag, need_rep):
    s = cx.s
    cc = cx.A("condc", [128, KC, ncond], F32)
    sg = cx.A("conds", [128, KC, ncond], F32)
    ck, sk = tag + "silu", tag + "sg"
    s.dma("sp", cc[:], cond_cols_dram, writes=[ck])
    s.op("act", lambda e: e.activation(out=sg[:], in_=cc[:], func=AF.Exp, scale=-1.0), reads=[ck], writes=[sk])
    s.op("dve", lambda e: e.tensor_scalar(out=sg[:], in0=sg[:], scalar1=1.0, scalar2=None, op0=ALU.add),
         reads=[sk], writes=[sk])
    s.op("dve", lambda e: e.reciprocal(out=sg[:], in_=sg[:]), reads=[sk], writes=[sk])
    s.op("dve", lambda e: e.tensor_tensor(out=cc[:], in0=cc[:], in1=sg[:], op=ALU.mult),
         reads=[sk, ck], writes=[ck])
    rep = None
    if need_rep:
        rep = cx.A("condrep", [128, ncond, KC, 128], F32)
        s.op("pool", lambda e: e.memset(rep[:], 1.0), writes=[tag + "silurep"])
        for c in range(ncond):
            for kc in range(KC):
                s.op("dve", lambda e, c=c, kc=kc: e.tensor_scalar(
                    out=rep[:, c, kc, :], in0=rep[:, c, kc, :], scalar1=cc[:, kc, c:c + 1], scalar2=None,
                    op0=ALU.mult), reads=[ck, tag + "silurep"], writes=[tag + "silurep"])
    return cc, rep


def emit_post_params(cx, dr, ncond, tg):
    s, A = cx.s, cx.A
    m2 = A("m2cols", [128, 16, ncond], F32)
    A2 = A("A2", [128, KC, ncond], F32)
    g1rep = A("g1rep", [128, ncond, D], F32)
    g2rep = A("g2rep", [128, ncond, D], F32)
    with cx.scope():
        modb_row = A("modb", [1, 6 * D], F32)
        s.dma("sp", modb_row[:], dr["modb"], writes=[tg + "modb"])
        silu_cols, silu_rep = emit_silu_cond(cx, dr["cond_cols"], ncond, tg, True)
        emit_mod_cols(cx, dr["modw"], modb_row, silu_cols, ncond, list(range(24, 40)), m2, tg)
        n2g = A("n2g", [128, KC], F32)
        s.dma("sp", n2g[:], dr["n2g_col"], writes=[tg + "n2g"])
        for c in range(ncond):
            s.op("dve", lambda e, c=c: e.scalar_tensor_tensor(
                out=A2[:, :, c], in0=m2[:, 8:16, c], scalar=1.0, in1=n2g[:, :], op0=ALU.add, op1=ALU.mult),
                reads=["+" + tg + "cols", tg + "n2g"], writes=[tg + "A2"])
        emit_mod_rows(cx, dr["modw"], modb_row, silu_rep, ncond, 2 * D, g1rep, tg + "g1", tg + "g1rep")
        emit_mod_rows(cx, dr["modw"], modb_row, silu_rep, ncond, 5 * D, g2rep, tg + "g2", tg + "g2rep")
    return dict(m2=m2, A2=A2, g1rep=g1rep, g2rep=g2rep)


def emit_post(cx, tiles, ncond, dr, final_norm, n_exp=NE, tagp="P", shared=None):
    s = cx.s
    A = cx.A
    T = sum(r for _, r, _ in tiles)
    nt = len(tiles)
    col0 = []
    c = 0
    for _, r, _ in tiles:
        col0.append(c)
        c += r
    tg = tagp
    ngr = -(-T // 512)
    gsz = -(-T // ngr)
    groups = []
    c = 0
    while c < T:
        w = min(gsz, T - c)
        groups.append((c, w))
        c += w

    with cx.scope():
        XA = A("XA", [128, nt, D], F32)
        H2T = A("H2T", [128, KC, T], BF16)
        gates = A("gates", [128, nt, n_exp], F32)
        if shared is None:
            shared = emit_post_params(cx, dr, ncond, tg)
        m2, A2, g1rep, g2rep = shared["m2"], shared["A2"], shared["g1rep"], shared["g2rep"]
        tmpa = [A("tmpa", [128, 512], F32) for _ in range(2)]
        junk = A("junk", [128, D], F32)
        ss = A("ss", [128, 1], F32)
        rstd = A("rstd", [128, 1], F32)
        b1c = A("b1c", [128, n_exp * 16], F32)
        s.dma("sp", b1c[:], dr["b1c"], writes=[tg + "b1c"])
        b1c1 = A("b1c1", [128, n_exp * 16], F32)
        s.op("dve", lambda e: e.tensor_scalar(out=b1c1[:], in0=b1c[:], scalar1=1.0, scalar2=None, op0=ALU.add),
             reads=[tg + "b1c"], writes=[tg + "b1c1"])
        if final_norm:
            finrep = A("finrep", [128, D], F32)
            s.dma("sp", finrep[:], dr["fin_rep"], writes=[tg + "finrep"])

        with cx.scope():
            wout = A("wout", [128, KC, D], BF16)
            s.dma("pool", wout[:], dr["wout"].rearrange("(kc p) n -> p kc n", p=128), writes=[tg + "wout"])
            wr = A("wr", [128, KC, n_exp], F32)
            s.dma("sp", wr[:], dr["wr"].rearrange("(kc p) n -> p kc n", p=128), writes=[tg + "wr"])
            br = A("br", [1, n_exp], F32)
            s.dma("sp", br[:], dr["br"], writes=[tg + "br"])
            b2 = A("b2", [n_exp, D], F32)
            s.dma("sp", b2[:], dr["b2"], writes=[tg + "b2"])

            yt = [A("yt", [128, D], F32) for _ in range(2)]
            tmpf = [[A("tmpf", [128, 512], F32) for _ in range(2)] for _ in range(2)]
            ssf = [A("ssf", [128, 1], F32) for _ in range(2)]
            rstdf = [A("rstdf", [128, 1], F32) for _ in range(2)]
            junkf = [A("junkf", [128, D], BF16) for _ in range(2)]
            if "ypre_cands" in dr:
                cand = [[A("cand", [128, 4, 256], BF16) for _ in range(4)] for _ in range(2)]
                sel = A("sel", [128, 4], F32)
                s.dma("sp", sel[:], dr["sel"], writes=[tg + "sel"])
            xt = [A("xt", [128, D], F32) for _ in range(2)]
            xn = [A("xn", [128, D], F32) for _ in range(2)]
            ypT = [A("ypT", [128, KC, 128], BF16) for _ in range(2)]
            h2f = [A("h2f", [128, KC, 128], F32) for _ in range(2)]
            lg = [A("lg", [128, n_exp], F32) for _ in range(2)]
            mx8 = [A("mx8", [128, 8], F32) for _ in range(2)]
            negm = [A("negm", [128, 1], F32) for _ in range(2)]
            msk = [A("msk", [128, n_exp], F32) for _ in range(2)]
            ex = [A("ex", [128, n_exp], F32) for _ in range(2)]
            ssum = [A("ssum", [128, 1], F32) for _ in range(2)]
            gT = [A("gT", [n_exp, 128], F32) for _ in range(2)]

            def front_gen(ti, row0, rows, cond):
                b = ti % 2
                B0 = 4 * b
                ytk, xtk, xnk, ypk, h2k = (tg + "yt%d" % b, tg + "xt%d" % b, tg + "xn%d" % b,
                                           "+" + tg + "ypT%d" % b, "+" + tg + "h2f%d" % b)
                xak = tg + "XA%d" % ti
                R = slice(0, rows)
                if "ypre_cands" in dr:
                    cands = dr["ypre_cands"](row0, rows)
                    for kq, cap in enumerate(cands):
                        s.dma("sp", cand[b][kq][R, :, :], cap, reads=dr.get("ypre_keys", []), writes=[tg + "cand%d_%d" % (b, kq)])
                    s.op("dve", lambda e, b=b, R=R: e.tensor_scalar(
                        out=yt[b][R, :], in0=cand[b][0][R, :, :].rearrange("p g c -> p (g c)"), scalar1=sel[R, 0:1],
                        scalar2=None, op0=ALU.mult), reads=[tg + "cand%d_0" % b, tg + "sel"], writes=[ytk])
                    yield
                    for kq in range(1, 4):
                        s.op("dve", lambda e, b=b, R=R, kq=kq: e.scalar_tensor_tensor(
                            out=yt[b][R, :], in0=cand[b][kq][R, :, :].rearrange("p g c -> p (g c)"),
                            scalar=sel[R, kq:kq + 1], in1=yt[b][R, :], op0=ALU.mult, op1=ALU.add),
                            reads=[tg + "cand%d_%d" % (b, kq), tg + "sel", ytk], writes=[ytk])
                        yield
                else:
                    s.dma("sp", yt[b][R, :], dr["ypre"][row0:row0 + rows, :], writes=[ytk])
                xsrc = dr["xres_tile"](row0, rows) if "xres_tile" in dr else dr["xres"][row0:row0 + rows, :]
                s.dma("sp", xt[b][R, :], xsrc, writes=[xtk])
                emit_transpose8(cx, yt[b], rows, B0, B0 + 1, ytk)
                yield
                s.op("act", lambda e, b=b, rows=rows: e.copy(
                    out=ypT[b][:, 0:4, 0:rows],
                    in_=cx.ps[B0][:, :].rearrange("p (q t) -> p q t", q=4)[:, :, 0:rows]),
                    reads=["ps%d" % B0], writes=[ypk])
                yield
                s.op("dve", lambda e, b=b, rows=rows: e.tensor_copy(
                    ypT[b][:, 4:8, 0:rows], cx.ps[B0 + 1][:, :].rearrange("p (q t) -> p q t", q=4)[:, :, 0:rows]),
                    reads=["ps%d" % (B0 + 1)], writes=[ypk])
                yield
                for h in range(2):
                    bank = B0 + 2 + h
                    H = slice(h * 512, (h + 1) * 512)

                    def emit(e, b=b, H=H, bank=bank, rows=rows):
                        ins = None
                        for kc in range(KC):
                            ins = e.matmul(cx.ps[bank][0:rows, :], ypT[b][:, kc, 0:rows], wout[:, kc, H],
                                           start=(kc == 0), stop=(kc == KC - 1))
                        return ins
                    s.op("pe", emit, reads=[ypk, tg + "wout"], writes=["ps%d" % bank])
                    yield
                    tk = tg + "tmpf%d_%d" % (b, h)
                    s.op("dve", lambda e, h=h, H=H, bank=bank, R=R, cond=cond: e.tensor_tensor(
                        out=tmpf[b][h][R, :], in0=cx.ps[bank][R, :], in1=g1rep[R, cond, H], op=ALU.mult),
                        reads=["ps%d" % bank, tg + "g1rep"], writes=[tk])
                    yield
                    s.op("pool", lambda e, h=h, H=H, R=R, b=b, ti=ti: e.tensor_tensor(
                        out=XA[R, ti, H], in0=tmpf[b][h][R, :], in1=xt[b][R, H], op=ALU.add),
                        reads=[tk, xtk], writes=[xak])
                    yield
                emit_rstd(cx, XA[R, ti, :], rows, junkf[b], ssf[b], rstdf[b], xak, tg + "f%d" % b)
                yield
                s.op("dve", lambda e, R=R, b=b, ti=ti: e.tensor_scalar(
                    out=xn[b][R, :], in0=XA[R, ti, :], scalar1=rstdf[b][R, :], scalar2=None, op0=ALU.mult),
                    reads=[xak, tg + "f%d" % b + "rstd"], writes=[xnk])
                yield
                emit_transpose8(cx, xn[b], rows, B0, B0 + 1, xnk)
                yield
                for kc in range(KC):
                    bank = B0 + kc // 4
                    q = kc % 4
                    if kc % 2 == 0:
                        s.op("act", lambda e, b=b, kc=kc, q=q, bank=bank, rows=rows, cond=cond: e.activation(
                            out=h2f[b][:, kc, 0:rows], in_=cx.ps[bank][:, q * 128:q * 128 + rows],
                            func=AF.Identity, bias=m2[:, kc, cond:cond + 1], scale=A2[:, kc, cond:cond + 1]),
                            reads=["ps%d" % bank, tg + "A2", "+" + tg + "cols"], writes=[h2k])
                        yield
                    else:
                        s.op("dve", lambda e, b=b, kc=kc, q=q, bank=bank, rows=rows, cond=cond: e.tensor_scalar(
                            out=h2f[b][:, kc, 0:rows], in0=cx.ps[bank][:, q * 128:q * 128 + rows],
                            scalar1=A2[:, kc, cond:cond + 1], scalar2=m2[:, kc, cond:cond + 1],
                            op0=ALU.mult, op1=ALU.add),
                            reads=["ps%d" % bank, tg + "A2", "+" + tg + "cols"], writes=[h2k])
                        yield
                c0 = col0[ti]
                s.op("pool", lambda e, b=b, rows=rows, c0=c0: e.tensor_copy(
                    H2T[:, :, c0:c0 + rows], h2f[b][:, :, 0:rows]), reads=[h2k], writes=[tg + "H2T%d" % ti])
                yield

                def emit_r(e, b=b, rows=rows):
                    for kc in range(KC):
                        e.matmul(cx.ps[B0 + 2][0:rows, 0:n_exp], h2f[b][:, kc, 0:rows], wr[:, kc, :],
                                 start=(kc == 0), stop=False)
                    return e.matmul(cx.ps[B0 + 2][0:rows, 0:n_exp], cx.ones_row[0:1, 0:rows], br[0:1, :],
                                    start=False, stop=True)
                s.op("pe", emit_r, reads=[h2k, tg + "wr", tg + "br", "ones_row"], writes=["ps%d" % (B0 + 2)])
                yield
                s.op("dve", lambda e, R=R: e.tensor_copy(lg[b][R, :], cx.ps[B0 + 2][R, 0:n_exp]),
                     reads=["ps%d" % (B0 + 2)], writes=[tg + "lg%d" % b])
                yield
                s.op("dve", lambda e, R=R: e.max(out=mx8[b][R, :], in_=lg[b][R, :]), reads=[tg + "lg%d" % b], writes=[tg + "mx8%d" % b])
                yield
                s.op("dve", lambda e, R=R: e.tensor_scalar(out=negm[b][R, :], in0=mx8[b][R, 0:1], scalar1=-1.0,
                                                           scalar2=None, op0=ALU.mult),
                     reads=[tg + "mx8%d" % b], writes=[tg + "negm%d" % b])
                yield
                s.op("dve", lambda e, R=R: e.tensor_scalar(out=msk[b][R, :], in0=lg[b][R, :], scalar1=mx8[b][R, 3:4],
                                                           scalar2=None, op0=ALU.is_ge),
                     reads=[tg + "lg%d" % b, tg + "mx8%d" % b], writes=[tg + "msk%d" % b])
                yield
                s.op("act", lambda e, R=R: e.activation(out=ex[b][R, :], in_=lg[b][R, :], func=AF.Exp, bias=negm[b][R, :],
                                                        scale=1.0),
                     reads=[tg + "lg%d" % b, tg + "negm%d" % b], writes=[tg + "ex%d" % b])
                yield
                s.op("dve", lambda e, R=R: e.tensor_tensor(out=ex[b][R, :], in0=ex[b][R, :], in1=msk[b][R, :], op=ALU.mult),
                     reads=[tg + "ex%d" % b, tg + "msk%d" % b], writes=[tg + "ex%d" % b])
                yield
                s.op("dve", lambda e, R=R: e.reduce_sum(out=ssum[b][R, :], in_=ex[b][R, :], axis=AX.X),
                     reads=[tg + "ex%d" % b], writes=[tg + "ssum%d" % b])
                yield
                s.op("dve", lambda e, R=R: e.reciprocal(out=ssum[b][R, :], in_=ssum[b][R, :]), reads=[tg + "ssum%d" % b],
                     writes=[tg + "ssum%d" % b])
                yield
                gk = tg + "gates%d" % ti
                s.op("dve", lambda e, R=R, ti=ti: e.tensor_scalar(out=gates[R, ti, :], in0=ex[b][R, :],
                                                                  scalar1=ssum[b][R, :], scalar2=None, op0=ALU.mult),
                     reads=[tg + "ex%d" % b, tg + "ssum%d" % b], writes=[gk])
                yield
                s.op("pe", lambda e, R=R, ti=ti, rows=rows: e.transpose(
                    out=cx.ps[B0 + 3][0:n_exp, 0:rows], in_=gates[R, ti, :], identity=cx.ident[R, R]),
                    reads=[gk, "ident"], writes=["ps%d" % (B0 + 3)])
                yield
                s.op("dve", lambda e, rows=rows: e.tensor_copy(gT[b][:, 0:rows], cx.ps[B0 + 3][0:n_exp, 0:rows]),
                     reads=["ps%d" % (B0 + 3)], writes=[tg + "gT%d" % b])
                yield
                for h in range(2):
                    bank = B0 + h
                    H = slice(h * 512, (h + 1) * 512)
                    s.op("pe", lambda e, H=H, bank=bank, rows=rows: e.matmul(
                        cx.ps[bank][0:rows, :], gT[b][:, 0:rows], b2[:, H], start=True, stop=True),
                        reads=[tg + "gT%d" % b, tg + "b2"], writes=["ps%d" % bank])
                    yield
                    tk = tg + "tmpf%d_%d" % (b, h)
                    s.op("dve", lambda e, h=h, H=H, bank=bank, R=R, cond=cond: e.tensor_tensor(
                        out=tmpf[b][h][R, :], in0=cx.ps[bank][R, :], in1=g2rep[R, cond, H], op=ALU.mult),
                        reads=["ps%d" % bank, tg + "g2rep"], writes=[tk])
                    yield
                    s.op("pool", lambda e, h=h, H=H, R=R, ti=ti: e.tensor_tensor(
                        out=XA[R, ti, H], in0=tmpf[b][h][R, :], in1=XA[R, ti, H], op=ALU.add),
                        reads=[tk, xak], writes=[xak])
                    yield

            for ti0 in range(0, len(tiles), 2):
                gens = [front_gen(ti, *tiles[ti]) for ti in range(ti0, min(ti0 + 2, len(tiles)))]
                alive = list(gens)
                while alive:
                    for g_ in list(alive):
                        try:
                            next(g_)
                        except StopIteration:
                            alive.remove(g_)

        with cx.scope():
            ACTT = A("ACTT", [128, KC, T], BF16)
            NW1 = 3
            w1b = [A("w1b", [128, KC, 256], BF16) for _ in range(NW1)]
            w2b = [A("w2b", [128, KC, D], BF16) for _ in range(2)]
            wstage = [A("wstage", [128, 2048], F32) for _ in range(4)]
            gbuf = [A("gbuf", [128, 512], F32) for _ in range(2)]
            sgbuf = [A("sgbuf", [128, 512], F32) for _ in range(2)]
            l0buf = [A("l0buf", [128, 512], F32) for _ in range(2)]
            tbuf = [A("tbuf", [128, 512], F32) for _ in range(2)]
            h2keys = [tg + "H2T%d" % ti for ti in range(nt)]
            wsc = [0]

            def fetch_w1(ci):
                ex_i, j = divmod(ci, KC)
                sb_ = wstage[wsc[0] % len(wstage)]
                sk_w = tg + "wst%d" % (wsc[0] % len(wstage))
                wsc[0] += 1
                s.dma("sp", sb_[:], dr["w1r"][ex_i, j], writes=[sk_w])
                s.op("act", lambda e, wb=w1b[ci % NW1], sb_=sb_: e.copy(
                    out=wb[:, :, :].rearrange("p kc n -> p (kc n)"), in_=sb_[:]),
                    reads=[sk_w], writes=[tg + "w1b%d" % (ci % NW1)])

            def fetch_w2(ex_i, qq):
                sb_ = wstage[wsc[0] % len(wstage)]
                sk_w = tg + "wst%d" % (wsc[0] % len(wstage))
                wsc[0] += 1
                s.dma("sp", sb_[:], dr["w2r"][ex_i][:, qq * 2048:(qq + 1) * 2048], writes=[sk_w])
                s.op("act", lambda e, wb=w2b[ex_i % 2], qq=qq, sb_=sb_: e.copy(
                    out=wb[:, 2 * qq:2 * qq + 2, :].rearrange("p j n -> p (j n)"), in_=sb_[:]),
                    reads=[sk_w], writes=[tg + "w2b%d" % (ex_i % 2)])

            nchunks = n_exp * KC
            fetch_w1(0)
            if nchunks > 1:
                fetch_w1(1)
            it = 0
            st2 = 0
            for ex_i in range(n_exp):
                wb2 = w2b[ex_i % 2]
                w2k = tg + "w2b%d" % (ex_i % 2)
                for j in range(KC):
                    ci = ex_i * KC + j
                    if ci + 2 < nchunks:
                        fetch_w1(ci + 2)
                    if j % 2 == 0:
                        fetch_w2(ex_i, j // 2)
                    wb1 = w1b[ci % NW1]
                    w1k = tg + "w1b%d" % (ci % NW1)
                    bg = b1c[:, ex_i * 16 + j:ex_i * 16 + j + 1]
                    bl1 = b1c1[:, ex_i * 16 + 8 + j:ex_i * 16 + 8 + j + 1]
                    for (g0, gw) in groups:
                        p = it % 2
                        it += 1
                        bg_bank, bl_bank = 2 * p, 2 * p + 1

                        def emit1(e, wb1=wb1, g0=g0, gw=gw, bg_bank=bg_bank, bl_bank=bl_bank):
                            ins = None
                            for kc in range(KC):
                                ins = e.matmul(cx.ps[bg_bank][:, 0:gw], wb1[:, kc, 0:128], H2T[:, kc, g0:g0 + gw],
                                               start=(kc == 0), stop=(kc == KC - 1))
                            for kc in range(KC):
                                ins = e.matmul(cx.ps[bl_bank][:, 0:gw], wb1[:, kc, 128:256], H2T[:, kc, g0:g0 + gw],
                                               start=(kc == 0), stop=(kc == KC - 1))
                            return ins
                        s.op("pe", emit1, reads=[w1k] + h2keys, writes=["ps%d" % bg_bank, "ps%d" % bl_bank])
                        gk_, sk_, l0k, tk_ = (tg + "gb%d" % p, tg + "sb%d" % p, tg + "l0%d" % p, tg + "tb%d" % p)
                        W = slice(0, gw)
                        s.op("dve", lambda e, p=p, W=W, bg=bg, bank=bg_bank: e.tensor_scalar(
                            out=gbuf[p][:, W], in0=cx.ps[bank][:, W], scalar1=bg, scalar2=7.0,
                            op0=ALU.add, op1=ALU.min), reads=["ps%d" % bg_bank, tg + "b1c"], writes=[gk_])
                        s.op("act", lambda e, p=p, W=W: e.activation(
                            out=sgbuf[p][:, W], in_=gbuf[p][:, W], func=AF.Sigmoid, scale=1.702),
                            reads=[gk_], writes=[sk_])
                        s.op("dve", lambda e, p=p, W=W, bl1=bl1, bank=bl_bank: e.tensor_scalar(
                            out=l0buf[p][:, W], in0=cx.ps[bank][:, W], scalar1=bl1, scalar2=8.0,
                            op0=ALU.add, op1=ALU.min), reads=["ps%d" % bl_bank, tg + "b1c1"], writes=[l0k])
                        s.op("pool", lambda e, p=p, W=W: e.tensor_tensor(
                            out=tbuf[p][:, W], in0=gbuf[p][:, W], in1=sgbuf[p][:, W], op=ALU.mult),
                            reads=[gk_, sk_], writes=[tk_])
                        s.op("dve", lambda e, p=p, W=W, j=j, g0=g0, gw=gw: e.scalar_tensor_tensor(
                            out=ACTT[:, j, g0:g0 + gw], in0=l0buf[p][:, W], scalar=-6.0, in1=tbuf[p][:, W],
                            op0=ALU.max, op1=ALU.mult),
                            reads=[tk_, l0k], writes=[tg + "ACTT%d_%d" % (j, g0)])
                for ti, (row0, rows, cond) in enumerate(tiles):
                    c0 = col0[ti]
                    gs = [g0 for (g0, gw) in groups if g0 < c0 + rows and c0 < g0 + gw]
                    akeys = [tg + "ACTT%d_%d" % (j, g0) for j in range(KC) for g0 in gs]
                    xak = tg + "XA%d" % ti
                    R = slice(0, rows)
                    for h in range(2):
                        bank = 4 + (st2 % 4)
                        pp = st2 % 2
                        st2 += 1
                        H = slice(h * 512, (h + 1) * 512)

                        def emit2(e, c0=c0, rows=rows, H=H, bank=bank, wb2=wb2):
                            ins = None
                            for j in range(KC):
                                ins = e.matmul(cx.ps[bank][0:rows, :], ACTT[:, j, c0:c0 + rows], wb2[:, j, H],
                                               start=(j == 0), stop=(j == KC - 1))
                            return ins
                        s.op("pe", emit2, reads=akeys + [w2k], writes=["ps%d" % bank])
                        tk = tg + "tmpa%d" % pp
                        s.op("dve", lambda e, H=H, bank=bank, R=R, cond=cond, pp=pp: e.tensor_tensor(
                            out=tmpa[pp][R, :], in0=cx.ps[bank][R, :], in1=g2rep[R, cond, H], op=ALU.mult),
                            reads=["ps%d" % bank, tg + "g2rep"], writes=[tk])
                        s.op("dve", lambda e, H=H, R=R, ti=ti, pp=pp, ex_i=ex_i: e.scalar_tensor_tensor(
                            out=XA[R, ti, H], in0=tmpa[pp][R, :], scalar=gates[R, ti, ex_i:ex_i + 1],
                            in1=XA[R, ti, H], op0=ALU.mult, op1=ALU.add),
                            reads=[tk, tg + "gates%d" % ti, xak], writes=[xak])

        for ti, (row0, rows, cond) in enumerate(tiles):
            xak = tg + "XA%d" % ti
            R = slice(0, rows)
            if final_norm:
                emit_rstd(cx, XA[R, ti, :], rows, junk, ss, rstd, xak, tg)
                s.op("dve", lambda e, R=R, ti=ti: e.scalar_tensor_tensor(
                    out=XA[R, ti, :], in0=XA[R, ti, :], scalar=rstd[R, :], in1=finrep[R, :],
                    op0=ALU.mult, op1=ALU.mult), reads=[xak, tg + "rstd", tg + "finrep"], writes=[xak])
            xdst = dr["xout_tile"](row0, rows) if "xout_tile" in dr else dr["xout"][row0:row0 + rows, :]
            s.dma("sp", xdst, XA[R, ti, :], reads=[xak], writes=[tg + "xout%d" % ti])
        if "h1" in dr:
            h1 = dr["h1"]
            with cx.scope():
                if "nA1" in shared:
                    nA1, nm1 = shared["nA1"], shared["nm1"]
                else:
                    nA1, nm1 = emit_mod1(cx, h1, ncond, tg + "n")
                hxn = [A("hxn", [128, D], F32) for _ in range(2)]
                hTf = [A("hTf", [128, KC, 128], BF16) for _ in range(2)]
                for ti, (row0, rows, cond) in enumerate(tiles):
                    b = ti % 2
                    emit_norm_T(cx, XA[:, ti, :], rows, junk, ss, rstd, hxn[b], tg + "XA%d" % ti, tg + "hxn%d" % b, tg)
                    emit_evac_hT(cx, rows, hTf[b], nA1, nm1, cond, "+" + tg + "hTf%d" % b, tg + "n")
                    s.dma("sp", h1["tile_dst"](row0, rows).rearrange("p (kc t) -> p kc t", kc=KC),
                          hTf[b][:, :, 0:rows], reads=["+" + tg + "hTf%d" % b], writes=[tg + "h1o%d" % ti])


def lay_w1(w1):
    E = w1.shape[0]
    v = w1.reshape(E, 8, 128, 2, 8, 128)
    v = v.transpose(0, 4, 2, 1, 3, 5)
    return np.ascontiguousarray(v).reshape(E, 8, 128, 2048)


def lay_b1(b1):
    E = b1.shape[0]
    return np.ascontiguousarray(b1.reshape(E, 16, 128).transpose(2, 0, 1)).reshape(128, E * 16)


def lay_w2(w2):
    E = w2.shape[0]
    return np.ascontiguousarray(w2.reshape(E, 8, 128, 1024).transpose(0, 2, 1, 3)).reshape(E, 128, 8192)


def lay_cols(v):
    v = np.asarray(v, dtype=np.float32).reshape(-1, 8, 128)
    return np.ascontiguousarray(v.transpose(2, 1, 0))


def dft_consts():
    def cs(n, scale):
        k = np.arange(n)
        ang = 2 * np.pi * np.outer(k, k) / n
        return np.cos(ang) * scale, np.sin(ang) * scale
    cch, sch = cs(256, 1 / 16.0)
    cr, sr = cs(128, 1 / np.sqrt(128.0))
    cc, sc = cs(64, 1 / 8.0)
    c256, s256 = cs(256, 1 / 16.0)
    bdc = np.zeros((128, 128))
    bds = np.zeros((128, 128))
    for i in range(2):
        bdc[i * 64:(i + 1) * 64, i * 64:(i + 1) * 64] = cc
        bds[i * 64:(i + 1) * 64, i * 64:(i + 1) * 64] = -sc
    f = lambda a: np.ascontiguousarray(a, dtype=np.float32)
    return dict(cs_ch=f(np.concatenate([cch, sch], 1)),
                rs1=f(np.concatenate([cr, sr], 1)),
                rs2=f(np.concatenate([-sr, cr], 1)),
                bdc=f(bdc), bds=f(bds),
                c256=f(c256), s256n=f(-s256))


def emit_norm_T(cx, src, rows, junk, ss, rstd, xn, srckey, xnkey, tag, banks=(0, 1)):
    s = cx.s
    R = slice(0, rows)
    emit_rstd(cx, src[R, :], rows, junk, ss, rstd, srckey, tag)
    s.op("dve", lambda e: e.tensor_scalar(out=xn[R, :], in0=src[R, :], scalar1=rstd[R, :], scalar2=None,
                                          op0=ALU.mult), reads=[srckey, tag + "rstd"], writes=[xnkey])
    emit_transpose8(cx, xn, rows, banks[0], banks[1], xnkey)


def emit_evac_hT(cx, rows, hT, A1, B1, cond, hkey, tag, banks=(0, 1)):
    s = cx.s
    for kc in range(KC):
        bank, q = banks[kc // 4], kc % 4
        if kc // 4 == 0:
            s.op("act", lambda e, kc=kc, q=q, bank=bank: e.activation(
                out=hT[:, kc, 0:rows], in_=cx.ps[bank][:, q * 128:q * 128 + rows], func=AF.Identity,
                bias=B1[:, kc, cond:cond + 1], scale=A1[:, kc, cond:cond + 1]),
                reads=["ps%d" % bank, tag + "A1", "+" + tag + "cols"], writes=[hkey])
        else:
            s.op("dve", lambda e, kc=kc, q=q, bank=bank: e.tensor_scalar(
                out=hT[:, kc, 0:rows], in0=cx.ps[bank][:, q * 128:q * 128 + rows],
                scalar1=A1[:, kc, cond:cond + 1], scalar2=B1[:, kc, cond:cond + 1], op0=ALU.mult, op1=ALU.add),
                reads=["ps%d" % bank, tag + "A1", "+" + tag + "cols"], writes=[hkey])


def emit_norm_to_hT(cx, src, rows, hT, A1, B1, cond, junk, ss, rstd, xn, srckey, xnkey, hkey, tag):
    emit_norm_T(cx, src, rows, junk, ss, rstd, xn, srckey, xnkey, tag)
    emit_evac_hT(cx, rows, hT, A1, B1, cond, hkey, tag)


def emit_mod1(cx, dr, ncond, tag):
    s, A = cx.s, cx.A
    m1 = A("m1cols", [128, 16, ncond], F32)
    A1 = A("A1", [128, KC, ncond], F32)
    with cx.scope():
        modb_row = A("modb", [1, 6 * D], F32)
        s.dma("sp", modb_row[:], dr["modb"], writes=[tag + "modb"])
        silu_cols, _ = emit_silu_cond(cx, dr["cond_cols"], ncond, tag, False)
        emit_mod_cols(cx, dr["modw"], modb_row, silu_cols, ncond, list(range(0, 16)), m1, tag)
        n1g = A("n1g", [128, KC], F32)
        s.dma("sp", n1g[:], dr["n1g_col"], writes=[tag + "n1g"])
        for c in range(ncond):
            s.op("dve", lambda e, c=c: e.scalar_tensor_tensor(
                out=A1[:, :, c], in0=m1[:, 8:16, c], scalar=1.0, in1=n1g[:, :], op0=ALU.add, op1=ALU.mult),
                reads=["+" + tag + "cols", tag + "n1g"], writes=[tag + "A1"])
    return A1, m1


def emit_fourier(cx, dr, tag="F"):
    s, A = cx.s, cx.A
    tg = tag
    with cx.scope():
        A1, m1 = emit_mod1(cx, dr, 2, tg)
        WW = A("WW", [128, KC, 512], BF16)
        rs1 = A("rs1", [128, 256], BF16)
        rs2 = A("rs2", [128, 256], BF16)
        bdc = A("bdc", [128, 128], BF16)
        bds = A("bds", [128, 128], BF16)
        c256 = A("c256", [128, 2, 256], BF16)
        s256 = A("s256", [128, 2, 256], BF16)
        s.dma("pool", rs1[:], dr["rs1"], writes=[tg + "rs1"])
        s.dma("pool", rs2[:], dr["rs2"], writes=[tg + "rs2"])
        s.dma("pool", bdc[:], dr["bdc"], writes=[tg + "bdc"])
        s.dma("pool", bds[:], dr["bds"], writes=[tg + "bds"])
        s.dma("pool", c256[:], dr["c256"].rearrange("(k p) n -> p k n", p=128), writes=[tg + "c256"])
        s.dma("pool", s256[:], dr["s256n"].rearrange("(k p) n -> p k n", p=128), writes=[tg + "s256"])
        with cx.scope():
            wT = A("wT", [128, 2, D], F32)
            csch = A("csch", [128, 2, 512], F32)
            s.dma("sp", wT[:], dr["winT"].rearrange("(k p) n -> p k n", p=128), writes=[tg + "wT"])
            s.dma("sp", csch[:], dr["cs_ch"].rearrange("(k p) n -> p k n", p=128), writes=[tg + "csch"])
            for ic in range(KC):
                bank = 4 + ic % 4

                def emit(e, ic=ic, bank=bank):
                    e.matmul(cx.ps[bank][:, :], wT[:, 0, ic * 128:(ic + 1) * 128], csch[:, 0, :], start=True, stop=False)
                    return e.matmul(cx.ps[bank][:, :], wT[:, 1, ic * 128:(ic + 1) * 128], csch[:, 1, :],
                                    start=False, stop=True)
                s.op("pe", emit, reads=[tg + "wT", tg + "csch"], writes=["ps%d" % bank])
                s.op("act", lambda e, ic=ic, bank=bank: e.copy(out=WW[:, ic, :], in_=cx.ps[bank][:, :]),
                     reads=["ps%d" % bank], writes=["+" + tg + "WW"])

        xt = [A("xt", [128, D], F32) for _ in range(2)]
        xn = [A("xn", [128, D], F32) for _ in range(2)]
        hT = [A("hT", [128, KC, 128], BF16) for _ in range(2)]
        junk = A("junk", [128, D], F32)
        ss = A("ss", [128, 1], F32)
        rstd = A("rstd", [128, 1], F32)
        odt = dr.get("out_dt", F32)
        yo = [A("yo", [128, 512], odt) for _ in range(2)]
        lat_chunks = dr["ylat_chunks"]
        gr_per_chunk = 128 // len(lat_chunks)

        with cx.scope():
            PQc = A("PQc", [128, 2, 512], BF16)
            for m in range(2):
                b = m % 2
                s.dma("sp", xt[b][:], dr["ctx"][m * 128:(m + 1) * 128, :], writes=[tg + "xt%d" % b])
                emit_norm_to_hT(cx, xt[b], 128, hT[b], A1, m1, 0, junk, ss, rstd, xn[b],
                                tg + "xt%d" % b, tg + "xn%d" % b, "+" + tg + "hT%d" % b, tg)

                def emit(e, b=b):
                    ins = None
                    for kc in range(KC):
                        ins = e.matmul(cx.ps[2][:, :], hT[b][:, kc, :], WW[:, kc, :], start=(kc == 0),
                                       stop=(kc == KC - 1))
                    return ins
                s.op("pe", emit, reads=["+" + tg + "hT%d" % b, "+" + tg + "WW"], writes=["ps2"])
                s.op("act", lambda e, m=m: e.copy(out=PQc[:, m, :], in_=cx.ps[2][:, :]), reads=["ps2"],
                     writes=["+" + tg + "PQc"])
            for m in range(2):
                def emit(e, m=m):
                    M = slice(m * 128, (m + 1) * 128)
                    e.matmul(cx.ps[3][:, 0:256], c256[:, 0, M], PQc[:, 0, 0:256], start=True, stop=False)
                    e.matmul(cx.ps[3][:, 0:256], c256[:, 1, M], PQc[:, 1, 0:256], start=False, stop=False)
                    e.matmul(cx.ps[3][:, 0:256], s256[:, 0, M], PQc[:, 0, 256:512], start=False, stop=False)
                    return e.matmul(cx.ps[3][:, 0:256], s256[:, 1, M], PQc[:, 1, 256:512], start=False, stop=True)
                s.op("pe", emit, reads=["+" + tg + "PQc", tg + "c256", tg + "s256"], writes=["ps3"])
                s.op("dve", lambda e, m=m: e.tensor_copy(yo[m][:, 0:256], cx.ps[3][:, 0:256]), reads=["ps3"],
                     writes=[tg + "yo%d" % m])
                s.dma("sp", dr["yctx"][m * 128:(m + 1) * 128, :], yo[m][:, 0:256], reads=[tg + "yo%d" % m],
                      writes=["+yctxo"])
            if "after_ctx" in dr:
                dr["after_ctx"]()

        PQ = A("PQ", [128, 2, 128, 2, 64], BF16)
        AB = A("AB", [128, 2, 128, 128], BF16)
        xv = dr["x"].rearrange("(r c) d -> c r d", c=64)
        xt3 = xt + [A("xt", [128, D], F32)]

        def stL(c):
            s.dma("sp", xt3[c % 3][:], xv[c], writes=[tg + "xl%d" % (c % 3)])

        def stN(c):
            banks = (0, 1) if c % 2 == 0 else (4, 5)
            src_, skey, xnk = xt3[c % 3], tg + "xl%d" % (c % 3), tg + "xn%d" % (c % 2)
            emit_rstd(cx, src_[:, :], 128, junk, ss, rstd, skey, tg)
            yield
            s.op("dve", lambda e: e.tensor_scalar(out=xn[c % 2][:, :], in0=src_[:, :], scalar1=rstd[:, :], scalar2=None,
                                                  op0=ALU.mult), reads=[skey, tg + "rstd"], writes=[xnk])
            yield
            emit_transpose8(cx, xn[c % 2], 128, banks[0], banks[1], xnk)
            yield

        def stE(c):
            b = c % 2
            banks = (0, 1) if c % 2 == 0 else (4, 5)
            hkey = "+" + tg + "hT%d" % b
            for kc in range(KC):
                bank, q = banks[kc // 4], kc % 4
                if kc // 4 == 0:
                    s.op("act", lambda e, kc=kc, q=q, bank=bank: e.activation(
                        out=hT[b][:, kc, :], in_=cx.ps[bank][:, q * 128:(q + 1) * 128], func=AF.Identity,
                        bias=m1[:, kc, 1:2], scale=A1[:, kc, 1:2]),
                        reads=["ps%d" % bank, tg + "A1", "+" + tg + "cols"], writes=[hkey])
                else:
                    s.op("dve", lambda e, kc=kc, q=q, bank=bank: e.tensor_scalar(
                        out=hT[b][:, kc, :], in0=cx.ps[bank][:, q * 128:(q + 1) * 128],
                        scalar1=A1[:, kc, 1:2], scalar2=m1[:, kc, 1:2], op0=ALU.mult, op1=ALU.add),
                        reads=["ps%d" % bank, tg + "A1", "+" + tg + "cols"], writes=[hkey])
                if kc % 2 == 1:
                    yield
            bank = 2 + c % 2

            def emit(e, b=b, bank=bank):
                ins = None
                for kc in range(KC):
                    ins = e.matmul(cx.ps[bank][:, :], hT[b][:, kc, :], WW[:, kc, :], start=(kc == 0),
                                   stop=(kc == KC - 1))
                return ins
            s.op("pe", emit, reads=[hkey, "+" + tg + "WW"], writes=["ps%d" % bank])
            yield
            for pq in range(2):
                src2 = cx.ps[bank][:, pq * 256:(pq + 1) * 256].rearrange("p (l h) -> p l h", l=2)
                dst = PQ[:, pq, :, :, c].rearrange("p h l -> p l h")
                if c % 2 == 0:
                    s.op("act", lambda e, src2=src2, dst=dst: e.copy(out=dst, in_=src2), reads=["ps%d" % bank],
                         writes=["+" + tg + "PQ"])
                else:
                    s.op("dve", lambda e, src2=src2, dst=dst: e.tensor_copy(dst, src2), reads=["ps%d" % bank],
                         writes=["+" + tg + "PQ"])
                yield

        def run2(gens):
            alive = [g for g in gens if g is not None]
            while alive:
                for g in list(alive):
                    try:
                        next(g)
                    except StopIteration:
                        alive.remove(g)

        stL(0)
        stL(1)
        run2([stN(0)])
        for c in range(64):
            if c + 2 < 64:
                stL(c + 2)
            run2([stN(c + 1) if c + 1 < 64 else None, stE(c)])
        for hh in range(128):
            bank = 4 + hh % 4

            def emit(e, hh=hh, bank=bank):
                e.matmul(cx.ps[bank][:, 0:256], PQ[:, 0, hh, :, :].rearrange("p l c -> p (l c)"), rs1[:, :],
                         start=True, stop=False)
                return e.matmul(cx.ps[bank][:, 0:256], PQ[:, 1, hh, :, :].rearrange("p l c -> p (l c)"), rs2[:, :],
                                start=False, stop=True)
            s.op("pe", emit, reads=["+" + tg + "PQ", tg + "rs1", tg + "rs2"], writes=["ps%d" % bank])
            src = cx.ps[bank][:, 0:256].rearrange("p (a r) -> p a r", a=2)
            dst = AB[:, :, :, hh]
            if hh % 2 == 0:
                s.op("act", lambda e, src=src, dst=dst: e.copy(out=dst, in_=src), reads=["ps%d" % bank],
                     writes=["+" + tg + "AB"])
            else:
                s.op("dve", lambda e, src=src, dst=dst: e.tensor_copy(dst, src), reads=["ps%d" % bank],
                     writes=["+" + tg + "AB"])
        yvs = [ch.rearrange("(r c) n -> c r n", c=64) for ch in lat_chunks]
        for r4 in range(32):
            yv = yvs[(r4 * 4) // gr_per_chunk]
            rr0 = (r4 * 4) % gr_per_chunk
            bank = r4 % 2
            b = r4 % 2

            def emit(e, r4=r4, bank=bank):
                e.matmul(cx.ps[bank][:, :], bdc[:, :], AB[:, 0, r4 * 4:(r4 + 1) * 4, :].rearrange("p r h -> p (r h)"),
                         start=True, stop=False)
                return e.matmul(cx.ps[bank][:, :], bds[:, :],
                                AB[:, 1, r4 * 4:(r4 + 1) * 4, :].rearrange("p r h -> p (r h)"), start=False, stop=True)
            s.op("pe", emit, reads=["+" + tg + "AB", tg + "bdc", tg + "bds"], writes=["ps%d" % bank])
            if r4 % 2 == 0:
                s.op("act", lambda e, b=b, bank=bank: e.copy(out=yo[b][:, :], in_=cx.ps[bank][:, :]),
                     reads=["ps%d" % bank], writes=[tg + "yo%d" % b])
            else:
                s.op("dve", lambda e, b=b, bank=bank: e.tensor_copy(yo[b][:, :], cx.ps[bank][:, :]),
                     reads=["ps%d" % bank], writes=[tg + "yo%d" % b])
            for lo in range(2):
                s.dma("sp", yv[:, rr0:rr0 + 4, lo * 128:(lo + 1) * 128],
                      yo[b][lo * 64:(lo + 1) * 64, :].rearrange("p (r h) -> p r h", r=4),
                      reads=[tg + "yo%d" % b], writes=["+ylatc%d" % ((r4 * 4) // gr_per_chunk)])
            if "after_out" in dr and (r4 * 4 + 4) % gr_per_chunk == 0:
                dr["after_out"]((r4 * 4) // gr_per_chunk)


def hgrn_consts():
    t = np.arange(128)
    same = (t[:, None] // 64) == (t[None, :] // 64)
    tri_fw = (same & (t[:, None] <= t[None, :])).astype(np.float32)
    tri_bw = (same & (t[:, None] >= t[None, :])).astype(np.float32)
    su_fw = (same & (t[:, None] > t[None, :])).astype(np.float32)
    su_bw = (same & (t[:, None] < t[None, :])).astype(np.float32)
    return dict(tri_fw=tri_fw, tri_bw=tri_bw, su_fw=su_fw, su_bw=su_bw)


def emit_hgrn(cx, dr, tag="H", n_lat_tiles=64, n_ctx_tiles=2):
    s, A, nc = cx.s, cx.A, cx.nc
    tg = tag
    NL = n_lat_tiles
    pass
    with cx.scope():
        if "h_tile" not in dr:
            A1, m1 = emit_mod1(cx, dr, 2, tg)
        W = A("W5", [128, KC, 1280], BF16)
        s.dma("pool", W[:], dr["win5"].rearrange("(kc p) n -> p kc n", p=128), writes=[tg + "W"])
        tri = [A("tri", [128, 128], F32) for _ in range(2)]
        su = [A("su", [128, 128], F32) for _ in range(2)]
        for d, nm in enumerate(("fw", "bw")):
            s.dma("sp", tri[d][:], dr["tri_" + nm], writes=[tg + "tri%d" % d])
            s.dma("sp", su[d][:], dr["su_" + nm], writes=[tg + "su%d" % d])
        hng = A("hng", [128, 256], F32)
        s.dma("sp", hng[:], dr["hng_rep"], writes=[tg + "hng"])
        lb = A("lb", [128, 2, 256], F32)
        oml = A("oml", [128, 2, 256], F32)
        with cx.scope():
            lbr = A("lbr", [128, 2, 2, 256], F32)
            s.dma("sp", lbr[:], dr["lbrep"], writes=[tg + "lbr"])
            s.op("dve", lambda e: e.tensor_tensor(out=lb[:], in0=lbr[:, 1], in1=lbr[:, 0], op=ALU.subtract),
                 reads=[tg + "lbr"], writes=[tg + "lb"])
            s.op("act", lambda e: e.activation(out=lb[:], in_=lb[:], func=AF.Exp, scale=-1.0),
                 reads=[tg + "lb"], writes=[tg + "lb"])
            s.op("dve", lambda e: e.tensor_scalar(out=lb[:], in0=lb[:], scalar1=1.0, scalar2=None, op0=ALU.add),
                 reads=[tg + "lb"], writes=[tg + "lb"])
            s.op("dve", lambda e: e.reciprocal(out=lb[:], in_=lb[:]), reads=[tg + "lb"], writes=[tg + "lb"])
            s.op("dve", lambda e: e.tensor_scalar(out=oml[:], in0=lb[:], scalar1=-1.0, scalar2=1.0, op0=ALU.mult,
                                                  op1=ALU.add), reads=[tg + "lb"], writes=[tg + "oml"])

        OFW = A("OFW", [128, max(NL, 1), 256], F32)
        xt = [A("xt", [128, D], F32) for _ in range(2)]
        xn = [A("xn", [128, D], F32) for _ in range(2)]
        hT = [A("hT", [128, KC, 128], BF16) for _ in range(2)]
        junk = A("junk", [128, D], F32)
        ss = A("ss", [128, 1], F32)
        rstd = A("rstd", [128, 1], F32)
        NB = 2
        EA = [A("EA", [128, 768], F32) for _ in range(NB)]
        fg = [A("fg", [128, 256], F32) for _ in range(NB)]
        logf = [A("logf", [128, 256], F32) for _ in range(NB)]
        kf = [A("kf", [128, 256], F32) for _ in range(NB)]
        kh = [A("kh", [128, 256], BF16) for _ in range(NB)]
        kh2 = [A("kh2", [128, 256], BF16) for _ in range(NB)]
        rmask = A("rmask", [128, 2], F32)
        s.op("dve", lambda e: e.memset(rmask[:], 0.0), writes=[tg + "rmask"])
        s.op("dve", lambda e: e.memset(rmask[0:64, 0:1], 1.0), writes=[tg + "rmask"])
        s.op("dve", lambda e: e.memset(rmask[64:128, 1:2], 1.0), writes=[tg + "rmask"])
        vv = [A("vv", [128, 256], BF16) for _ in range(NB)]
        qs = [A("qs", [128, 256], F32) for _ in range(NB)]
        gg = [A("gg", [128, 256], F32) for _ in range(NB)]
        ec = [A("ec", [128, 256], F32) for _ in range(NB)]
        ebT = [[A("ebT", [128, 128], F32) for _ in range(NB)] for _ in range(2)]
        enbT = [[A("enbT", [128, 128], F32) for _ in range(NB)] for _ in range(2)]
        Z = [[A("Z", [128, 256], BF16) for _ in range(NB)] for _ in range(2)]
        ktT = [[A("ktT", [128, 128], BF16) for _ in range(NB)] for _ in range(2)]
        scm = [[A("scm", [128, 128], BF16) for _ in range(NB)] for _ in range(2)]
        S = [[A("S", [128, 128], F32) for _ in range(3)] for _ in range(2)]
        Sb = [[A("Sb", [128, 128], BF16) for _ in range(4)] for _ in range(2)]
        obuf = [A("obuf", [128, 256], F32) for _ in range(NB)]
        yout = [A("yout", [128, 256], dr.get("out_dt", F32)) for _ in range(NB)]
        ssq = A("ssq", [128, 2], F32)
        rsq = A("rsq", [128, 2], F32)
        junkB = A("junkB", [128, 256], F32)
        for hd in range(2):
            for b in range(NB):
                s.op("pool", lambda e, hd=hd, b=b: e.memset(Z[hd][b][:], 0.0), writes=[tg + "Z%d_%d" % (hd, b)])

        steps = []
        for d in range(2):
            if d == 0:
                order = [("ctx", i) for i in range(n_ctx_tiles)] + [("lat", i) for i in range(NL)]
            else:
                order = [("ctx", i) for i in reversed(range(n_ctx_tiles))] + [("lat", i) for i in reversed(range(NL))]
            for idx, (kind, i) in enumerate(order):
                steps.append((d, kind, i, idx == 0))
        scur = [0, 0]
        sbc = [0, 0]

        xt4 = xt + [A("xt", [128, D], F32) for _ in range(2)]
        hT4 = [A("hT4", [128, KC, 128], BF16) for _ in range(4)]
        hscr_t = nc.dram_tensor(cx.name("hscr"), [n_ctx_tiles + max(NL, 1), 128, KC * 128], BF16)
        hscr = [hscr_t.ap()[j_] for j_ in range(n_ctx_tiles + max(NL, 1))]

        def stageL(n):
            d, kind, i, first_of_sweep = steps[n]
            if "h_tile" in dr:
                for (dst_cols, hsrc_ap) in dr["h_tile"](kind, i):
                    s.dma("sp", hT4[n % 4][:, :, dst_cols], hsrc_ap.rearrange("p (kc t) -> p kc t", kc=KC),
                          writes=["+" + tg + "hl%d" % (n % 4)])
                return
            if d == 1:
                slot = i if kind == "ctx" else n_ctx_tiles + i
                s.dma("sp", hT4[n % 4][:], hscr[slot].rearrange("p (kc t) -> p kc t", kc=KC),
                      reads=["hscr%d" % slot], writes=["+" + tg + "hl%d" % (n % 4)])
                return
            if "x_tile" in dr:
                src = dr["x_tile"](kind, i)
            else:
                src = dr["x"][i * 128:(i + 1) * 128, :] if kind == "lat" else dr["xctx"][i * 128:(i + 1) * 128, :]
            s.dma("sp", xt4[n % 4][:], src, writes=[tg + "xl%d" % (n % 4)])

        def stageA(n):
            d, kind, i, first_of_sweep = steps[n]
            lat = kind == "lat"
            b = n % 2
            k = lambda nm: tg + nm + "%d" % b
            if d == 0 and "h_tile" not in dr:
                emit_norm_T(cx, xt4[n % 4], 128, junk, ss, rstd, xn[b], tg + "xl%d" % (n % 4), k("xn"), tg)
                yield
                emit_evac_hT(cx, 128, hT[b], A1, m1, 1 if lat else 0, "+" + k("hT"), tg)
                slot = i if kind == "ctx" else n_ctx_tiles + i
                s.dma("sp", hscr[slot].rearrange("p (kc t) -> p kc t", kc=KC), hT[b][:], reads=["+" + k("hT")],
                      writes=["hscr%d" % slot])
                hsrc, hkey_ = hT[b], "+" + k("hT")
            else:
                hsrc, hkey_ = hT4[n % 4], "+" + tg + "hl%d" % (n % 4)
            c0 = 256 if d == 0 else 512

            def emit_p(e, hsrc=hsrc, c0=c0):
                ins = None
                for kc in range(KC):
                    ins = e.matmul(cx.ps[2][:, :], hsrc[:, kc, :], W[:, kc, c0:c0 + 512], start=(kc == 0),
                                   stop=(kc == KC - 1))
                return ins
            s.op("pe", emit_p, reads=[hkey_, tg + "W"], writes=["ps2"])
            yield
            if lat:
                def emit_q(e, hsrc=hsrc, d=d):
                    ins = None
                    if d == 1:
                        for kc in range(KC):
                            ins = e.matmul(cx.ps[3][:, 0:256], hsrc[:, kc, :], W[:, kc, 1024:1280],
                                           start=(kc == 0), stop=(kc == KC - 1))
                    for hd in range(2):
                        for kc in range(KC):
                            ins = e.matmul(cx.ps[3][:, 256 + hd * 128:384 + hd * 128],
                                           W[:, kc, hd * 128:(hd + 1) * 128], hsrc[:, kc, :],
                                           start=(kc == 0), stop=(kc == KC - 1))
                    return ins
                s.op("pe", emit_q, reads=[hkey_, tg + "W"], writes=["ps3"])
                yield
            fsl = cx.ps[2][:, 0:256] if d == 0 else cx.ps[2][:, 256:512]
            isl = cx.ps[2][:, 256:512] if d == 0 else cx.ps[2][:, 0:256]
            yield
            EAb = EA[b]
            s.op("act", lambda e, EAb=EAb, fsl=fsl: e.activation(out=EAb[:, 0:256], in_=fsl, func=AF.Exp, scale=-1.0),
                 reads=["ps2"], writes=[k("EA")])
            yield
            s.op("act", lambda e, b=b, isl=isl: e.copy(out=vv[b][:], in_=isl), reads=["ps2"], writes=[k("vv")])
            yield
            if lat and d == 1:
                s.op("act", lambda e, EAb=EAb: e.activation(out=EAb[:, 256:768], in_=cx.ps[3][:, 0:512], func=AF.Exp,
                                                            scale=-1.0), reads=["ps3", k("EA")], writes=[k("EA")])
                yield
                reg = EAb[:, 0:768]
            elif lat:
                s.op("act", lambda e, EAb=EAb: e.activation(out=EAb[:, 512:768], in_=cx.ps[3][:, 256:512], func=AF.Exp,
                                                            scale=-1.0), reads=["ps3", k("EA")], writes=[k("EA")])
                yield
                reg = EAb[:, :].rearrange("p (a x) -> p a x", a=3)[:, 0:3:2, :]
            else:
                reg = EAb[:, 0:256]
            s.op("act", lambda e, reg=reg: e.activation(out=reg, in_=reg, func=AF.Ln, bias=cx.one_col[:, :], scale=1.0),
                 reads=[k("EA"), "one_col"], writes=[k("EA")])
            yield
            s.op("act", lambda e, reg=reg: e.activation(out=reg, in_=reg, func=AF.Exp, scale=-1.0),
                 reads=[k("EA")], writes=[k("EA")])
            yield
            s.op("dve", lambda e, b=b, d=d, EAb=EAb: e.tensor_tensor(out=fg[b][:], in0=EAb[:, 0:256], in1=oml[:, d, :],
                                                                     op=ALU.mult),
                 reads=[k("EA"), tg + "oml"], writes=[k("fg")])
            yield
            s.op("dve", lambda e, b=b, d=d: e.tensor_tensor(out=fg[b][:], in0=fg[b][:], in1=lb[:, d, :],
                                                            op=ALU.add),
                 reads=[k("fg"), tg + "lb"], writes=[k("fg")])
            yield
            s.op("act", lambda e, b=b: e.activation(out=logf[b][:], in_=fg[b][:], func=AF.Ln),
                 reads=[k("fg")], writes=[k("logf")])
            yield
            s.op("pool", lambda e, b=b: e.tensor_scalar(out=kf[b][:], in0=fg[b][:], scalar1=-1.0, scalar2=1.0,
                                                        op0=ALU.mult, op1=ALU.add),
                 reads=[k("fg")], writes=[k("kf")])
            yield
            yield
            if lat:
                qsl = cx.ps[3][:, 256:512]
                s.op("dve", lambda e, b=b, qsl=qsl, EAb=EAb: e.tensor_tensor(out=qs[b][:], in0=EAb[:, 512:768], in1=qsl,
                                                                             op=ALU.mult),
                     reads=[k("EA"), "ps3"], writes=[k("qs")])
                yield
                if d == 1:
                    gsl = cx.ps[3][:, 0:256]
                    s.op("dve", lambda e, b=b, gsl=gsl, EAb=EAb: e.tensor_tensor(out=gg[b][:], in0=EAb[:, 256:512],
                                                                                 in1=gsl, op=ALU.mult),
                         reads=[k("EA"), "ps3"], writes=[k("gg")])
                    yield
                    s.op("pool", lambda e, b=b: e.tensor_tensor(out=gg[b][:], in0=gg[b][:], in1=hng[:],
                                                                op=ALU.mult),
                         reads=[k("gg"), tg + "hng"], writes=[k("gg")])
                    yield

        def stageB(n):
            d, kind, i, first_of_sweep = steps[n]
            lat = kind == "lat"
            b = n % 2
            k = lambda nm: tg + nm + "%d" % b
            if first_of_sweep:
                scur[0] = scur[1] = 0
                sbc[0] = sbc[1] = 0
                for hd in range(2):
                    s.op("dve", lambda e, hd=hd: e.memset(S[hd][0][:], 0.0), writes=[tg + "S%d_0" % hd])
                    yield
            s.op("pe", lambda e, b=b, d=d: e.matmul(cx.ps[6][:, 0:256], su[d][:, :], logf[b][:, :],
                                                    start=True, stop=True),
                 reads=[k("logf"), tg + "su%d" % d], writes=["ps6"])
            yield
            s.op("act", lambda e, b=b: e.activation(out=ec[b][:], in_=cx.ps[6][:, 0:256], func=AF.Exp),
                 reads=["ps6"], writes=[k("ec")])
            yield
            s.op("dve", lambda e, b=b: e.scalar_tensor_tensor(
                out=kh[b][:], in0=kf[b][:], scalar=rmask[:, 0:1], in1=ec[b][:], op0=ALU.mult, op1=ALU.mult),
                reads=[k("kf"), k("ec"), tg + "rmask"], writes=["+" + k("kh")])
            yield
            s.op("dve", lambda e, b=b: e.scalar_tensor_tensor(
                out=kh2[b][:], in0=kf[b][:], scalar=rmask[:, 1:2], in1=ec[b][:], op0=ALU.mult, op1=ALU.mult),
                reads=[k("kf"), k("ec"), tg + "rmask"], writes=["+" + k("kh")])
            yield
            first, second = (0, 1) if d == 0 else (1, 0)
            yield

            def head_gen(hd):
                H = slice(hd * 128, (hd + 1) * 128)
                kb = lambda nm: tg + nm + "%d_%d" % (hd, b)
                hb = 4 + hd
                hbk = "ps%d" % hb
                BT, KT, SC, OO = slice(0, 128), slice(128, 256), slice(256, 384), slice(384, 512)

                def emit_bt(e, b=b, d=d, H=H, hb=hb, lat=lat):
                    ins = e.matmul(cx.ps[hb][:, BT], logf[b][:, H], tri[d][:, :], start=True, stop=True)
                    if lat:
                        ins = e.transpose(out=cx.ps[hb][:, KT], in_=kf[b][:, H], identity=cx.ident[:, :])
                    return ins
                s.op("pe", emit_bt, reads=[k("logf"), tg + "tri%d" % d, k("kf"), "ident"], writes=[hbk])
                yield
                s.op("act", lambda e, b=b, hd=hd, hb=hb: e.activation(out=ebT[hd][b][:], in_=cx.ps[hb][:, BT],
                                                                      func=AF.Exp),
                     reads=[hbk], writes=[kb("ebT")])
                yield
                if lat:
                    s.op("dve", lambda e, b=b, hd=hd, hb=hb: e.tensor_scalar(
                        out=enbT[hd][b][:], in0=cx.ps[hb][:, BT], scalar1=-87.0, scalar2=None, op0=ALU.max),
                        reads=[hbk], writes=[kb("enbT")])
                    yield
                    s.op("act", lambda e, b=b, hd=hd: e.activation(
                        out=enbT[hd][b][:], in_=enbT[hd][b][:], func=AF.Exp, scale=-1.0),
                        reads=[kb("enbT")], writes=[kb("enbT")])
                    yield
                    Zv = Z[hd][b][:, :].rearrange("p (a x) -> p a x", a=4)[:, 0:4:3, :]
                    s.op("dve", lambda e, b=b, hd=hd, H=H, Zv=Zv: e.tensor_tensor(
                        out=Zv, in0=qs[b][:, H].rearrange("p (a x) -> p a x", a=2),
                        in1=ebT[hd][b][:, :].rearrange("p (a x) -> p a x", a=2), op=ALU.mult),
                        reads=[k("qs"), kb("ebT")], writes=[kb("Z")])
                    yield
                    s.op("dve", lambda e, b=b, hd=hd, hb=hb: e.tensor_tensor(
                        out=ktT[hd][b][:], in0=cx.ps[hb][:, KT], in1=enbT[hd][b][:], op=ALU.mult),
                        reads=[hbk, kb("enbT")], writes=[kb("ktT")])
                    yield
                    s.op("pe", lambda e, b=b, hd=hd, hb=hb, Zv=Zv: e.matmul(
                        cx.ps[hb][:, SC].rearrange("p (a x) -> p a x", a=2), ktT[hd][b][:, :], Zv,
                        start=True, stop=True),
                        reads=[kb("ktT"), kb("Z")], writes=[hbk])
                    yield
                    s.op("dve", lambda e, b=b, hd=hd, hb=hb, d=d: e.tensor_tensor(
                        out=scm[hd][b][:], in0=cx.ps[hb][:, SC], in1=tri[d][:, :], op=ALU.mult),
                        reads=[hbk, tg + "tri%d" % d], writes=[kb("scm")])
                    yield
                yield
                kvb = 6 if hd == 0 else 7
                kvk = "ps%d" % kvb
                o0 = 256 if hd == 0 else 0
                kv1 = slice(o0, o0 + 128)
                kv2 = slice(o0 + 128, o0 + 256)
                P1 = slice(first * 64, first * 64 + 64)
                P2 = slice(second * 64, second * 64 + 64)

                khf = kh if first == 0 else kh2
                khs = kh2 if first == 0 else kh

                def emit_kv(e, b=b, H=H, kv1=kv1, kv2=kv2, khf=khf, khs=khs, kvb=kvb):
                    e.matmul(cx.ps[kvb][:, kv1], khf[b][:, H], vv[b][:, H], start=True, stop=True)
                    return e.matmul(cx.ps[kvb][:, kv2], khs[b][:, H], vv[b][:, H], start=True, stop=True)
                s.op("pe", emit_kv, reads=["+" + k("kh"), k("vv")], writes=[kvk])
                yield
                if d == 0:
                    c1, c2 = 63, 127
                else:
                    c1, c2 = 64, 0
                si, sm, so = scur[hd] % 3, (scur[hd] + 1) % 3, (scur[hd] + 2) % 3
                scur[hd] += 2
                kS = lambda j: tg + "S%d_%d" % (hd, j)
                if lat:
                    bi, bm = sbc[hd] % 4, (sbc[hd] + 1) % 4
                    sbc[hd] += 2
                    kSb = lambda j: tg + "Sb%d_%d" % (hd, j)
                    s.op("pool", lambda e, hd=hd, si=si, bi=bi: e.tensor_copy(Sb[hd][bi][:], S[hd][si][:]),
                         reads=[kS(si)], writes=[kSb(bi)])
                    yield
                s.op("dve", lambda e, hd=hd, b=b, si=si, sm=sm, c1=c1, kv1=kv1, kvb=kvb: e.scalar_tensor_tensor(
                    out=S[hd][sm][:], in0=S[hd][si][:], scalar=ebT[hd][b][:, c1:c1 + 1], in1=cx.ps[kvb][:, kv1],
                    op0=ALU.mult, op1=ALU.add),
                    reads=[kS(si), kb("ebT"), kvk], writes=[kS(sm)])
                yield
                if lat:
                    s.op("pool", lambda e, hd=hd, sm=sm, bm=bm: e.tensor_copy(Sb[hd][bm][:], S[hd][sm][:]),
                         reads=[kS(sm)], writes=[kSb(bm)])
                    yield
                s.op("dve", lambda e, hd=hd, b=b, sm=sm, so=so, c2=c2, kv2=kv2, kvb=kvb: e.scalar_tensor_tensor(
                    out=S[hd][so][:], in0=S[hd][sm][:], scalar=ebT[hd][b][:, c2:c2 + 1], in1=cx.ps[kvb][:, kv2],
                    op0=ALU.mult, op1=ALU.add),
                    reads=[kS(sm), kb("ebT"), kvk], writes=[kS(so)])
                yield
                if lat:
                    Zf = Z[hd][b][:, first * 128:(first + 1) * 128]
                    Zs = Z[hd][b][:, second * 128:(second + 1) * 128]

                    def emit_o(e, b=b, hd=hd, H=H, Zf=Zf, Zs=Zs, bi=bi, bm=bm, hb=hb):
                        e.matmul(cx.ps[hb][:, OO], scm[hd][b][:, :], vv[b][:, H], start=True, stop=False)
                        e.matmul(cx.ps[hb][:, OO], Zf, Sb[hd][bi][:, :], start=False, stop=False)
                        return e.matmul(cx.ps[hb][:, OO], Zs, Sb[hd][bm][:, :], start=False, stop=True)
                    s.op("pe", emit_o, reads=[kb("scm"), k("vv"), kb("Z"), kSb(bi), kSb(bm)], writes=[hbk])
                    yield
                    if d == 0:
                        s.op("act", lambda e, i=i, H=H, hb=hb: e.copy(out=OFW[:, i, H], in_=cx.ps[hb][:, OO]),
                             reads=[hbk], writes=[tg + "OFW%d_%d" % (i, hd)])
                        yield
                    else:
                        s.op("dve", lambda e, b=b, i=i, H=H, hb=hb: e.tensor_tensor(
                            out=obuf[b][:, H], in0=cx.ps[hb][:, OO], in1=OFW[:, i, H], op=ALU.add),
                            reads=[hbk, tg + "OFW%d_%d" % (i, hd)], writes=["+" + k("obuf")])
                        yield
                        s.op("act", lambda e, b=b, hd=hd, H=H: e.activation(
                            out=junk[:, H], in_=obuf[b][:, H], func=AF.Square, accum_out=ssq[:, hd:hd + 1]),
                            reads=["+" + k("obuf")], writes=[tg + "junk", "+" + tg + "ssq"])
                        yield

            yield from interleave_gen([head_gen(0), head_gen(1)])

            if lat and d == 1:
                s.op("act", lambda e: e.activation(out=rsq[:], in_=ssq[:], func=AF.Ln, bias=cx.eps_col[:, :],
                                                   scale=1.0 / 128),
                     reads=["+" + tg + "ssq", "eps_col"], writes=[tg + "rsq"])
                s.op("act", lambda e: e.activation(out=rsq[:], in_=rsq[:], func=AF.Exp, scale=-0.5),
                     reads=[tg + "rsq"], writes=[tg + "rsq"])
                for hd in range(2):
                    H = slice(hd * 128, (hd + 1) * 128)
                    s.op("dve", lambda e, b=b, hd=hd, H=H: e.scalar_tensor_tensor(
                        out=yout[b][:, H], in0=obuf[b][:, H], scalar=rsq[:, hd:hd + 1], in1=gg[b][:, H],
                        op0=ALU.mult, op1=ALU.mult),
                        reads=["+" + k("obuf"), tg + "rsq", k("gg")], writes=["+" + k("yout")])
                ydst = dr["ypre_tile"](i) if "ypre_tile" in dr else dr["ypre"][i * 128:(i + 1) * 128, :]
                ykey = dr["ypre_key"](i) if "ypre_key" in dr else tg + "ypre%d" % i
                s.dma("sp", ydst, yout[b][:], reads=["+" + k("yout")], writes=[ykey])
                if "after_out" in dr:
                    dr["after_out"](i)

        def interleave_gen(gens):
            alive = list(gens)
            while alive:
                for g in list(alive):
                    try:
                        next(g)
                    except StopIteration:
                        alive.remove(g)
                yield

        def run_interleaved(gens):
            alive = [g for g in gens if g is not None]
            while alive:
                for g in list(alive):
                    try:
                        next(g)
                    except StopIteration:
                        alive.remove(g)

        for n in range(min(3, len(steps))):
            stageL(n)
        run_interleaved([stageA(0)])
        for n in range(len(steps)):
            run_interleaved([stageA(n + 1) if n + 1 < len(steps) else None, stageB(n)])
            if n + 3 < len(steps):
                stageL(n + 3)


def lay_win5(w_in, hp):
    sec = lambda j: w_in[:, j * D + hp * 256: j * D + (hp + 1) * 256]
    return np.ascontiguousarray(np.concatenate([sec(0), sec(1), sec(3), sec(2), sec(4)], axis=1))


def _dt(nc, name, shape, kind="ExternalInput", dtype=F32):
    return nc.dram_tensor(name, list(shape), dtype, kind=kind).ap()


def build_fourier_prog():
    nc = bass.Bass("TRN2", target_bir_lowering=False)
    dr = dict(x=_dt(nc, "x", [8192, D]), ctx=_dt(nc, "ctx", [256, D]),
              yctx=_dt(nc, "yctx", [256, 256], "ExternalOutput"),
              modw=_dt(nc, "modw", [D, 6 * D]), modb=_dt(nc, "modb", [1, 6 * D]),
              cond_cols=_dt(nc, "cond_cols", [128, 8, 2]), n1g_col=_dt(nc, "n1g_col", [128, 8]),
              winT=_dt(nc, "winT", [256, D]), cs_ch=_dt(nc, "cs_ch", [256, 512]), rs1=_dt(nc, "rs1", [128, 256]),
              rs2=_dt(nc, "rs2", [128, 256]), bdc=_dt(nc, "bdc", [128, 128]), bds=_dt(nc, "bds", [128, 128]),
              c256=_dt(nc, "c256", [256, 256]), s256n=_dt(nc, "s256n", [256, 256]))
    dr["ylat_chunks"] = [_dt(nc, "ylat", [8192, 256], "ExternalOutput")]
    ident = _dt(nc, "ident", [128, 128])
    cx = Ctx(nc)
    load_consts(cx, ident)
    emit_fourier(cx, dr)
    cx.s.barrier(["sp"])
    return nc


def build_hgrn_prog():
    nc = bass.Bass("TRN2", target_bir_lowering=False)
    dr = dict(x=_dt(nc, "x", [8192, D]), xctx=_dt(nc, "xctx", [256, D]),
              ypre=_dt(nc, "ypre", [8192, 256], "ExternalOutput"),
              modw=_dt(nc, "modw", [D, 6 * D]), modb=_dt(nc, "modb", [1, 6 * D]),
              cond_cols=_dt(nc, "cond_cols", [128, 8, 2]), n1g_col=_dt(nc, "n1g_col", [128, 8]),
              win5=_dt(nc, "win5", [D, 1280]), lbrep=_dt(nc, "lbrep", [128, 2, 2, 256]),
              hng_rep=_dt(nc, "hng_rep", [128, 256]), tri_fw=_dt(nc, "tri_fw", [128, 128]),
              tri_bw=_dt(nc, "tri_bw", [128, 128]), su_fw=_dt(nc, "su_fw", [128, 128]),
              su_bw=_dt(nc, "su_bw", [128, 128]))
    ident = _dt(nc, "ident", [128, 128])
    cx = Ctx(nc)
    load_consts(cx, ident)
    emit_hgrn(cx, dr)
    cx.s.barrier(["sp"])
    return nc


def post_tiles(with_ctx):
    p0 = [(i * 128, 128, 1) for i in range(0, 8)]
    p1 = [(i * 128, 128, 1) for i in range(8, 16)]
    if with_ctx:
        p0 = p0 + [(2048, 64, 0)]
    return [p0, p1]


def build_post_prog(with_ctx, final_norm):
    nc = bass.Bass("TRN2", target_bir_lowering=False)
    T = 2112 if with_ctx else 2048
    dr = dict(xres=_dt(nc, "xres", [T, D]), ypre=_dt(nc, "ypre", [T, D]), xout=_dt(nc, "xout", [T, D], "ExternalOutput"),
              wout=_dt(nc, "wout", [D, D]), modw=_dt(nc, "modw", [D, 6 * D]), modb=_dt(nc, "modb", [1, 6 * D]),
              cond_cols=_dt(nc, "cond_cols", [128, 8, 2]), n2g_col=_dt(nc, "n2g_col", [128, 8]),
              wr=_dt(nc, "wr", [D, NE]), br=_dt(nc, "br", [1, NE]), w1r=_dt(nc, "w1r", [NE, 8, 128, 2048]),
              b1c=_dt(nc, "b1c", [128, NE * 16]), w2r=_dt(nc, "w2r", [NE, 128, 8192]), b2=_dt(nc, "b2", [NE, D]),
              fin_rep=_dt(nc, "fin_rep", [128, D]))
    ident = _dt(nc, "ident", [128, 128])
    cx = Ctx(nc)
    load_consts(cx, ident)
    for pi, tiles in enumerate(post_tiles(with_ctx)):
        emit_post(cx, tiles, 2, dr, final_norm, tagp="P%d" % pi)
    cx.s.barrier(["sp"])
    return nc


def _run(nc, in_maps):
    res = run_bass_kernel_spmd(nc, in_maps, core_ids=list(range(NCORES)))
    return res.results


_DEBUG = None


def kernel_unfused(x, c, ctx, c_ctx, mod_w, mod_b, norm1_g, norm2_g, fourier_w_in, fourier_w_out,
           hgrn_w_in, hgrn_lower_bounds, hgrn_norm_g, hgrn_w_out, router_w, router_b,
           expert_w1, expert_b1, expert_w2, expert_b2, final_norm_g):
    dbg = _DEBUG
    f32 = lambda a: np.ascontiguousarray(np.asarray(a, dtype=np.float32))
    x, c, ctx, c_ctx = f32(x), f32(c), f32(ctx), f32(c_ctx)
    mod_w, mod_b = f32(mod_w), f32(mod_b)
    norm1_g, norm2_g = f32(norm1_g), f32(norm2_g)
    ident = np.eye(128, dtype=np.float32)
    B = x.shape[0]
    cond_cols = [lay_cols(np.stack([c_ctx, c[b]])) for b in range(B)]

    fw_in = f32(fourier_w_in)[0]
    consts = dft_consts()
    ims = []
    for j in range(NCORES):
        b, g = j // 4, j % 4
        m = dict(x=x[b], ctx=ctx[b], modw=mod_w[0], modb=mod_b[0][None, :], cond_cols=cond_cols[b],
                 n1g_col=lay_cols(norm1_g[0])[:, :, 0], winT=np.ascontiguousarray(fw_in[:, g * 256:(g + 1) * 256].T),
                 ident=ident)
        m.update(consts)
        ims.append(m)
    r = _run(build_fourier_prog(), ims)
    y_lat = np.empty((B, 8192, D), np.float32)
    y_ctx = np.empty((B, 256, D), np.float32)
    for j in range(NCORES):
        b, g = j // 4, j % 4
        y_lat[b][:, g * 256:(g + 1) * 256] = r[j]["ylat"]
        y_ctx[b][:, g * 256:(g + 1) * 256] = r[j]["yctx"]

    def run_post(layer, xl, xc, yl, yc, wout, with_ctx, final_norm):
        w1r, w2r = lay_w1(f32(expert_w1[layer])), lay_w2(f32(expert_w2[layer]))
        b1c, b2 = lay_b1(f32(expert_b1[layer])), f32(expert_b2[layer])
        xl_f, yl_f = xl.reshape(-1, D), yl.reshape(-1, D)
        ims = []
        for j in range(NCORES):
            b = j // 4
            xr, yp = xl_f[j * 2048:(j + 1) * 2048], yl_f[j * 2048:(j + 1) * 2048]
            if with_ctx:
                xr = np.concatenate([xr, xc.reshape(-1, D)[j * 64:(j + 1) * 64]], 0)
                yp = np.concatenate([yp, yc.reshape(-1, D)[j * 64:(j + 1) * 64]], 0)
            ims.append(dict(xres=np.ascontiguousarray(xr), ypre=np.ascontiguousarray(yp), wout=wout, modw=mod_w[layer],
                            modb=mod_b[layer][None, :], cond_cols=cond_cols[b],
                            n2g_col=lay_cols(norm2_g[layer])[:, :, 0], wr=f32(router_w[layer]),
                            br=f32(router_b[layer])[None, :], w1r=w1r, b1c=b1c, w2r=w2r, b2=b2,
                            fin_rep=np.ascontiguousarray(np.broadcast_to(f32(final_norm_g), (128, D))), ident=ident))
        r = _run(build_post_prog(with_ctx, final_norm), ims)
        xo = np.concatenate([r[j]["xout"][0:2048] for j in range(NCORES)], 0).reshape(B, 8192, D)
        xco = None
        if with_ctx:
            xco = np.concatenate([r[j]["xout"][2048:2112] for j in range(NCORES)], 0).reshape(B, 256, D)
        return xo, xco

    if dbg is not None:
        dbg["y_lat"], dbg["y_ctx"] = y_lat, y_ctx
    x1_lat, x1_ctx = run_post(0, x, ctx, y_lat, y_ctx, f32(fourier_w_out)[0], True, False)
    if dbg is not None:
        dbg["x1_lat"], dbg["x1_ctx"] = x1_lat, x1_ctx

    hw_in = f32(hgrn_w_in)[0]
    lbr = f32(hgrn_lower_bounds)
    hng = f32(hgrn_norm_g)[0]
    hc = hgrn_consts()
    ims = []
    for j in range(NCORES):
        b, hp = j // 4, j % 4
        cols = slice(hp * 256, (hp + 1) * 256)
        m = dict(x=x1_lat[b], xctx=x1_ctx[b], modw=mod_w[1], modb=mod_b[1][None, :], cond_cols=cond_cols[b],
                 n1g_col=lay_cols(norm1_g[1])[:, :, 0], win5=lay_win5(hw_in, hp),
                 lbrep=np.ascontiguousarray(np.broadcast_to(lbr[:, :, cols], (128, 2, 2, 256))),
                 hng_rep=np.ascontiguousarray(np.broadcast_to(hng[cols], (128, 256))), ident=ident)
        m.update(hc)
        ims.append(m)
    r = _run(build_hgrn_prog(), ims)
    y1 = np.empty((B, 8192, D), np.float32)
    for j in range(NCORES):
        b, hp = j // 4, j % 4
        y1[b][:, hp * 256:(hp + 1) * 256] = r[j]["ypre"]

    if dbg is not None:
        dbg["y1"] = y1
    out, _ = run_post(1, x1_lat, None, y1, None, f32(hgrn_w_out)[0], False, True)
    return out


GROUPS = [[0, 1, 2, 3], [4, 5, 6, 7]]


def build_fused_prog():
    nc = bass.Bass("TRN2", target_bir_lowering=False)
    E = lambda name, shape: _dt(nc, name, shape)
    I = lambda name, shape, dt=F32: nc.dram_tensor(name, list(shape), dt)
    ext = dict(
        x=E("x", [8192, D]), ctx=E("ctx", [256, D]), xres0=E("xres0", [2112, D]),
        cond_cols=E("cond_cols", [128, 8, 2]), sel=E("sel", [128, 4]), ident=E("ident", [128, 128]),
        modw0=E("modw0", [D, 6 * D]), modb0=E("modb0", [1, 6 * D]), modw1=E("modw1", [D, 6 * D]),
        modb1=E("modb1", [1, 6 * D]),
        n1g0=E("n1g0", [128, 8]), n1g1=E("n1g1", [128, 8]), n2g0=E("n2g0", [128, 8]), n2g1=E("n2g1", [128, 8]),
        winT=E("winT", [256, D]), fwout=E("fwout", [D, D]), hwout=E("hwout", [D, D]),
        win5=E("win5", [D, 1280]), lbrep=E("lbrep", [128, 2, 2, 256]), hng_rep=E("hng_rep", [128, 256]),
        fin_rep=E("fin_rep", [128, D]))
    for nm, shp in (("cs_ch", [256, 512]), ("rs1", [128, 256]), ("rs2", [128, 256]), ("bdc", [128, 128]),
                    ("bds", [128, 128]), ("c256", [256, 256]), ("s256n", [256, 256]), ("tri_fw", [128, 128]),
                    ("tri_bw", [128, 128]), ("su_fw", [128, 128]), ("su_bw", [128, 128])):
        ext[nm] = E(nm, shp)
    for l in range(2):
        ext["wr%d" % l] = E("wr%d" % l, [D, NE])
        ext["br%d" % l] = E("br%d" % l, [1, NE])
        ext["w1r%d" % l] = E("w1r%d" % l, [NE, 8, 128, 2048])
        ext["b1c%d" % l] = E("b1c%d" % l, [128, NE * 16])
        ext["w2r%d" % l] = E("w2r%d" % l, [NE, 128, 8192])
        ext["b2%d" % l] = E("b2%d" % l, [NE, D])
    xout = _dt(nc, "xout", [2048, D], "ExternalOutput")

    yF = [I("yF%d" % c, [2048, 256], BF16) for c in range(4)]
    yFc = I("yFc", [256, 256], BF16)
    G1 = [I("G1_%d" % c, [4 * 2048, 256], BF16) for c in range(4)]
    G1c = I("G1c", [4 * 256, 256], BF16)
    x1 = [I("x1_%d" % c, [256, D]) for c in range(8)]
    x1c = I("x1c", [64, D])
    h1s = [I("h1s_%d" % c, [4 * 128, KC * 128], BF16) for c in range(4)]
    h1sc = I("h1sc", [128, KC * 64], BF16)
    G2 = [I("G2_%d" % c, [4 * 512, KC * 128], BF16) for c in range(4)]
    G2c = I("G2c", [4 * 128, KC * 64], BF16)
    yH = [I("yH%d" % c, [2048, 256], BF16) for c in range(4)]
    G3 = [I("G3_%d" % c, [4 * 2048, 256], BF16) for c in range(4)]

    cx = Ctx(nc)
    s = cx.s
    load_consts(cx, ext["ident"])

    drF = dict(x=ext["x"], ctx=ext["ctx"], yctx=yFc.ap(), ylat_chunks=[t.ap() for t in yF], out_dt=BF16,
               modw=ext["modw0"], modb=ext["modb0"], cond_cols=ext["cond_cols"], n1g_col=ext["n1g0"],
               winT=ext["winT"])
    for nm in ("cs_ch", "rs1", "rs2", "bdc", "bds", "c256", "s256n"):
        drF[nm] = ext[nm]
    drF["after_out"] = lambda c: s.collective("AllGather", [yF[c].ap().opt()], [G1[c].ap().opt()], GROUPS,
                                              reads=["+ylatc%d" % c])
    drF["after_ctx"] = lambda: s.collective("AllGather", [yFc.ap().opt()], [G1c.ap().opt()], GROUPS,
                                            reads=["+yctxo"])
    emit_fourier(cx, drF)
    s.barrier()

    def x1_tile(row0, rows):
        if row0 >= 2048:
            return x1c.ap()[0:rows, :]
        return x1[row0 // 256].ap()[row0 % 256:row0 % 256 + rows, :]

    def cands0(row0, rows):
        if row0 >= 2048:
            v = G1c.ap().rearrange("(g r) c -> r g c", g=4)
            return [v[64 * k:64 * k + rows] for k in range(4)]
        return [G1[k].ap().rearrange("(g r) c -> r g c", g=4)[row0:row0 + rows] for k in range(4)]

    def post_dr(l, wout, cands, xres_tile, xout_tile):
        return dict(ypre_cands=cands, sel=ext["sel"], xres_tile=xres_tile, xout_tile=xout_tile, wout=wout,
                    modw=ext["modw%d" % l], modb=ext["modb%d" % l], cond_cols=ext["cond_cols"],
                    n2g_col=ext["n2g%d" % l], wr=ext["wr%d" % l], br=ext["br%d" % l], w1r=ext["w1r%d" % l],
                    b1c=ext["b1c%d" % l], w2r=ext["w2r%d" % l], b2=ext["b2%d" % l], fin_rep=ext["fin_rep"])

    def h1_dst(row0, rows):
        if row0 >= 2048:
            return h1sc.ap()[:, :]
        lt = row0 // 128
        return h1s[lt // 4].ap()[(lt % 4) * 128:(lt % 4) * 128 + 128, :]

    dr0 = post_dr(0, ext["fwout"], cands0, lambda row0, rows: ext["xres0"][row0:row0 + rows, :], x1_tile)
    dr0["h1"] = dict(modw=ext["modw1"], modb=ext["modb1"], cond_cols=ext["cond_cols"], n1g_col=ext["n1g1"],
                     tile_dst=h1_dst)
    with cx.scope():
        shared0 = emit_post_params(cx, dr0, 2, "L0")
        shared0["nA1"], shared0["nm1"] = emit_mod1(cx, dr0["h1"], 2, "L0n")
        for pi, tiles in enumerate(post_tiles(True)):
            emit_post(cx, tiles, 2, dr0, False, tagp="P0%d" % pi, shared=shared0)
            for c in range(2 * pi, 2 * pi + 2):
                s.collective("AllGather", [h1s[c].ap().opt()], [G2[c].ap().opt()], GROUPS)
            if pi == 0:
                s.collective("AllGather", [h1sc.ap().opt()], [G2c.ap().opt()], GROUPS)
    s.barrier()

    def hx_tile(kind, i):
        if kind == "ctx":
            return [(slice(64 * q, 64 * q + 64), G2c.ap()[(2 * i + q) * 128:(2 * i + q) * 128 + 128, :])
                    for q in range(2)]
        r, lt = i // 16, i % 16
        return [(slice(0, 128), G2[lt // 4].ap()[r * 512 + (lt % 4) * 128:r * 512 + (lt % 4) * 128 + 128, :])]

    drH = dict(h_tile=hx_tile, ypre_tile=lambda i: yH[i // 16].ap()[(128 * i) % 2048:(128 * i) % 2048 + 128, :],
               out_dt=BF16, modw=ext["modw1"], modb=ext["modb1"], cond_cols=ext["cond_cols"], n1g_col=ext["n1g1"],
               win5=ext["win5"], lbrep=ext["lbrep"], hng_rep=ext["hng_rep"])
    for nm in ("tri_fw", "tri_bw", "su_fw", "su_bw"):
        drH[nm] = ext[nm]
    drH["ypre_key"] = lambda i: "+yHc%d" % (i // 16)

    def after_h(i):
        if i % 16 == 0:
            s.collective("AllGather", [yH[i // 16].ap().opt()], [G3[i // 16].ap().opt()], GROUPS,
                         reads=["+yHc%d" % (i // 16)])
    drH["after_out"] = after_h
    emit_hgrn(cx, drH)
    s.barrier()

    cands1 = lambda row0, rows: [G3[k].ap().rearrange("(g r) c -> r g c", g=4)[row0:row0 + rows] for k in range(4)]
    dr1 = post_dr(1, ext["hwout"], cands1, x1_tile, lambda row0, rows: xout[row0:row0 + rows, :])
    with cx.scope():
        shared1 = emit_post_params(cx, dr1, 2, "L1")
        for pi, tiles in enumerate(post_tiles(False)):
            emit_post(cx, tiles, 2, dr1, True, tagp="P1%d" % pi, shared=shared1)
    s.barrier(["sp"])
    return nc


def kernel(x, c, ctx, c_ctx, mod_w, mod_b, norm1_g, norm2_g, fourier_w_in, fourier_w_out,
           hgrn_w_in, hgrn_lower_bounds, hgrn_norm_g, hgrn_w_out, router_w, router_b,
           expert_w1, expert_b1, expert_w2, expert_b2, final_norm_g):
    f32 = lambda a: np.ascontiguousarray(np.asarray(a, dtype=np.float32))
    x, c, ctx, c_ctx = f32(x), f32(c), f32(ctx), f32(c_ctx)
    mod_w, mod_b = f32(mod_w), f32(mod_b)
    norm1_g, norm2_g = f32(norm1_g), f32(norm2_g)
    B = x.shape[0]
    shared = dict(ident=np.eye(128, dtype=np.float32),
                  modw0=mod_w[0], modb0=mod_b[0][None, :], modw1=mod_w[1], modb1=mod_b[1][None, :],
                  n1g0=lay_cols(norm1_g[0])[:, :, 0], n1g1=lay_cols(norm1_g[1])[:, :, 0],
                  n2g0=lay_cols(norm2_g[0])[:, :, 0], n2g1=lay_cols(norm2_g[1])[:, :, 0],
                  fwout=f32(fourier_w_out)[0], hwout=f32(hgrn_w_out)[0],
                  fin_rep=np.ascontiguousarray(np.broadcast_to(f32(final_norm_g), (128, D))))
    shared.update(dft_consts())
    shared.update(hgrn_consts())
    for l in range(2):
        shared["wr%d" % l] = f32(router_w[l])
        shared["br%d" % l] = f32(router_b[l])[None, :]
        shared["w1r%d" % l] = lay_w1(f32(expert_w1[l]))
        shared["b1c%d" % l] = lay_b1(f32(expert_b1[l]))
        shared["w2r%d" % l] = lay_w2(f32(expert_w2[l]))
        shared["b2%d" % l] = f32(expert_b2[l])
    fw_in = f32(fourier_w_in)[0]
    hw_in = f32(hgrn_w_in)[0]
    lbr = f32(hgrn_lower_bounds)
    hng = f32(hgrn_norm_g)[0]
    x_f, ctx_f = x.reshape(-1, D), ctx.reshape(-1, D)
    ims = []
    for j in range(NCORES):
        b, g = j // 4, j % 4
        cols = slice(g * 256, (g + 1) * 256)
        sel = np.zeros((128, 4), np.float32)
        sel[:, g] = 1.0
        m = dict(shared)
        m.update(x=x[b], ctx=ctx[b],
                 xres0=np.ascontiguousarray(np.concatenate([x_f[j * 2048:(j + 1) * 2048], ctx_f[j * 64:(j + 1) * 64]], 0)),
                 cond_cols=lay_cols(np.stack([c_ctx, c[b]])), sel=sel,
                 winT=np.ascontiguousarray(fw_in[:, cols].T), win5=lay_win5(hw_in, g),
                 lbrep=np.ascontiguousarray(np.broadcast_to(lbr[:, :, cols], (128, 2, 2, 256))),
                 hng_rep=np.ascontiguousarray(np.broadcast_to(hng[cols], (128, 256))))
        ims.append(m)
    r = _run(build_fused_prog(), ims)
    return np.concatenate([r[j]["xout"] for j in range(NCORES)], 0).reshape(B, 8192, D)
```
